# Optimizing a Trainium2 kernel written in Bass

```python
import math
import jax, jax.numpy as jnp
from jax import lax
import numpy as np

D_MODEL = 1024
BATCH = 16
SEQ = 2048
DEPTH = 1

ATTN_GROUPS = ((128, 1), (512, 4), (2048, 16))
HEADS_PER_GROUP = 4
HEAD_DIM = 128
N_ATTN_HEADS = HEADS_PER_GROUP * len(ATTN_GROUPS)
ATTN_OUT = HEADS_PER_GROUP * HEAD_DIM
Q_DIM = N_ATTN_HEADS * HEAD_DIM
ROPE_DIM = HEAD_DIM // 4
ROPE_THETA = 500000.0

SSM_INNER = D_MODEL
SSM_HEADDIM = 64
SSM_HEADS = SSM_INNER // SSM_HEADDIM
SSM_GROUPS = 4
SSM_STATE = 128
SSM_CONV = 4
SSM_CHUNK = 128
SSM_CONV_DIM = SSM_INNER + 2 * SSM_GROUPS * SSM_STATE

IN_DIM = 3 * Q_DIM + SSM_INNER + SSM_CONV_DIM + SSM_HEADS + 2 * D_MODEL

N_EXPERTS = 256
TOP_K = 8
N_EXPERT_GROUPS = 8
TOPK_GROUPS = 4
EXPERT_FF = 256
SHARED_FF = 256
ROUTED_SCALE = 2.5
MOE_BLOCK = 128

NORM_EPS = 1e-6
N_MOD = 6

kernel_name = "hybrid_dilated_attn_ssd_moe_block"


def rms_norm(x, w):
    xf = x.astype(jnp.float32)
    y = xf * lax.rsqrt(jnp.mean(xf * xf, axis=-1, keepdims=True) + NORM_EPS)
    return (y * w.astype(jnp.float32)).astype(x.dtype)


def partial_rope(x, positions):
    half = ROPE_DIM // 2
    inv_freq = ROPE_THETA ** (-jnp.arange(half, dtype=jnp.float32) / half)
    ang = positions.astype(jnp.float32)[..., None] * inv_freq
    cos = jnp.cos(ang)[:, :, None, :]
    sin = jnp.sin(ang)[:, :, None, :]
    xr = x[..., :ROPE_DIM].astype(jnp.float32)
    x1, x2 = xr[..., :half], xr[..., half:]
    rot = jnp.concatenate([x1 * cos - x2 * sin, x2 * cos + x1 * sin], axis=-1)
    return jnp.concatenate([rot.astype(x.dtype), x[..., ROPE_DIM:]], axis=-1)


def dilated_window_attention(q, k, v, window, dilation):
    b, s, h, e = q.shape
    blk = window // dilation
    n_sub = s // dilation
    nb = -(-n_sub // blk)
    n_pad = nb * blk

    def strided(t):
        t = t.reshape(b, n_sub, dilation, h, e).transpose(0, 2, 1, 3, 4)
        return jnp.pad(t, ((0, 0), (0, 0), (0, n_pad - n_sub), (0, 0), (0, 0)))

    def banded(t):
        t = jnp.pad(t, ((0, 0), (0, 0), (blk, 0), (0, 0), (0, 0))).reshape(b, dilation, nb + 1, blk, h, e)
        return jnp.concatenate([t[:, :, :-1], t[:, :, 1:]], axis=3)

    qb = strided(q).reshape(b, dilation, nb, blk, h, e)
    kb = banded(strided(k))
    vb = banded(strided(v))
    scores = jnp.einsum("brnqhe,brnkhe->brnhqk", qb, kb,
                        preferred_element_type=jnp.float32) * (1.0 / math.sqrt(e))
    qi = jnp.arange(blk)[:, None]
    kj = jnp.arange(2 * blk)[None, :]
    k_abs = jnp.arange(nb)[:, None, None] * blk - blk + kj[None]
    valid = (kj >= qi) & (kj <= qi + blk) & (k_abs >= 0)
    scores = jnp.where(valid[None, None, :, None], scores, -jnp.inf)
    m = jnp.max(scores, axis=-1)
    p = jnp.exp(scores - m[..., None])
    l = jnp.sum(p, axis=-1)
    o = jnp.einsum("brnhqk,brnkhe->brnqhe", p, vb.astype(jnp.float32))
    o = o / l.transpose(0, 1, 2, 4, 3)[..., None]
    o = o.reshape(b, dilation, n_pad, h, e)[:, :, :n_sub].transpose(0, 2, 1, 3, 4).reshape(b, s, h, e)

    def unblock(t):
        t = t.transpose(0, 1, 2, 4, 3).reshape(b, dilation, n_pad, h)[:, :, :n_sub]
        return t.transpose(0, 2, 1, 3).reshape(b, s, h)

    return o, unblock(m), unblock(l)


def causal_depthwise_conv(x, w, bias):
    kw, ch = w.shape
    y = lax.conv_general_dilated(x, w[:, None, :], window_strides=(1,), padding=[(kw - 1, 0)],
                                 dimension_numbers=("NWC", "WIO", "NWC"), feature_group_count=ch)
    return y + bias


def segsum(a):
    cs = jnp.cumsum(a, axis=-1)
    n = a.shape[-1]
    mask = jnp.tril(jnp.ones((n, n), dtype=bool))
    return jnp.where(mask, cs[..., :, None] - cs[..., None, :], -jnp.inf)


def ssd_chunked(xh, dt, A, Bm, Cm):
    b, s, nh, hp = xh.shape
    g, n = Bm.shape[2], Bm.shape[3]
    hpg = nh // g
    l = SSM_CHUNK
    nc = s // l
    x = (xh * dt[..., None]).reshape(b, nc, l, g, hpg, hp)
    a = (dt * A).reshape(b, nc, l, g, hpg).transpose(0, 1, 3, 4, 2)
    Bc = Bm.reshape(b, nc, l, g, n)
    Cc = Cm.reshape(b, nc, l, g, n)
    a_cs = jnp.cumsum(a, axis=-1)
    decay_in = jnp.exp(segsum(a))
    cb = jnp.einsum("bclgn,bcsgn->bcgls", Cc, Bc)
    y_diag = jnp.einsum("bcghls,bcsghp->bclghp", cb[:, :, :, None] * decay_in, x)
    decay_states = jnp.exp(a_cs[..., -1:] - a_cs)
    states = jnp.einsum("bclgn,bcghl,bclghp->bcghpn", Bc, decay_states, x)
    chunk_decay = jnp.exp(a_cs[..., -1])

    def step(h_prev, inp):
        st, dec = inp
        return h_prev * dec[..., None, None] + st, h_prev

    h0 = jnp.zeros((b, g, hpg, hp, n), dtype=jnp.float32)
    _, prev = lax.scan(step, h0, (jnp.moveaxis(states, 1, 0), jnp.moveaxis(chunk_decay, 1, 0)))
    prev = jnp.moveaxis(prev, 0, 1)
    y_off = jnp.einsum("bclgn,bcghpn,bcghl->bclghp", Cc, prev, jnp.exp(a_cs))
    return (y_diag + y_off).reshape(b, s, nh, hp)


def ssd_mixer(z, xbc, dt_raw, conv_w, conv_b, dt_bias, a_log, d_skip, ssm_norm_w):
    b, s, _ = z.shape
    xbc = jax.nn.silu(causal_depthwise_conv(xbc, conv_w, conv_b)).astype(jnp.float32)
    xs = xbc[..., :SSM_INNER]
    Bm = xbc[..., SSM_INNER:SSM_INNER + SSM_GROUPS * SSM_STATE].reshape(b, s, SSM_GROUPS, SSM_STATE)
    Cm = xbc[..., SSM_INNER + SSM_GROUPS * SSM_STATE:].reshape(b, s, SSM_GROUPS, SSM_STATE)
    xh = xs.reshape(b, s, SSM_HEADS, SSM_HEADDIM)
    dt = jax.nn.softplus(dt_raw.astype(jnp.float32) + dt_bias.astype(jnp.float32))
    A = -jnp.exp(a_log.astype(jnp.float32))
    y = ssd_chunked(xh, dt, A, Bm, Cm) + d_skip.astype(jnp.float32)[:, None] * xh
    y = y.reshape(b, s, SSM_INNER) * jax.nn.silu(z.astype(jnp.float32))
    yg = y.reshape(b, s, SSM_GROUPS, -1)
    yg = yg * lax.rsqrt(jnp.mean(yg * yg, axis=-1, keepdims=True) + NORM_EPS)
    return (yg.reshape(b, s, SSM_INNER) * ssm_norm_w.astype(jnp.float32)).astype(z.dtype)


def hybrid_mixer(u, positions, w_in, conv_w, conv_b, dt_bias, a_log, d_skip, ssm_norm_w,
                 w_branch_attn, w_branch_ssm, w_out):
    b, s, _ = u.shape
    proj = jnp.einsum("bsd,df->bsf", u, w_in)
    cuts = np.cumsum([Q_DIM, Q_DIM, Q_DIM, SSM_INNER, SSM_CONV_DIM, SSM_HEADS]).tolist()
    q, k, v, z, xbc, dt_raw, gate_logits = jnp.split(proj, cuts, axis=-1)
    q = partial_rope(q.reshape(b, s, N_ATTN_HEADS, HEAD_DIM), positions)
    k = partial_rope(k.reshape(b, s, N_ATTN_HEADS, HEAD_DIM), positions)
    v = v.reshape(b, s, N_ATTN_HEADS, HEAD_DIM)
    outs, maxes, dens = [], [], []
    for gi, (win, dil) in enumerate(ATTN_GROUPS):
        sl = slice(gi * HEADS_PER_GROUP, (gi + 1) * HEADS_PER_GROUP)
        o, m, l = dilated_window_attention(q[:, :, sl], k[:, :, sl], v[:, :, sl], win, dil)
        outs.append(o); maxes.append(m); dens.append(l)
    m_all = jnp.stack(maxes)
    wts = jnp.stack(dens) * jnp.exp(m_all - jnp.max(m_all, axis=0))
    attn = jnp.einsum("gbsh,gbshe->bshe", wts, jnp.stack(outs)) / jnp.sum(wts, axis=0)[..., None]
    attn = attn.reshape(b, s, ATTN_OUT).astype(u.dtype)
    ssm = ssd_mixer(z, xbc, dt_raw, conv_w, conv_b, dt_bias, a_log, d_skip, ssm_norm_w)
    gate_a, gate_s = jnp.split(gate_logits, 2, axis=-1)
    merged = (jax.nn.sigmoid(gate_a) * jnp.einsum("bsf,fd->bsd", attn, w_branch_attn)
              + jax.nn.sigmoid(gate_s) * jnp.einsum("bsf,fd->bsd", ssm, w_branch_ssm))
    return jnp.einsum("bsd,de->bse", merged, w_out)


def moe_ffn(u, w_router, router_bias, w_gate_e, w_up_e, w_down_e, w_gate_s, w_up_s, w_down_s):
    b, s, d = u.shape
    t = u.reshape(b * s, d)
    n_tok = b * s
    scores = jax.nn.sigmoid(jnp.dot(t, w_router, preferred_element_type=jnp.float32))
    biased = scores + router_bias.astype(jnp.float32)
    grp = biased.reshape(n_tok, N_EXPERT_GROUPS, N_EXPERTS // N_EXPERT_GROUPS)
    grp_score = jnp.sum(lax.top_k(grp, 2)[0], axis=-1)
    _, grp_idx = lax.top_k(grp_score, TOPK_GROUPS)
    grp_mask = jnp.sum(jax.nn.one_hot(grp_idx, N_EXPERT_GROUPS, dtype=jnp.float32), axis=1) > 0
    masked = jnp.where(jnp.repeat(grp_mask, N_EXPERTS // N_EXPERT_GROUPS, axis=1), biased, -jnp.inf)
    _, idx = lax.top_k(masked, TOP_K)
    wsel = jnp.take_along_axis(scores, idx, axis=1)
    wsel = wsel / jnp.sum(wsel, axis=-1, keepdims=True) * ROUTED_SCALE

    n_assign = n_tok * TOP_K
    flat_e = idx.reshape(-1)
    flat_tok = jnp.repeat(jnp.arange(n_tok, dtype=jnp.int32), TOP_K)
    flat_w = wsel.reshape(-1)
    order = jnp.argsort(flat_e)
    se = flat_e[order]
    counts = jnp.bincount(flat_e, length=N_EXPERTS)
    padded = (counts + MOE_BLOCK - 1) // MOE_BLOCK * MOE_BLOCK
    pad_end = jnp.cumsum(padded)
    pad_start = pad_end - padded
    start = jnp.cumsum(counts) - counts
    dest = pad_start[se] + (jnp.arange(n_assign) - start[se])
    n_rows = n_assign + N_EXPERTS * MOE_BLOCK
    n_blk = n_rows // MOE_BLOCK
    row_tok = jnp.zeros((n_rows,), jnp.int32).at[dest].set(flat_tok[order])
    row_w = jnp.zeros((n_rows,), jnp.float32).at[dest].set(flat_w[order])
    blk_e = jnp.minimum(jnp.searchsorted(pad_end, jnp.arange(n_blk) * MOE_BLOCK, side="right"),
                        N_EXPERTS - 1)

    def expert_block(acc, inp):
        tok, wt, e = inp
        xb = t[tok]
        hb = jax.nn.silu(xb @ w_gate_e[e]) * (xb @ w_up_e[e])
        yb = (hb @ w_down_e[e]).astype(jnp.float32) * wt[:, None]
        return acc.at[tok].add(yb), None

    routed, _ = lax.scan(expert_block, jnp.zeros((n_tok, d), jnp.float32),
                         (row_tok.reshape(n_blk, MOE_BLOCK), row_w.reshape(n_blk, MOE_BLOCK), blk_e))
    shared = (jax.nn.silu(t @ w_gate_s) * (t @ w_up_s)) @ w_down_s
    return (routed + shared.astype(jnp.float32)).astype(u.dtype).reshape(b, s, d)


def setup_inputs(seed: int = 0) -> dict:
    key = jax.random.key(seed)
    ks = jax.random.split(key, 26)
    f32 = jnp.float32
    L = DEPTH

    def nrm(k, shape, scale):
        return jax.random.normal(k, shape, f32) * scale

    dt0 = jnp.exp(jax.random.uniform(ks[10], (L, SSM_HEADS), f32) * (math.log(0.1) - math.log(0.001))
                  + math.log(0.001))
    return {
        "x": nrm(ks[0], (BATCH, SEQ, D_MODEL), 1.0),
        "c": nrm(ks[1], (BATCH, D_MODEL), 1.0),
        "positions": (jnp.arange(SEQ, dtype=jnp.int32)[None, :]
                      + jax.random.randint(ks[2], (BATCH, 1), 0, 4096, dtype=jnp.int32)),
        "w_mod": nrm(ks[3], (L, D_MODEL, N_MOD * D_MODEL), 0.5 * D_MODEL ** -0.5),
        "b_mod": nrm(ks[4], (L, N_MOD * D_MODEL), 0.02),
        "norm_mix_w": 1.0 + nrm(ks[5], (L, D_MODEL), 0.05),
        "norm_ffn_w": 1.0 + nrm(ks[6], (L, D_MODEL), 0.05),
        "w_in": nrm(ks[7], (L, D_MODEL, IN_DIM), D_MODEL ** -0.5),
        "conv_w": nrm(ks[8], (L, SSM_CONV, SSM_CONV_DIM), SSM_CONV ** -0.5),
        "conv_b": nrm(ks[9], (L, SSM_CONV_DIM), 0.02),
        "dt_bias": dt0 + jnp.log(-jnp.expm1(-dt0)),
        "a_log": jnp.log(jax.random.uniform(ks[11], (L, SSM_HEADS), f32, 1.0, 16.0)),
        "d_skip": 1.0 + nrm(ks[12], (L, SSM_HEADS), 0.1),
        "ssm_norm_w": 1.0 + nrm(ks[13], (L, SSM_INNER), 0.05),
        "w_branch_attn": nrm(ks[14], (L, ATTN_OUT, D_MODEL), ATTN_OUT ** -0.5),
        "w_branch_ssm": nrm(ks[15], (L, SSM_INNER, D_MODEL), SSM_INNER ** -0.5),
        "w_out": nrm(ks[16], (L, D_MODEL, D_MODEL), D_MODEL ** -0.5),
        "w_router": nrm(ks[17], (L, D_MODEL, N_EXPERTS), D_MODEL ** -0.5),
        "router_bias": nrm(ks[18], (L, N_EXPERTS), 0.01),
        "w_gate_e": nrm(ks[19], (L, N_EXPERTS, D_MODEL, EXPERT_FF), D_MODEL ** -0.5),
        "w_up_e": nrm(ks[20], (L, N_EXPERTS, D_MODEL, EXPERT_FF), D_MODEL ** -0.5),
        "w_down_e": nrm(ks[21], (L, N_EXPERTS, EXPERT_FF, D_MODEL), EXPERT_FF ** -0.5),
        "w_gate_s": nrm(ks[22], (L, D_MODEL, SHARED_FF), D_MODEL ** -0.5),
        "w_up_s": nrm(ks[23], (L, D_MODEL, SHARED_FF), D_MODEL ** -0.5),
        "w_down_s": nrm(ks[24], (L, SHARED_FF, D_MODEL), SHARED_FF ** -0.5),
        "norm_final_w": 1.0 + nrm(ks[25], (D_MODEL,), 0.05),
    }


def reference(x, c, positions, w_mod, b_mod, norm_mix_w, norm_ffn_w, w_in, conv_w, conv_b, dt_bias,
              a_log, d_skip, ssm_norm_w, w_branch_attn, w_branch_ssm, w_out, w_router, router_bias,
              w_gate_e, w_up_e, w_down_e, w_gate_s, w_up_s, w_down_s, norm_final_w):
    h = x
    cond = jax.nn.silu(c)
    for i in range(DEPTH):
        mod = (jnp.dot(cond, w_mod[i]) + b_mod[i])[:, None, :]
        sh1, sc1, g1, sh2, sc2, g2 = jnp.split(mod, N_MOD, axis=-1)
        u = rms_norm(h, norm_mix_w[i]) * (1.0 + sc1) + sh1
        h = h + g1 * hybrid_mixer(u, positions, w_in[i], conv_w[i], conv_b[i], dt_bias[i], a_log[i],
                                  d_skip[i], ssm_norm_w[i], w_branch_attn[i], w_branch_ssm[i], w_out[i])
        u = rms_norm(h, norm_ffn_w[i]) * (1.0 + sc2) + sh2
        h = h + g2 * moe_ffn(u, w_router[i], router_bias[i], w_gate_e[i], w_up_e[i], w_down_e[i],
                             w_gate_s[i], w_up_s[i], w_down_s[i])
    return rms_norm(h, norm_final_w)
```

```python
import numpy as np
import ml_dtypes
from contextlib import ExitStack
import concourse.bass as bass
import concourse.mybir as mybir
from concourse.bass_utils import run_bass_kernel_spmd

F32 = mybir.dt.float32
BF16 = mybir.dt.bfloat16
I32 = mybir.dt.int32
U32 = mybir.dt.uint32
ALU = mybir.AluOpType
AF = mybir.ActivationFunctionType
AX = mybir.AxisListType

NCORES = 8
D = 1024
SEQ = 2048
NSEQ = 2
NTOK = NSEQ * SEQ
IN_DIM = 9744
Q0, K0, V0, Z0, X0, DT0, G0 = 0, 1536, 3072, 4608, 5632, 7680, 7696
NE = 256
CAP = 512
EPS = 1e-6
NEG = -30000.0


class Tok:
    __slots__ = ("key", "sem", "val")

    def __init__(self, key, sem, val):
        self.key, self.sem, self.val = key, sem, val


class T:
    __slots__ = ("name", "w", "r", "multi", "ws")

    def __init__(self, name="", multi=False):
        self.name, self.w, self.r, self.multi, self.ws = name, None, {}, multi, {}


class Sched:
    def __init__(self, nc, es, nslots=8):
        self.nc = nc
        self.eng = dict(pe=nc.tensor, act=nc.scalar, dve=nc.vector, pool=nc.gpsimd, sp=nc.sync)
        self.sem = {k: es.enter_context(nc.semaphore("sem_" + k)) for k in ("pe", "act", "dve", "pool")}
        self.cnt = dict.fromkeys(self.sem, 0)
        self.seen = {k: {} for k in self.eng}
        self.slots = {q: [[es.enter_context(nc.semaphore(f"dq_{q}{i}")), 0] for i in range(nslots)]
                      for q in ("sp", "pool", "act")}
        self.slot_i = dict.fromkeys(self.slots, 0)
        self.ninst = 0

    def _wait(self, en, toks):
        e, seen = self.eng[en], self.seen[en]
        for tk in toks:
            if tk is None or seen.get(tk.key, 0) >= tk.val:
                continue
            if tk.key == en == "pe":
                continue
            e.wait_ge(tk.sem, tk.val)
            seen[tk.key] = tk.val

    @staticmethod
    def _deps(r, w):
        toks = []
        for t in r:
            toks.append(t.w)
            if t.multi:
                toks.extend(t.ws.values())
        for t in w:
            if not t.multi:
                toks.append(t.w)
            toks.extend(t.r.values())
        return toks

    @staticmethod
    def _commit(tok, r, w):
        for t in w:
            if t.multi:
                t.ws[tok.key] = tok
                t.r = {}
            else:
                t.w, t.r = tok, {}
        for t in r:
            if t.w is not tok and t not in w:
                t.r[tok.key] = tok

    def op(self, en, fn, r=(), w=()):
        self._wait(en, self._deps(r, w))
        ins = fn(self.eng[en])
        self.cnt[en] += 1
        ins.then_inc(self.sem[en], 1)
        self._commit(Tok(en, self.sem[en], self.cnt[en]), r, w)
        self.ninst += 1

    def mm(self, fns, r=(), w=()):
        self._wait("pe", self._deps(r, w))
        ins = None
        for fn in fns:
            ins = fn(self.eng["pe"])
        self.cnt["pe"] += 1
        ins.then_inc(self.sem["pe"], 1)
        self._commit(Tok("pe", self.sem["pe"], self.cnt["pe"]), r, w)
        self.ninst += len(fns)

    def dma(self, q, fn, r=(), w=()):
        slots = self.slots[q]
        i = self.slot_i[q]
        self.slot_i[q] = (i + 1) % len(slots)
        sl = slots[i]
        key = f"{q}{i}"
        if sl[1] > 0:
            self._wait(q, [Tok(key, sl[0], sl[1])])
        self._wait(q, self._deps(r, w))
        ins = fn(self.eng[q])
        sl[1] += 16
        ins.then_inc(sl[0], 16)
        self._commit(Tok(key, sl[0], sl[1]), r, w)
        self.ninst += 1

    def all_toks(self):
        toks = []
        for q, slots in self.slots.items():
            for i, sl in enumerate(slots):
                if sl[1] > 0:
                    toks.append(Tok(f"{q}{i}", sl[0], sl[1]))
        for en in ("pe", "act", "dve", "pool"):
            if self.cnt[en] > 0:
                toks.append(Tok(en, self.sem[en], self.cnt[en]))
        return toks

    def barrier(self):
        toks = self.all_toks()
        for en in ("pe", "act", "dve", "pool", "sp"):
            self._wait(en, [t for t in toks if t.key != en])

    def finish(self):
        toks = []
        for q, slots in self.slots.items():
            for i, sl in enumerate(slots):
                if sl[1] > 0:
                    toks.append(Tok(f"{q}{i}", sl[0], sl[1]))
        for en in ("pe", "act", "dve", "pool"):
            if self.cnt[en] > 0:
                toks.append(Tok(en, self.sem[en], self.cnt[en]))
        self._wait("sp", toks)


class Ctx:
    pass


def build(dbg=()):
    nc = bass.Bass("TRN2", target_bir_lowering=False)
    es = ExitStack()
    S = Sched(nc, es)
    C = Ctx()
    C.nc, C.es, C.S, C.dbg, C.dbgout = nc, es, S, dbg, {}

    def din(name, shape, dt=F32):
        return nc.dram_tensor(name, list(shape), dt, kind="ExternalInput").ap()

    C.nalloc = 0

    def sb(name, shape, dt=F32, scope=None):
        C.nalloc += 1
        return (scope or C.scope).enter_context(nc.sbuf_tensor(f"s{C.nalloc}_{name}", list(shape), dt))

    C.scope = es

    C.din, C.sb = din, sb
    I = Ctx()
    C.I = I
    I.x = din("x", [NTOK, D])
    I.cT = din("cT", [128, 8, NSEQ])
    I.pos = din("pos", [NSEQ, SEQ], I32)
    I.w_mod = din("w_mod", [D, 6 * D])
    I.b_modT = din("b_modT", [128, 48])
    I.b_mod_row = din("b_mod_row", [1, 6 * D])
    I.nmwT = din("nmwT", [128, 8])
    I.nfw_row = din("nfw_row", [1, D])
    I.nfin_row = din("nfin_row", [1, D])
    I.w_in = din("w_in", [D, IN_DIM])
    I.ident = din("ident", [128, 128])
    I.w_ba = din("w_ba", [512, D])
    I.w_bs = din("w_bs", [D, D])
    I.w_out = din("w_out", [D, D])
    I.conv_wT = din("conv_wT", [128, 16, 4])
    I.conv_bT = din("conv_bT", [128, 16])
    I.snwT = din("snwT", [128, 8])
    I.hrow = din("hrow", [1, 48])
    C.h_scr = nc.dram_tensor("h_scr", [NTOK, D], F32, kind="Internal").ap()
    C.h_scr_t = T("h_scr", multi=True)
    I.w_router = din("w_router", [D, NE])
    I.rbias_row = din("rbias_row", [1, NE])
    I.iota_row = din("iota_row", [1, NE])
    I.aid = din("aid", [128, NTOK // 128, 8], I32)
    I.bigtab = din("bigtab", [128, 4 * NE], I32)
    I.w_gate_s = din("w_gate_s", [D, 256])
    I.w_up_s = din("w_up_s", [D, 256])
    I.w_down_s = din("w_down_s", [256, D])
    C.u2_rows = nc.dram_tensor("u2_rows", [NTOK, D], BF16, kind="Internal").ap()
    C.u2rows_t = T("u2_rows", multi=True)
    si_h = nc.dram_tensor("slot_info", [128 * 4 * NE, 1], I32, kind="Internal")
    C.slot_info_flat = si_h.ap()
    C.slot_info = si_h.ap().rearrange("(p n) o -> p (n o)", p=128)
    C.slot_t = T("slot_info")
    C.slot_sc = T("slot_scatter", multi=True)
    C.sh_out = nc.dram_tensor("sh_out", [NTOK, D], F32, kind="Internal").ap()
    C.ye2 = nc.dram_tensor("ye2", [NTOK * 8, D], F32, kind="Internal").ap()
    C.ye2_t = T("ye2", multi=True)
    I.w_gate_e = din("w_gate_e", [NE, D, 256])
    I.w_up_e = din("w_up_e", [NE, D, 256])
    I.w_down_e = din("w_down_e", [NE, 256, D])
    C.sh_out_t = T("sh_out", multi=True)
    C.out = nc.dram_tensor("out", [NTOK, D], F32, kind="ExternalOutput").ap()

    K = Ctx()
    C.K = K
    K.t = T("consts")
    K.ident_f = sb("ident_f", [128, 128], F32)
    K.ident_b = sb("ident_b", [128, 128], BF16)
    K.ones_f = sb("ones_f", [128, 128], F32)
    S.dma("sp", lambda e: e.dma_start(out=K.ident_f[:], in_=I.ident), w=[K.t])
    S.op("dve", lambda e: e.tensor_copy(out=K.ident_b[:], in_=K.ident_f[:]), r=[K.t], w=[K.t])
    S.op("dve", lambda e: e.memset(K.ones_f[:], 1.0), w=[K.t])
    K.ones_b = sb("ones_b", [128, 128], BF16)
    S.op("dve", lambda e: e.memset(K.ones_b[:], 1.0), w=[K.t])
    I.cmat = din("cmat", [128, 6, 128])
    I.ropec = din("ropec", [128, 2])
    cm_f = sb("cmat_f", [128, 6, 128], F32)
    K.cm_b = sb("cmat_b", [128, 6, 128], BF16)
    K.ropec = sb("ropec", [128, 2])
    S.dma("sp", lambda e: e.dma_start(out=cm_f[:], in_=I.cmat), w=[K.t])
    S.dma("sp", lambda e: e.dma_start(out=K.ropec[:], in_=I.ropec), w=[K.t])
    S.op("dve", lambda e: e.tensor_copy(out=K.cm_b[:], in_=cm_f[:]), r=[K.t], w=[K.t])
    K.cm_f = cm_f
    K.p32, K.mcur, K.mprev = K.cm_b[:, 0, :], K.cm_b[:, 1, :], K.cm_b[:, 2, :]

    C.ps = [es.enter_context(nc.psum_tensor(f"ps{i}", [128, 512], F32)) for i in range(8)]
    C.pst = [T(f"ps{i}") for i in range(8)]

    M = Ctx()
    C.M = M
    M.t = T("mod")
    M.modT = sb("modT", [128, 48, NSEQ])
    M.scale1T = sb("scale1T", [128, 8, NSEQ])
    with ExitStack() as loc:
        C.scope = loc
        phase_mod(C, None)
        S.barrier()
    for s in range(NSEQ):
        with ExitStack() as sq:
            C.scope = sq
            C.uT = sb("uT", [128, 8, SEQ], BF16)
            C.uT_t = [T(f"uT{g}") for g in range(4)]
            C.attnT = sb("attnT", [128, 4, SEQ], BF16)
            C.attnT_t = T("attnT")
            M.bc = {nm: sb(f"bc_{nm}", [128, D]) for nm in ("g1",)}
            with ExitStack() as loc:
                C.scope = loc
                phase_mod(C, s, which=("g1",))
                S.barrier()
            with ExitStack() as loc:
                C.scope = loc
                phase_norm1(C, s)
                S.barrier()
            if "uT" in dbg:
                dump(C, f"uT{s}", C.uT[:].rearrange("p k t -> p (k t)"), [128, 8 * SEQ], BF16, C.uT_t)
            with ExitStack() as loc:
                C.scope = loc
                phase_rope_tables(C, s)
                if s == 0:
                    zt = sb("zero_tile", [128, 512])
                    zt_t = T("zero_tile")
                    S.op("dve", lambda e: e.memset(zt[:], 0.0), w=[zt_t])
                    for zi in range(NTOK * 8 // 128):
                        for zh in range(2):
                            S.dma("sp", lambda e, zi=zi, zh=zh: e.dma_start(
                                out=C.ye2[zi * 128:(zi + 1) * 128, zh * 512:(zh + 1) * 512], in_=zt[:]), r=[zt_t], w=[C.ye2_t])
                phase_attn(C, s)
                if "attn" in dbg:
                    dump(C, f"attnT{s}", C.attnT[:].rearrange("p h t -> p (h t)"), [128, 4 * SEQ], BF16, [C.attnT_t])
                    dump(C, f"qk{s}", C.A.qk[1][1][:], [128, SEQ], BF16, [C.A.qk_t[1][1]])
                S.barrier()
            with ExitStack() as loc:
                C.scope = loc
                phase_ssd(C, s)
                S.barrier()
            S.barrier()
    C.scope = es
    R = Ctx()
    C.R = R
    R.reg_slot = nc.gpsimd.to_reg(128 * 4 * NE - 1)
    R.reg_tok = nc.gpsimd.to_reg(NTOK - 1)
    R.reg_aid = nc.gpsimd.to_reg(NTOK * 8 - 1)
    R.wsel = sb("wsel", [128, NTOK // 128, 8])
    R.wsel_t = T("wsel", multi=True)
    with ExitStack() as loc:
        C.scope = loc
        phase_route(C)
        S.barrier()
    C.scope = es
    with ExitStack() as loc:
        C.scope = loc
        phase_experts(C)
        S.barrier()
    with ExitStack() as loc:
        C.scope = loc
        phase_final(C)
        S.barrier()
    C.scope = es
    if "route" in dbg:
        dump(C, "wsel", R.wsel[:].rearrange("p g k -> p (g k)"), [128, NTOK // 128 * 8], F32, [R.wsel_t])
        dump(C, "slot_info", C.slot_info, [128, 4 * NE], I32, [C.slot_t, C.slot_sc])
        dump(C, "sh_out", C.sh_out, [NTOK, D], F32, [C.sh_out_t])
        dump(C, "u2_rows", C.u2_rows, [NTOK, D], BF16, [C.u2rows_t])
    if "h" in dbg:
        d_ = nc.dram_tensor("dbg_h", [NTOK, D], F32, kind="ExternalOutput").ap()
        S.dma("sp", lambda e: e.dma_start(out=d_, in_=C.h_scr), r=[C.h_scr_t])
        C.dbgout["h"] = "dbg_h"
    S.finish()
    return nc, C


def dump(C, name, ap, shape, dt, tiles):
    d = C.nc.dram_tensor("dbg_" + name, list(shape), dt, kind="ExternalOutput").ap()
    C.S.dma("sp", lambda e: e.dma_start(out=d, in_=ap), r=tiles)
    C.dbgout[name] = "dbg_" + name


def phase_mod(C, seq, which=("g1", "sh2", "sc2")):
    nc, S, sb, I, K, M = C.nc, C.S, C.sb, C.I, C.K, C.M
    condT = sb("condT", [128, 8, NSEQ])
    cond_bc = sb("cond_bc", [128, 8, NSEQ, 128])
    bmod_row = sb("bmod_row", [1, 6 * D])
    tl = T("modload")
    S.dma("sp", lambda e: e.dma_start(out=condT[:], in_=I.cT), w=[tl])
    S.dma("sp", lambda e: e.dma_start(out=bmod_row[:], in_=I.b_mod_row), w=[tl])
    S.op("act", lambda e: e.activation(out=condT[:], in_=condT[:], func=AF.Silu), r=[tl], w=[tl])
    S.op("dve", lambda e: e.tensor_copy(out=cond_bc[:], in_=condT[:].unsqueeze(3).to_broadcast([128, 8, NSEQ, 128])),
         r=[tl], w=[tl])
    wblk = [sb(f"wmod_blk{i}", [128, 8, 512]) for i in range(2)]
    wblk_t = [T(f"wmod_blk{i}") for i in range(2)]
    psm, psm_t = C.ps[0], C.pst[0]
    if seq is None:
        b_modT = sb("b_modT", [128, 48])
        nmwT = sb("nmwT", [128, 8])
        S.dma("sp", lambda e: e.dma_start(out=b_modT[:], in_=I.b_modT), w=[tl])
        S.dma("sp", lambda e: e.dma_start(out=nmwT[:], in_=I.nmwT), w=[tl])
        blocks = list(range(12))
        names = {}
        bs = []
    elif seq == "g2":
        blocks = [10, 11]
        names = {10: "g2", 11: "g2"}
        bs = list(range(NSEQ))
    else:
        nfw_bc = sb("nfw_bc", [128, D])
        S.dma("sp", lambda e: e.dma_start(out=nfw_bc[:], in_=I.nfw_row.partition_broadcast(128)), w=[M.t])
        names = {jb: nm for jb, nm in {4: "g1", 5: "g1", 6: "sh2", 7: "sh2", 8: "sc2", 9: "sc2"}.items() if nm in which}
        blocks = sorted(names)
        bs = [seq]
    for ib, jb in enumerate(blocks):
        wb, wt = wblk[ib % 2], wblk_t[ib % 2]
        S.dma("sp", lambda e: e.dma_start(out=wb[:], in_=I.w_mod[:, jb * 512:(jb + 1) * 512]
                                          .rearrange("(k p) n -> p k n", p=128)), w=[wt])
        if seq is None:
            for j in range(4):
                jc = jb * 4 + j
                S.mm([lambda e, k=k: e.matmul(psm[:, jc * 2:jc * 2 + 2], lhsT=wb[:, k, j * 128:(j + 1) * 128],
                                               rhs=condT[:, k, :], start=(k == 0), stop=(k == 7)) for k in range(8)],
                     r=[wt, tl], w=[psm_t])
        if jb in names:
            half = jb % 2
            for b in bs:
                pb, pbt = C.ps[1 + b], C.pst[1 + b]
                fns = [lambda e, k=k: e.matmul(pb[:, :], lhsT=cond_bc[:, k, b, :], rhs=wb[:, k, :],
                                               start=(k == 0), stop=False) for k in range(8)]
                fns.append(lambda e: e.matmul(pb[:, :], lhsT=K.ones_f[0:1, :], rhs=bmod_row[0:1, jb * 512:(jb + 1) * 512],
                                              start=False, stop=True))
                S.mm(fns, r=[wt, tl, K.t], w=[pbt])
                dst = M.g2_bc[b] if names[jb] == "g2" else M.bc[names[jb]]
                S.op("act", lambda e: e.copy(out=dst[:, half * 512:(half + 1) * 512], in_=pb[:, :]), r=[pbt], w=[M.t])
    if seq is None:
        S.op("dve", lambda e: e.tensor_tensor(out=M.modT[:], in0=psm[:, 0:96].rearrange("p (j b) -> p j b", b=NSEQ),
                                              in1=b_modT[:].unsqueeze(2).to_broadcast([128, 48, NSEQ]), op=ALU.add),
             r=[psm_t, tl], w=[M.t])
        S.op("dve", lambda e: e.scalar_tensor_tensor(out=M.scale1T[:], in0=M.modT[:, 8:16, :], scalar=1.0,
                                                     in1=nmwT[:].unsqueeze(2).to_broadcast([128, 8, NSEQ]),
                                                     op0=ALU.add, op1=ALU.mult), r=[M.t, tl], w=[M.t])
    elif seq != "g2" and "sc2" in which:
        t = M.bc["sc2"]
        S.op("dve", lambda e: e.scalar_tensor_tensor(out=t[:], in0=t[:], scalar=1.0, in1=nfw_bc[:],
                                                     op0=ALU.add, op1=ALU.mult), r=[M.t], w=[M.t])


def phase_norm1(C, s):
    nc, S, sb, I, K, M = C.nc, C.S, C.sb, C.I, C.K, C.M
    if True:
        C.xin = [sb(f"xin{i}", [128, D]) for i in range(2)]
        C.xin_t = [T(f"xin{i}") for i in range(2)]
        C.xn = [sb(f"xn{i}", [128, D], BF16) for i in range(2)]
        C.xn_t = [T(f"xn{i}") for i in range(2)]
        C.sq = sb("sq_junk", [128, D])
        C.sq_t = T("sq")
        C.ss = [sb(f"ss{i}", [128, 2]) for i in range(2)]
        C.n1tmp = [sb(f"n1tmp{i}", [128, 8, 128]) for i in range(2)]
        C.n1tmp_t = [T(f"n1tmp{i}") for i in range(2)]
    for i in range(SEQ // 128):
        xt, xtt = C.xin[i % 2], C.xin_t[i % 2]
        xn, xnt = C.xn[i % 2], C.xn_t[i % 2]
        ss = C.ss[i % 2]
        r0 = s * SEQ + i * 128
        S.dma("sp", lambda e: e.dma_start(out=xt[:], in_=I.x[r0:r0 + 128, :]), w=[xtt])
        S.op("act", lambda e: e.activation(out=C.sq[:], in_=xt[:], func=AF.Square, accum_out=ss[:, 0:1]),
             r=[xtt], w=[C.sq_t, xnt])
        S.op("dve", lambda e: e.tensor_scalar(out=ss[:, 1:2], in0=ss[:, 0:1], scalar1=1.0 / D, scalar2=EPS,
                                              op0=ALU.mult, op1=ALU.add), r=[xnt], w=[xnt])
        S.op("act", lambda e: e.sqrt(out=ss[:, 1:2], in_=ss[:, 1:2]), r=[xnt], w=[xnt])
        S.op("dve", lambda e: e.reciprocal(out=ss[:, 1:2], in_=ss[:, 1:2]), r=[xnt], w=[xnt])
        S.op("dve", lambda e: e.tensor_scalar(out=xn[:], in0=xt[:], scalar1=ss[:, 1:2], scalar2=None,
                                              op0=ALU.mult), r=[xtt, xnt], w=[xnt])
        pb, pbt = C.ps[i % 2], C.pst[i % 2]
        pbb = pb[:, :].bitcast(BF16)
        S.mm([lambda e, k=k: e.transpose(out=pbb[:, k * 128:(k + 1) * 128], in_=xn[:, k * 128:(k + 1) * 128],
                                          identity=K.ident_b[:]) for k in range(8)], r=[xnt, K.t], w=[pbt])
        ut = C.uT_t[i // 4]
        tmp32, tmp32_t = C.n1tmp[i % 2], C.n1tmp_t[i % 2]
        S.op("dve", lambda e: e.tensor_tensor(out=tmp32[:], in0=pbb.rearrange("p (k t) -> p k t", k=8),
                                              in1=M.scale1T[:, :, s].unsqueeze(2).to_broadcast([128, 8, 128]), op=ALU.mult),
             r=[pbt, M.t], w=[tmp32_t])
        S.op("dve", lambda e: e.tensor_tensor(out=C.uT[:, :, i * 128:(i + 1) * 128], in0=tmp32[:],
                                              in1=M.modT[:, 0:8, s].unsqueeze(2).to_broadcast([128, 8, 128]), op=ALU.add),
             r=[tmp32_t, M.t], w=[ut])


def qsel(d, r, n):
    st = d * 128 * n + r
    return slice(st, st + d * 127 + 1, d)


def phase_rope_tables(C, s):
    nc, S, sb, I, K = C.nc, C.S, C.sb, C.I, C.K
    C.cosT = sb("cosT", [32, SEQ])
    C.sinT = sb("sinT", [32, SEQ])
    C.rope_t = T("rope")
    outer_scope = C.scope
    rloc = ExitStack()
    C.scope = rloc
    C.posi = sb("posi", [32, SEQ], I32)
    C.ang = sb("ang", [32, SEQ])
    S.dma("sp", lambda e: e.dma_start(out=C.posi[:], in_=I.pos[s:s + 1, :].partition_broadcast(32)), w=[C.rope_t])
    S.op("dve", lambda e: e.tensor_copy(out=C.ang[:], in_=C.posi[:]), r=[C.rope_t], w=[C.rope_t])
    S.op("dve", lambda e: e.tensor_scalar(out=C.ang[:], in0=C.ang[:], scalar1=K.ropec[0:32, 0:1], scalar2=None,
                                          op0=ALU.mult), r=[C.rope_t, K.t], w=[C.rope_t])
    PI = float(np.pi)
    TWO_PI = 2.0 * PI
    PI_LO = 3.1415925
    yy = C.sb("rope_y", [32, SEQ])
    for dst, sh in ((C.sinT, PI), (C.cosT, PI + PI / 2)):
        rt = [C.rope_t]
        S.op("dve", lambda e: e.tensor_scalar(out=yy[:], in0=C.ang[:], scalar1=sh, scalar2=None, op0=ALU.add), r=rt, w=rt)
        S.op("dve", lambda e: e.tensor_scalar(out=C.posi[:], in0=yy[:], scalar1=1.0 / TWO_PI, scalar2=None,
                                              op0=ALU.mult), r=rt, w=rt)
        S.op("dve", lambda e: e.tensor_copy(out=dst[:], in_=C.posi[:]), r=rt, w=rt)
        S.op("dve", lambda e: e.scalar_tensor_tensor(out=yy[:], in0=dst[:], scalar=-TWO_PI, in1=yy[:],
                                                     op0=ALU.mult, op1=ALU.add), r=rt, w=rt)
        S.op("dve", lambda e: e.tensor_scalar(out=dst[:], in0=yy[:], scalar1=0.0, scalar2=None, op0=ALU.is_lt), r=rt, w=rt)
        S.op("dve", lambda e: e.scalar_tensor_tensor(out=yy[:], in0=dst[:], scalar=TWO_PI, in1=yy[:],
                                                     op0=ALU.mult, op1=ALU.add), r=rt, w=rt)
        S.op("dve", lambda e: e.tensor_scalar(out=yy[:], in0=yy[:], scalar1=-PI, scalar2=None, op0=ALU.add), r=rt, w=rt)
        S.op("dve", lambda e: e.tensor_scalar(out=yy[:], in0=yy[:], scalar1=PI_LO, scalar2=-PI_LO,
                                              op0=ALU.min, op1=ALU.max), r=rt, w=rt)
        S.op("act", lambda e: e.activation(out=dst[:], in_=yy[:], func=AF.Sin), r=rt, w=rt)
    S.op("dve", lambda e: e.tensor_scalar(out=C.sinT[:], in0=C.sinT[:], scalar1=K.ropec[0:32, 1:2], scalar2=None,
                                          op0=ALU.mult), r=[C.rope_t, K.t], w=[C.rope_t])
    S.barrier()
    rloc.close()
    C.scope = outer_scope


def phase_attn(C, s):
    nc, S, sb, I, K = C.nc, C.S, C.sb, C.I, C.K
    A = Ctx()
    C.A = A
    A.w = [[sb(f"aw{b}_{i}", [128, 8, 128], BF16) for i in range(9)] for b in range(2)]
    A.w_t = [[T(f"aw{b}_{i}") for i in range(9)] for b in range(2)]
    A.qk = [[sb(f"qk{b}_{i}", [128, SEQ], BF16) for i in range(6)] for b in range(2)]
    A.qk_t = [[T(f"qk{b}_{i}") for i in range(6)] for b in range(2)]
    A.v = [[sb(f"v{b}_{i}", [128, 16, 128], BF16) for i in range(3)] for b in range(2)]
    A.v_t = [[T(f"v{b}_{i}") for i in range(3)] for b in range(2)]
    A.acc = sb("attacc", [128, 2, SEQ])
    A.acc_t = T("attacc")
    A.pT = [sb(f"pT{i}", [128, 512], BF16) for i in range(2)]
    A.pT_t = [T(f"pT{i}") for i in range(2)]
    A.rt = [sb(f"ropetmp{i}", [32, 512]) for i in range(2)]
    A.rt_t = T("ropetmp")
    A.ni = 0
    A.nb = 0
    scale = 1.0 / float(np.sqrt(128.0))

    def gen_inproj(hs, bs):
        W, W_t, QK, QK_t, V, V_t = A.w[bs], A.w_t[bs], A.qk[bs], A.qk_t[bs], A.v[bs], A.v_t[bs]
        for i in range(9):
            base = (Q0, K0, V0)[i // 3] + ((i % 3) * 4 + hs) * 128
            S.dma("pool", lambda e, i=i, base=base: e.dma_start(
                out=W[i][:], in_=I.w_in[:, base:base + 128].rearrange("(k p) n -> p k n", p=128)), w=[W_t[i]])
        for i in range(6):
            for tg in range(4):
                A.ni += 1
                pq, pqt = C.ps[A.ni % 2], C.pst[A.ni % 2]
                psw, pswt = C.ps[2 + A.ni % 2], C.pst[2 + A.ni % 2]
                tsl = slice(tg * 512, (tg + 1) * 512)
                S.mm([lambda e, k=k: e.matmul(pq[:, :], lhsT=W[i][:, k, :], rhs=C.uT[:, k, tsl],
                                               start=(k == 0), stop=(k == 7)) for k in range(8)],
                     r=[W_t[i], C.uT_t[tg]], w=[pqt])
                S.op("act", lambda e: e.copy(out=QK[i][:, tsl], in_=pq[:, :]), r=[pqt], w=[QK_t[i]])
                S.mm([lambda e: e.matmul(psw[0:32, :], lhsT=K.p32[0:32, 0:32], rhs=QK[i][0:32, tsl],
                                         start=True, stop=True)], r=[QK_t[i], K.t], w=[pswt])
                t0, t1 = A.rt
                S.op("dve", lambda e: e.tensor_tensor(out=t0[:], in0=psw[0:32, :], in1=C.sinT[:, tsl], op=ALU.mult),
                     r=[pswt, C.rope_t], w=[A.rt_t])
                S.op("dve", lambda e: e.tensor_tensor(out=t1[:], in0=pq[0:32, :], in1=C.cosT[:, tsl], op=ALU.mult),
                     r=[pqt, C.rope_t], w=[A.rt_t])
                S.op("dve", lambda e: e.tensor_tensor(out=QK[i][0:32, tsl], in0=t0[:], in1=t1[:], op=ALU.add),
                     r=[A.rt_t], w=[QK_t[i], A.rt_t])
                yield
        for g, d in enumerate((1, 4, 16)):
            nb = 16 // d
            for j0 in range(0, 16, 4):
                A.ni += 1
                pv, pvt = C.ps[A.ni % 2], C.pst[A.ni % 2]
                for jj in range(4):
                    j = j0 + jj
                    r_, n_ = j // nb, j % nb
                    sel = qsel(d, r_, n_)
                    S.mm([lambda e, k=k: e.matmul(pv[:, jj * 128:(jj + 1) * 128], lhsT=C.uT[:, k, sel],
                                                   rhs=W[6 + g][:, k, :], start=(k == 0), stop=(k == 7))
                          for k in range(8)], r=[W_t[6 + g]] + C.uT_t, w=[pvt])
                S.op("act", lambda e: e.copy(out=V[g][:, j0:j0 + 4, :].rearrange("p j e -> p (j e)"), in_=pv[:, :]),
                     r=[pvt], w=[V_t[g]])
                yield

    def gen_blocks(hs, bs):
        QK, QK_t, V, V_t = A.qk[bs], A.qk_t[bs], A.v[bs], A.v_t[bs]
        blocks = []
        for g, d in enumerate((1, 4, 16)):
            nb = 16 // d
            for r_ in range(d):
                for n_ in range(nb):
                    blocks.append((g, d, nb, r_, n_))
        for b0 in range(0, len(blocks), 2):
            pair = blocks[b0:b0 + 2]
            g = pair[0][0]
            assert all(p[0] == g for p in pair)
            qT, kT, qt_, kt_ = QK[g], QK[3 + g], QK_t[g], QK_t[3 + g]
            A.nb += 1
            ps_s, ps_st = C.ps[4 + A.nb % 2], C.pst[4 + A.nb % 2]
            ps_o, ps_ot = C.ps[6 + A.nb % 2], C.pst[6 + A.nb % 2]
            pT, pTt = A.pT[A.nb % 2], A.pT_t[A.nb % 2]
            fns = []
            metas = []
            for pi, (g_, d, nb, r_, n_) in enumerate(pair):
                qs = qsel(d, r_, n_)
                kbs = [kb for kb in (n_ - 1, n_) if kb >= 0]
                c0 = pi * 256 + 256 - 128 * len(kbs)
                for ci, kb in enumerate(kbs):
                    cs = slice(c0 + ci * 128, c0 + (ci + 1) * 128)
                    ks = qsel(d, r_, kb)
                    msk = K.mcur if kb == n_ else K.mprev
                    fns.append(lambda e, cs=cs, ks=ks, qs=qs: e.matmul(ps_s[:, cs], lhsT=kT[:, ks], rhs=qT[:, qs],
                                                                       start=True, stop=False))
                    fns.append(lambda e, cs=cs, msk=msk: e.matmul(ps_s[:, cs], lhsT=K.ident_b[:], rhs=msk[:],
                                                                  start=False, stop=True))
                metas.append((d, nb, r_, n_, qs, kbs, c0))
            S.mm(fns, r=[qt_, kt_, K.t], w=[ps_st])
            lo0, hi0 = metas[0][6], 256
            lo1, hi1 = metas[1][6], 512
            rngs = [(lo0, hi1)] if lo1 == 256 else [(lo0, hi0), (lo1, hi1)]
            for lo, hi in rngs:
                S.op("act", lambda e, lo=lo, hi=hi: e.activation(out=pT[:, lo:hi], in_=ps_s[:, lo:hi], func=AF.Exp, scale=scale),
                     r=[ps_st], w=[pTt])
            fns = []
            for pi, (d, nb, r_, n_, qs, kbs, c0) in enumerate(metas):
                ob = pi * 256
                for ci, kb in enumerate(kbs):
                    cs = slice(c0 + ci * 128, c0 + (ci + 1) * 128)
                    vj = r_ * nb + kb
                    fns.append(lambda e, cs=cs, vj=vj, ci=ci, ob=ob, nk=len(kbs): e.matmul(
                        ps_o[:, ob:ob + 128], lhsT=V[g][:, vj, :], rhs=pT[:, cs], start=(ci == 0), stop=(ci == nk - 1)))
                for ci, kb in enumerate(kbs):
                    cs = slice(c0 + ci * 128, c0 + (ci + 1) * 128)
                    fns.append(lambda e, cs=cs, ci=ci, ob=ob, nk=len(kbs): e.matmul(
                        ps_o[:, ob + 128:ob + 256], lhsT=K.ones_b[:], rhs=pT[:, cs], start=(ci == 0), stop=(ci == nk - 1)))
            S.mm(fns, r=[pTt, V_t[g], K.t], w=[ps_ot])
            for pi, (d, nb, r_, n_, qs, kbs, c0) in enumerate(metas):
                src = ps_o[:, pi * 256:(pi + 1) * 256].rearrange("p (a q) -> p a q", a=2)
                if g == 0:
                    S.op("act", lambda e, src=src, qs=qs: e.copy(out=A.acc[:, :, qs], in_=src), r=[ps_ot], w=[A.acc_t])
                else:
                    S.op("dve", lambda e, src=src, qs=qs: e.tensor_tensor(out=A.acc[:, :, qs], in0=src, in1=A.acc[:, :, qs], op=ALU.add),
                         r=[ps_ot, A.acc_t], w=[A.acc_t])
            yield
        S.op("dve", lambda e: e.reciprocal(out=A.acc[:, 1, :], in_=A.acc[:, 1, :]), r=[A.acc_t], w=[A.acc_t])
        S.op("dve", lambda e: e.tensor_tensor(out=C.attnT[:, hs, :], in0=A.acc[:, 0, :], in1=A.acc[:, 1, :], op=ALU.mult),
             r=[A.acc_t], w=[C.attnT_t])

    for _ in gen_inproj(0, 0):
        pass
    for hs in range(4):
        gb = gen_blocks(hs, hs % 2)
        gi = gen_inproj(hs + 1, (hs + 1) % 2) if hs + 1 < 4 else iter(())
        done_b = done_i = False
        step = 0
        while not (done_b and done_i):
            step += 1
            if not done_b:
                try:
                    next(gb)
                except StopIteration:
                    done_b = True
            for _rep in range(2 if step % 2 == 0 else 1):
                if not done_i:
                    try:
                        next(gi)
                    except StopIteration:
                        done_i = True


def phase_ssd(C, s):
    nc, S, sb, I, K, M = C.nc, C.S, C.sb, C.I, C.K, C.M
    ps, pst = C.ps, C.pst
    tri_f, sl_f = K.cm_f[:, 3, :], K.cm_f[:, 4, :]
    tw = T("ssd_w", multi=True)
    wdt = sb("wdt", [128, 8, 16], BF16)
    S.dma("pool", lambda e: e.dma_start(out=wdt[:], in_=I.w_in[:, DT0:DT0 + 16].rearrange("(k p) n -> p k n", p=128)), w=[tw])
    tc = T("ssd_c")
    convw = sb("convw", [128, 16, 4])
    convb = sb("convb", [128, 16])
    snwT = sb("snwT", [128, 8])
    hb16 = sb("hb16", [128, 3, 16])
    D_bc = sb("D_bc", [128, 16, 64])
    S.dma("sp", lambda e: e.dma_start(out=convw[:], in_=I.conv_wT), w=[tc])
    S.dma("sp", lambda e: e.dma_start(out=convb[:], in_=I.conv_bT), w=[tc])
    S.dma("sp", lambda e: e.dma_start(out=snwT[:], in_=I.snwT), w=[tc])
    S.dma("sp", lambda e: e.dma_start(out=hb16[:].rearrange("p a h -> p (a h)"), in_=I.hrow.partition_broadcast(128)), w=[tc])
    S.op("act", lambda e: e.activation(out=hb16[:, 1, :], in_=hb16[:, 1, :], func=AF.Exp), r=[tc], w=[tc])
    S.op("dve", lambda e: e.tensor_scalar(out=hb16[:, 1, :], in0=hb16[:, 1, :], scalar1=-1.0, scalar2=None, op0=ALU.mult),
         r=[tc], w=[tc])
    S.op("dve", lambda e: e.tensor_copy(out=D_bc[:], in_=hb16[:, 2, :].unsqueeze(2).to_broadcast([128, 16, 64])), r=[tc], w=[tc])
    nws = [0]
    halo = sb("halo", [128, 16, 3], BF16)
    halo_t = T("halo")
    S.op("dve", lambda e: e.memset(halo[:], 0.0), w=[halo_t])
    cdiag = sb("cdiag", [128, 16, 4, 128], BF16)
    cdiag_t = T("cdiag", multi=True)
    for c in range(16):
        for j in range(4):
            S.op("dve", lambda e, c=c, j=j: e.tensor_scalar(out=cdiag[:, c, j, :], in0=K.ident_f[:], scalar1=convw[:, c, j:j + 1],
                                                            scalar2=None, op0=ALU.mult), r=[tc, K.t], w=[cdiag_t])
    Hs = sb("Hs", [128, 16, 64])
    Hb = sb("Hb", [128, 16, 64], BF16)
    H_t = T("H")
    xbc = sb("xbc", [128, 16, 512], BF16)
    xbc_t = T("xbc")
    yT = sb("yT", [128, 8, 512], BF16)
    yT_t = T("yT")
    nb = [0]
    ssd_scope = C.scope
    W = {}

    def load_w(col0):
        i = nws[0] % 2
        nws[0] += 1
        wst, wst_t = W["wst"], W["wst_t"]
        S.dma("pool", lambda e: e.dma_start(out=wst[i][:], in_=I.w_in[:, col0:col0 + 512].rearrange("(k p) n -> p k n", p=128)),
              w=[wst_t[i]])
        return wst[i], wst_t[i]

    for tg in range(4):
        tsl = slice(tg * 512, (tg + 1) * 512)
        ut = C.uT_t[tg]
        loc = ExitStack()
        C.scope = loc
        W["wst"] = [sb(f"wst{i}", [128, 8, 512], BF16) for i in range(2)]
        W["wst_t"] = [T(f"wst{i}") for i in range(2)]
        pre = [sb(f"pre{i}", [128, 516], BF16) for i in range(2)]
        pre_t = [T(f"pre{i}") for i in range(2)]
        for c in range(16):
            if c % 4 == 0:
                wx, wxt = load_w(X0 + c * 128)
            nb[0] += 1
            pq, pqt = ps[nb[0] % 2], pst[nb[0] % 2]
            pc, pct = ps[2 + nb[0] % 2], pst[2 + nb[0] % 2]
            pr, prt = pre[nb[0] % 2], pre_t[nb[0] % 2]
            S.mm([lambda e, k=k: e.matmul(pq[:, :], lhsT=wx[:, k, (c % 4) * 128:(c % 4 + 1) * 128], rhs=C.uT[:, k, tsl],
                                           start=(k == 0), stop=(k == 7)) for k in range(8)], r=[wxt, ut], w=[pqt])
            S.op("act", lambda e: e.copy(out=pr[:, 3:515], in_=pq[:, :]), r=[pqt], w=[prt])
            S.op("act", lambda e: e.copy(out=pr[:, 0:3], in_=halo[:, c, :]), r=[halo_t], w=[prt])
            S.mm([lambda e, j=j: e.matmul(pc[:, :], lhsT=cdiag[:, c, j, :], rhs=pr[:, j:j + 512], start=(j == 0), stop=(j == 3))
                  for j in range(4)], r=[prt, cdiag_t], w=[pct])
            S.op("act", lambda e: e.copy(out=halo[:, c, :], in_=pr[:, 512:515]), r=[prt], w=[halo_t])
            S.op("act", lambda e: e.activation(out=xbc[:, c, :], in_=pc[:, :], func=AF.Silu, bias=convb[:, c:c + 1]),
                 r=[pct, tc], w=[xbc_t])
        S.barrier()
        loc.close()
        loc = ExitStack()
        C.scope = loc
        wz = sb("wz", [128, 8, D], BF16)
        twz = T("wz")
        S.dma("pool", lambda e: e.dma_start(out=wz[:], in_=I.w_in[:, Z0:Z0 + D].rearrange("(k p) n -> p k n", p=128)), w=[twz])
        dts = sb("dts", [128, 4, 4, 16])
        dts_t = T("dts")
        sm2 = [sb(f"ssd_small{i}", [128, 6, 16]) for i in range(2)]
        sm2_t = [T(f"ssd_small{i}") for i in range(2)]
        Lf = sb("Lf", [128, 16, 128])
        Lf_t = T("Lf")
        cbTm = sb("cbTm", [128, 4, 128])
        cbTm_t = T("cbTm")
        dec = [sb(f"dec{i}", [128, 4, 128], BF16) for i in range(2)]
        dec_t = [T(f"dec{i}") for i in range(2)]
        MT2 = [sb(f"MT{i}", [128, 16, 128], BF16) for i in range(2)]
        MT2_t = [T(f"MT{i}") for i in range(2)]
        xdt2 = [sb(f"xdt{i}", [128, 16, 64], BF16) for i in range(2)]
        xsD2 = [sb(f"xsD{i}", [128, 16, 64]) for i in range(2)]
        xdd2 = [sb(f"xdd{i}", [128, 16, 64], BF16) for i in range(2)]
        xd2_t = [T(f"xd{i}") for i in range(2)]
        Bt2 = [sb(f"Bt{i}", [128, 4, 128], BF16) for i in range(2)]
        Bt2_t = [T(f"Bt{i}") for i in range(2)]
        t1 = sb("t1", [128, 16, 64])
        t1_t = T("t1")
        yb = sb("yb", [128, D])
        yb_t = T("yb")
        sz2 = [sb(f"sz{i}", [128, D]) for i in range(2)]
        sz2_t = [T(f"sz{i}") for i in range(2)]
        ysq = sb("ysq", [128, 2, 4])
        yn = sb("yn", [128, D], BF16)
        yn_t = T("yn")
        pd, pdt = ps[2], pst[2]
        for ti in range(4):
            tok = slice(tg * 512 + ti * 128, tg * 512 + (ti + 1) * 128)
            S.mm([lambda e, k=k: e.matmul(pd[:, ti * 16:(ti + 1) * 16], lhsT=C.uT[:, k, tok], rhs=wdt[:, k, :],
                                           start=(k == 0), stop=(k == 7)) for k in range(8)], r=[tw, ut], w=[pdt])
        dt4 = [dts_t]
        S.op("dve", lambda e: e.tensor_tensor(out=dts[:, :, 0, :], in0=pd[:, 0:64].rearrange("p (t h) -> p t h", h=16),
                                              in1=hb16[:, 0, :].unsqueeze(1).to_broadcast([128, 4, 16]), op=ALU.add),
             r=[pdt, tc], w=dt4)
        S.op("act", lambda e: e.activation(out=dts[:, :, 1, :], in_=dts[:, :, 0, :], func=AF.Abs), r=dt4, w=dt4)
        S.op("act", lambda e: e.activation(out=dts[:, :, 1, :], in_=dts[:, :, 1, :], func=AF.Exp, scale=-1.0), r=dt4, w=dt4)
        S.op("dve", lambda e: e.tensor_scalar(out=dts[:, :, 1, :], in0=dts[:, :, 1, :], scalar1=1.0, scalar2=None, op0=ALU.add),
             r=dt4, w=dt4)
        S.op("act", lambda e: e.activation(out=dts[:, :, 1, :], in_=dts[:, :, 1, :], func=AF.Ln), r=dt4, w=dt4)
        S.op("dve", lambda e: e.scalar_tensor_tensor(out=dts[:, :, 2, :], in0=dts[:, :, 0, :], scalar=0.0, in1=dts[:, :, 1, :],
                                                     op0=ALU.max, op1=ALU.add), r=dt4, w=dt4)
        S.op("dve", lambda e: e.tensor_tensor(out=dts[:, :, 3, :], in0=dts[:, :, 2, :],
                                              in1=hb16[:, 1, :].unsqueeze(1).to_broadcast([128, 4, 16]), op=ALU.mult),
             r=dt4 + [tc], w=dt4)
        def chunk_front(ci):
                csl = slice(ci * 128, (ci + 1) * 128)
                tok = slice(tg * 512 + ci * 128, tg * 512 + (ci + 1) * 128)
                first = (tg == 0 and ci == 0)
                a_ = dts[:, ci, 3, :]
                dt_ = dts[:, ci, 2, :]
                par = ci % 2
                sm, sm_t = sm2[par], sm2_t[par]
                MT, MT_t = MT2[par], MT2_t[par]
                xdt, xsD, xdd, xd_t = xdt2[par], xsD2[par], xdd2[par], xd2_t[par]
                Bt, Bt_t = Bt2[par], Bt2_t[par]
                sz, sz_t = sz2[par], sz2_t[par]
                smt = [sm_t]
                xdw = [xd_t]
                p0, p0t = ps[0], pst[0]
                S.mm([lambda e: e.matmul(p0[:, 0:16], lhsT=tri_f, rhs=a_, start=True, stop=True),
                      lambda e: e.matmul(p0[:, 16:32], lhsT=K.ones_f[:], rhs=a_, start=True, stop=True)],
                     r=[K.t, dts_t], w=[p0t])
                smt = [sm_t]
                S.op("act", lambda e: e.copy(out=sm[:, 0, :], in_=p0[:, 0:16]), r=[p0t], w=smt)
                S.op("dve", lambda e: e.tensor_tensor(out=sm[:, 1, :], in0=p0[:, 16:32], in1=sm[:, 0, :], op=ALU.subtract),
                     r=[p0t] + smt, w=smt)
                S.op("act", lambda e: e.activation(out=sm[:, 2, :], in_=sm[:, 0, :], func=AF.Exp), r=smt, w=smt)
                S.op("act", lambda e: e.activation(out=sm[:, 3, :], in_=sm[:, 1, :], func=AF.Exp), r=smt, w=smt)
                S.op("act", lambda e: e.activation(out=sm[:, 4, :], in_=p0[:, 16:32], func=AF.Exp), r=[p0t] + smt, w=smt)
                S.op("dve", lambda e: e.tensor_tensor(out=Lf[:], in0=sl_f.unsqueeze(1).to_broadcast([128, 16, 128]),
                                                      in1=a_.unsqueeze(2).to_broadcast([128, 16, 128]), op=ALU.mult),
                     r=[K.t, dts_t], w=[Lf_t])
                p1, p1t = ps[1], pst[1]
                S.mm([lambda e, g=g: e.matmul(p1[:, g * 128:(g + 1) * 128], lhsT=xbc[:, 8 + g, csl], rhs=xbc[:, 12 + g, csl],
                                               start=True, stop=True) for g in range(4)], r=[xbc_t], w=[p1t])
                S.op("dve", lambda e: e.tensor_tensor(out=cbTm[:], in0=p1[:, :].rearrange("p (g l) -> p g l", g=4),
                                                      in1=tri_f.unsqueeze(1).to_broadcast([128, 4, 128]), op=ALU.mult),
                     r=[p1t, K.t], w=[cbTm_t])
                p2, p2t = ps[2], pst[2]
                p3, p3t = ps[3], pst[3]
                p2b = p2[:, :].bitcast(BF16)
                p3b = p3[:, :].bitcast(BF16)
                S.mm([lambda e, k=k: e.transpose(out=p2b[:, k * 128:(k + 1) * 128], in_=xbc[:, k, csl], identity=K.ident_b[:])
                      for k in range(8)], r=[xbc_t, K.t], w=[p2t])
                S.mm([lambda e, g=g: e.transpose(out=p3b[:, g * 128:(g + 1) * 128], in_=xbc[:, 8 + g, csl], identity=K.ident_b[:])
                      for g in range(4)], r=[xbc_t, K.t], w=[p3t])
                xsT = p2b.rearrange("p (h e) -> p h e", h=16)
                xdw = [xd_t]
                S.op("dve", lambda e: e.tensor_tensor(out=xdt[:], in0=xsT, in1=dt_.unsqueeze(2).to_broadcast([128, 16, 64]),
                                                      op=ALU.mult), r=[p2t, dts_t], w=xdw)
                S.op("dve", lambda e: e.tensor_tensor(out=xsD[:], in0=xsT, in1=D_bc[:], op=ALU.mult), r=[p2t, tc], w=xdw)
                S.op("dve", lambda e: e.tensor_tensor(out=xdd[:], in0=xdt[:], in1=sm[:, 3, :].unsqueeze(2).to_broadcast([128, 16, 64]),
                                                      op=ALU.mult), r=xdw + smt, w=xdw)
                S.op("act", lambda e: e.copy(out=Bt[:].rearrange("p g n -> p (g n)"), in_=p3b[:, 0:512]), r=[p3t], w=[Bt_t])
                for g in range(4):
                    pdx, pdxt = ps[4 + g % 2], pst[4 + g % 2]
                    S.mm([lambda e, hl=hl: e.matmul(pdx[:, hl * 128:(hl + 1) * 128], lhsT=Lf[:, g * 4 + hl, :], rhs=tri_f,
                                                     start=True, stop=True) for hl in range(4)], r=[Lf_t, K.t], w=[pdxt])
                    dc, dct = dec[g % 2], dec_t[g % 2]
                    S.op("act", lambda e: e.activation(out=dc[:].rearrange("p h l -> p (h l)"), in_=pdx[:, :], func=AF.Exp),
                         r=[pdxt], w=[dct])
                    S.op("dve", lambda e: e.tensor_tensor(out=MT[:, g * 4:(g + 1) * 4, :], in0=dc[:],
                                                          in1=cbTm[:, g, :].unsqueeze(1).to_broadcast([128, 4, 128]), op=ALU.mult),
                         r=[dct, cbTm_t], w=[MT_t])

                pz = (ps[6], ps[7])
                pzt = [pst[6], pst[7]]
                for hf in range(2):
                    S.mm([lambda e, k=k, hf=hf: e.matmul(pz[hf][:, :], lhsT=C.uT[:, k, tok], rhs=wz[:, k, hf * 512:(hf + 1) * 512],
                                                          start=(k == 0), stop=(k == 7)) for k in range(8)], r=[twz, ut], w=[pzt[hf]])
                    S.op("act", lambda e, hf=hf: e.activation(out=sz[:, hf * 512:(hf + 1) * 512], in_=pz[hf][:, :], func=AF.Silu),
                         r=[pzt[hf]], w=[sz_t])

        def chunk_back(ci):
                csl = slice(ci * 128, (ci + 1) * 128)
                tok = slice(tg * 512 + ci * 128, tg * 512 + (ci + 1) * 128)
                first = (tg == 0 and ci == 0)
                a_ = dts[:, ci, 3, :]
                dt_ = dts[:, ci, 2, :]
                par = ci % 2
                sm, sm_t = sm2[par], sm2_t[par]
                MT, MT_t = MT2[par], MT2_t[par]
                xdt, xsD, xdd, xd_t = xdt2[par], xsD2[par], xdd2[par], xd2_t[par]
                Bt, Bt_t = Bt2[par], Bt2_t[par]
                sz, sz_t = sz2[par], sz2_t[par]
                smt = [sm_t]
                xdw = [xd_t]
                py = (ps[6], ps[7])
                pyt = [pst[6], pst[7]]
                S.mm([lambda e, h=h: e.matmul(py[h // 8][:, (h % 8) * 64:(h % 8 + 1) * 64], lhsT=MT[:, h, :], rhs=xdt[:, h, :],
                                               start=True, stop=True) for h in range(16)], r=[MT_t, xd_t], w=pyt)
                po = (ps[0], ps[1])
                pot = [pst[0], pst[1]]
                if not first:
                    S.mm([lambda e, g=g: e.matmul(po[g // 2][:, (g % 2) * 256:(g % 2 + 1) * 256], lhsT=xbc[:, 12 + g, csl],
                                                   rhs=Hb[:, g * 4:(g + 1) * 4, :].rearrange("p h e -> p (h e)"),
                                                   start=True, stop=True) for g in range(4)], r=[xbc_t, H_t], w=pot)
                    for hf in range(2):
                        S.op("dve", lambda e, hf=hf: e.tensor_tensor(
                            out=t1[:, hf * 8:(hf + 1) * 8, :], in0=po[hf][:, :].rearrange("p (h e) -> p h e", h=8),
                            in1=sm[:, 2, hf * 8:(hf + 1) * 8].unsqueeze(2).to_broadcast([128, 8, 64]), op=ALU.mult),
                            r=[pot[hf]] + smt, w=[t1_t])
                    S.op("dve", lambda e: e.tensor_tensor(out=t1[:], in0=t1[:], in1=xsD[:], op=ALU.add), r=[t1_t, xd_t], w=[t1_t])
                    tsrc = t1
                else:
                    tsrc = xsD
                for hf in range(2):
                    S.op("dve", lambda e, hf=hf: e.tensor_tensor(
                        out=yb[:, hf * 512:(hf + 1) * 512], in0=py[hf][:, :],
                        in1=tsrc[:, hf * 8:(hf + 1) * 8, :].rearrange("p h e -> p (h e)"), op=ALU.add),
                        r=[pyt[hf], t1_t, xd_t], w=[yb_t])
                pS = (ps[2], ps[3])
                pSt = [pst[2], pst[3]]
                S.mm([lambda e, g=g: e.matmul(pS[g // 2][:, (g % 2) * 256:(g % 2 + 1) * 256], lhsT=Bt[:, g, :],
                                               rhs=xdd[:, g * 4:(g + 1) * 4, :].rearrange("p h e -> p (h e)"),
                                               start=True, stop=True) for g in range(4)], r=[Bt_t, xd_t], w=pSt)
                if not first:
                    S.op("dve", lambda e: e.tensor_tensor(out=Hs[:], in0=Hs[:], in1=sm[:, 4, :].unsqueeze(2).to_broadcast([128, 16, 64]),
                                                          op=ALU.mult), r=smt + [H_t], w=[H_t])
                    for hf in range(2):
                        S.op("dve", lambda e, hf=hf: e.tensor_tensor(
                            out=Hs[:, hf * 8:(hf + 1) * 8, :], in0=pS[hf][:, :].rearrange("p (h e) -> p h e", h=8),
                            in1=Hs[:, hf * 8:(hf + 1) * 8, :], op=ALU.add), r=[pSt[hf], H_t], w=[H_t])
                else:
                    for hf in range(2):
                        S.op("act", lambda e, hf=hf: e.copy(out=Hs[:, hf * 8:(hf + 1) * 8, :].rearrange("p h e -> p (h e)"),
                                                            in_=pS[hf][:, :]), r=[pSt[hf]], w=[H_t])
                S.op("act", lambda e: e.copy(out=Hb[:], in_=Hs[:]), r=[H_t], w=[H_t])
                S.op("dve", lambda e: e.tensor_tensor(out=yb[:], in0=yb[:], in1=sz[:], op=ALU.mult), r=[yb_t, sz_t], w=[yb_t])
                for g in range(4):
                    S.op("act", lambda e, g=g: e.activation(out=sz[:, g * 256:(g + 1) * 256], in_=yb[:, g * 256:(g + 1) * 256],
                                                            func=AF.Square, accum_out=ysq[:, 0, g:g + 1]), r=[yb_t], w=[sz_t])
                S.op("dve", lambda e: e.tensor_scalar(out=ysq[:, 1, :], in0=ysq[:, 0, :], scalar1=1.0 / 256, scalar2=EPS,
                                                      op0=ALU.mult, op1=ALU.add), r=[sz_t], w=[sz_t])
                S.op("act", lambda e: e.sqrt(out=ysq[:, 1, :], in_=ysq[:, 1, :]), r=[sz_t], w=[sz_t])
                S.op("dve", lambda e: e.reciprocal(out=ysq[:, 1, :], in_=ysq[:, 1, :]), r=[sz_t], w=[sz_t])
                S.op("dve", lambda e: e.tensor_tensor(out=yn[:].rearrange("p (g c) -> p g c", g=4),
                                                      in0=yb[:].rearrange("p (g c) -> p g c", g=4),
                                                      in1=ysq[:, 1, :].unsqueeze(2).to_broadcast([128, 4, 256]), op=ALU.mult),
                     r=[yb_t, sz_t], w=[yn_t])
                pT_, pTt = ps[0], pst[0]
                pTb = pT_[:, :].bitcast(BF16)
                S.mm([lambda e, k=k: e.transpose(out=pTb[:, k * 128:(k + 1) * 128], in_=yn[:, k * 128:(k + 1) * 128],
                                                  identity=K.ident_b[:]) for k in range(8)], r=[yn_t, K.t], w=[pTt])
                for k in range(8):
                    S.op("act", lambda e, k=k: e.activation(out=yT[:, k, csl], in_=pTb[:, k * 128:(k + 1) * 128], func=AF.Copy,
                                                            scale=snwT[:, k:k + 1]), r=[pTt, tc], w=[yT_t])

        chunk_front(0)
        for ci in range(4):
            if ci + 1 < 4:
                chunk_front(ci + 1)
            chunk_back(ci)
        if "ssm" in C.dbg:
            dump(C, f"yT{s}_{tg}", yT[:].rearrange("p k t -> p (k t)"), [128, 8 * 512], BF16, [yT_t])
            dump(C, f"xbc{s}_{tg}", xbc[:].rearrange("p k t -> p (k t)"), [128, 16 * 512], BF16, [xbc_t])
        S.barrier()
        loc.close()
        loc = ExitStack()
        C.scope = loc
        W["wst"] = [sb(f"wst{i}", [128, 8, 512], BF16) for i in range(2)]
        W["wst_t"] = [T(f"wst{i}") for i in range(2)]
        wba = sb("wba", [128, 4, D], BF16)
        wbs = sb("wbs", [128, 8, D], BF16)
        wout = sb("wout", [128, 8, D], BF16)
        gpre = [load_w(G0), load_w(G0 + 512)]
        S.dma("pool", lambda e: e.dma_start(out=wba[:], in_=I.w_ba.rearrange("(k p) n -> p k n", p=128)), w=[tw])
        S.dma("pool", lambda e: e.dma_start(out=wbs[:], in_=I.w_bs.rearrange("(k p) n -> p k n", p=128)), w=[tw])
        S.dma("pool", lambda e: e.dma_start(out=wout[:], in_=I.w_out.rearrange("(k p) n -> p k n", p=128)), w=[tw])
        sg = sb("sg", [128, 16, 512], BF16)
        sg_t = T("sg")
        m1 = [sb(f"m1{i}", [128, 512]) for i in range(1)]
        m1_t = [T(f"m1{i}") for i in range(1)]
        mgT = sb("mgT", [128, 8, 512], BF16)
        mgT_t = T("mgT")
        xr = [sb(f"xr{i}", [128, D]) for i in range(2)]
        xr_t = [T(f"xr{i}") for i in range(2)]
        hh = [sb(f"hh{i}", [128, D]) for i in range(2)]
        hh_t = [T(f"hh{i}") for i in range(2)]
        for c in range(16):
            if c % 4 == 0:
                wg, wgt = gpre[c // 4]
            nb[0] += 1
            pq, pqt = ps[4 + nb[0] % 2], pst[4 + nb[0] % 2]
            S.mm([lambda e, k=k: e.matmul(pq[:, :], lhsT=wg[:, k, (c % 4) * 128:(c % 4 + 1) * 128], rhs=C.uT[:, k, tsl],
                                           start=(k == 0), stop=(k == 7)) for k in range(8)], r=[wgt, ut], w=[pqt])
            S.op("act", lambda e: e.activation(out=sg[:, c, :], in_=pq[:, :], func=AF.Sigmoid), r=[pqt], w=[sg_t])
            if c % 4 == 3 and c // 4 + 2 < 4:
                gpre.append(load_w(G0 + (c // 4 + 2) * 512))
        for dc in range(8):
            nb[0] += 1
            pa, pat = ps[nb[0] % 2], pst[nb[0] % 2]
            pb_, pbt = ps[2 + nb[0] % 2], pst[2 + nb[0] % 2]
            mm1, mm1t = m1[0], m1_t[0]
            S.mm([lambda e, k=k: e.matmul(pa[:, :], lhsT=wba[:, k, dc * 128:(dc + 1) * 128], rhs=C.attnT[:, k, tsl],
                                           start=(k == 0), stop=(k == 3)) for k in range(4)], r=[tw, C.attnT_t], w=[pat])
            S.mm([lambda e, k=k: e.matmul(pb_[:, :], lhsT=wbs[:, k, dc * 128:(dc + 1) * 128], rhs=yT[:, k, :],
                                           start=(k == 0), stop=(k == 7)) for k in range(8)], r=[tw, yT_t], w=[pbt])
            S.op("dve", lambda e: e.tensor_tensor(out=mm1[:], in0=pa[:, :], in1=sg[:, dc, :], op=ALU.mult),
                 r=[pat, sg_t], w=[mm1t])
            S.op("dve", lambda e: e.tensor_tensor(out=mgT[:, dc, :], in0=pb_[:, :], in1=sg[:, 8 + dc, :], op=ALU.mult),
                 r=[pbt, sg_t], w=[mgT_t])
            S.op("dve", lambda e: e.tensor_tensor(out=mgT[:, dc, :], in0=mgT[:, dc, :], in1=mm1[:], op=ALU.add),
                 r=[mm1t, mgT_t], w=[mgT_t])
        if "mg" in C.dbg:
            dump(C, f"mgT{s}_{tg}", mgT[:].rearrange("p k t -> p (k t)"), [128, 8 * 512], BF16, [mgT_t])
        rx = s * SEQ + tg * 512
        S.dma("sp", lambda e: e.dma_start(out=xr[0][:], in_=I.x[rx:rx + 128, :]), w=[xr_t[0]])
        for ti in range(4):
            nb[0] += 1
            r0 = s * SEQ + tg * 512 + ti * 128
            x_, x_t = xr[ti % 2], xr_t[ti % 2]
            h_, h_t = hh[ti % 2], hh_t[ti % 2]
            if ti + 1 < 4:
                S.dma("sp", lambda e: e.dma_start(out=xr[(ti + 1) % 2][:], in_=I.x[r0 + 128:r0 + 256, :]), w=[xr_t[(ti + 1) % 2]])
            ph = (ps[6], ps[7])
            pht = [pst[6], pst[7]]
            for hf in range(2):
                S.mm([lambda e, k=k, hf=hf: e.matmul(ph[hf][:, :], lhsT=mgT[:, k, ti * 128:(ti + 1) * 128],
                                                      rhs=wout[:, k, hf * 512:(hf + 1) * 512], start=(k == 0), stop=(k == 7))
                      for k in range(8)], r=[mgT_t, tw], w=[pht[hf]])
                S.op("dve", lambda e, hf=hf: e.tensor_tensor(out=h_[:, hf * 512:(hf + 1) * 512], in0=ph[hf][:, :],
                                                             in1=M.bc["g1"][:, hf * 512:(hf + 1) * 512], op=ALU.mult),
                     r=[pht[hf], M.t], w=[h_t])
            S.op("dve", lambda e: e.tensor_tensor(out=h_[:], in0=h_[:], in1=x_[:], op=ALU.add), r=[h_t, x_t], w=[h_t])
            phase_post_h(C, s, tg * 4 + ti, h_, h_t)
        S.barrier()
        loc.close()
        C.scope = ssd_scope


def phase_post_h(C, s, ti, h_, h_t):
    S = C.S
    r0 = s * SEQ + ti * 128
    S.dma("sp", lambda e: e.dma_start(out=C.h_scr[r0:r0 + 128, :], in_=h_[:]), r=[h_t], w=[C.h_scr_t])


def rms_rstd(C, src, src_t, ss, junk, junk_t, dim):
    S = C.S
    S.op("act", lambda e: e.activation(out=junk[:], in_=src[:], func=AF.Square, accum_out=ss[:, 0:1]), r=[src_t], w=[junk_t])
    S.op("dve", lambda e: e.tensor_scalar(out=ss[:, 1:2], in0=ss[:, 0:1], scalar1=1.0 / dim, scalar2=EPS,
                                          op0=ALU.mult, op1=ALU.add), r=[junk_t], w=[junk_t])
    S.op("act", lambda e: e.sqrt(out=ss[:, 1:2], in_=ss[:, 1:2]), r=[junk_t], w=[junk_t])
    S.op("dve", lambda e: e.reciprocal(out=ss[:, 1:2], in_=ss[:, 1:2]), r=[junk_t], w=[junk_t])


def phase_route(C):
    nc, S, sb, I, K, M = C.nc, C.S, C.sb, C.I, C.K, C.M
    ps, pst = C.ps, C.pst
    R = C.R
    tw = T("route_w", multi=True)
    wr = sb("wr", [128, 8, NE])
    wgus = sb("wgus", [128, 8, 512], BF16)
    wds = sb("wds", [128, 2, D], BF16)
    rb_bc = sb("rb_bc", [128, NE])
    iota = sb("iota", [128, NE])
    aid = sb("aid", [128, NTOK // 128, 8], I32)
    big = sb("big", [128, 4 * NE], I32)
    S.dma("sp", lambda e: e.dma_start(out=wr[:], in_=I.w_router.rearrange("(k p) n -> p k n", p=128)), w=[tw])
    S.dma("pool", lambda e: e.dma_start(out=wgus[:, :, 0:256], in_=I.w_gate_s.rearrange("(k p) n -> p k n", p=128)), w=[tw])
    S.dma("pool", lambda e: e.dma_start(out=wgus[:, :, 256:512], in_=I.w_up_s.rearrange("(k p) n -> p k n", p=128)), w=[tw])
    S.dma("pool", lambda e: e.dma_start(out=wds[:], in_=I.w_down_s.rearrange("(k p) n -> p k n", p=128)), w=[tw])
    S.dma("sp", lambda e: e.dma_start(out=rb_bc[:], in_=I.rbias_row.partition_broadcast(128)), w=[tw])
    S.dma("sp", lambda e: e.dma_start(out=iota[:], in_=I.iota_row.partition_broadcast(128)), w=[tw])
    S.dma("sp", lambda e: e.dma_start(out=aid[:], in_=I.aid), w=[tw])
    S.dma("sp", lambda e: e.dma_start(out=big[:], in_=I.bigtab), w=[tw])
    S.dma("sp", lambda e: e.dma_start(out=C.slot_info, in_=big[:]), r=[tw], w=[C.slot_t])
    base = sb("cnt_base", [128, NE])
    base_t = T("cnt_base")
    S.op("dve", lambda e: e.memset(base[:], 0.0), w=[base_t])
    hin = [sb(f"hin{i}", [128, D]) for i in range(2)]
    hin_t = [T(f"hin{i}") for i in range(2)]
    junk = sb("rjunk", [128, D])
    junk_t = T("rjunk")
    ss2 = [sb(f"rss{i}", [128, 2]) for i in range(2)]
    DB = {}
    for nm, shp, dt_ in (("u2", [128, D], F32), ("u2b", [128, D], BF16), ("u2Tf", [128, 8, 128], F32), ("u2Tb", [128, 8, 128], BF16),
                         ("sc", [128, NE], F32), ("bi", [128, NE], F32), ("mk", [128, 8, 32], F32), ("posf", [128, NE], F32),
                         ("mask8", [128, NE], BF16), ("rj", [128, NE], F32), ("m8g", [128, 8, 8], F32), ("sm", [128, 12, 8], F32),
                         ("smi", [128, 4, 8], I32), ("idx8", [128, 8], U32)):
        DB[nm] = [sb(f"r_{nm}{i}", shp, dt_) for i in range(2)]
    DBT = {nm: [T(f"r_{nm}{i}") for i in range(2)] for nm in ("u2", "u2b", "u2T", "rt")}
    DBTT = [{n_: T(f"rr_{n_}{i}", multi=(n_ in ("m8g", "sel", "pos"))) for n_ in
             ("sc", "bi", "m8g", "g", "mk", "v8", "idx", "mask", "posf", "sel", "pos")} for i in range(2)]
    rj2s = [sb(f"r_rj2_{i}", [128, NE]) for i in range(2)]
    sgl = sb("s_sgl", [128, 256])
    hsb = sb("s_hsb", [128, 256], BF16)
    hsT = sb("s_hsT", [128, 2, 128], BF16)
    ysh = sb("s_ysh", [128, D])
    sh_t = T("shared_tmp")
    for s_ in range(NSEQ):
        with ExitStack() as loc:
            C.scope = loc
            M.bc = {nm: sb(f"bc_{nm}", [128, D]) for nm in ("sh2", "sc2")}
            with ExitStack() as loc2:
                C.scope = loc2
                phase_mod(C, s_, which=("sh2", "sc2"))
                S.barrier()
            C.scope = loc
            def bind(ti):
                gt = s_ * (SEQ // 128) + ti
                r0 = gt * 128
                q_ = gt % 2
                return dict(gt=gt, r0=r0, q_=q_)

            def front_stage(ti):
                    gt = s_ * (SEQ // 128) + ti
                    r0 = gt * 128
                    h_, h_t = hin[gt % 2], hin_t[gt % 2]
                    q_ = gt % 2
                    ss = ss2[q_]
                    u2, u2b, u2Tf, u2Tb = DB["u2"][q_], DB["u2b"][q_], DB["u2Tf"][q_], DB["u2Tb"][q_]
                    sc, bi, mk, posf, mask8, rj = DB["sc"][q_], DB["bi"][q_], DB["mk"][q_], DB["posf"][q_], DB["mask8"][q_], DB["rj"][q_]
                    m8g, sm, smi, idx8 = DB["m8g"][q_], DB["sm"][q_], DB["smi"][q_], DB["idx8"][q_]
                    u2_t, u2b_t, u2T_t, rt = DBT["u2"][q_], DBT["u2b"][q_], DBT["u2T"][q_], DBT["rt"][q_]
                    rj2 = rj2s[q_]
                    S.dma("sp", lambda e: e.dma_start(out=h_[:], in_=C.h_scr[r0:r0 + 128, :]), r=[C.h_scr_t], w=[h_t])
                    rms_rstd(C, h_, h_t, ss, junk, junk_t, D)
                    S.op("dve", lambda e: e.scalar_tensor_tensor(out=u2[:], in0=h_[:], scalar=ss[:, 1:2], in1=M.bc["sc2"][:],
                                                                 op0=ALU.mult, op1=ALU.mult), r=[h_t, junk_t, M.t], w=[u2_t])
                    S.op("dve", lambda e: e.tensor_tensor(out=u2[:], in0=u2[:], in1=M.bc["sh2"][:], op=ALU.add), r=[u2_t, M.t], w=[u2_t])
                    S.op("act", lambda e: e.copy(out=u2b[:], in_=u2[:]), r=[u2_t], w=[u2b_t])
                    S.dma("pool", lambda e: e.dma_start(out=C.u2_rows[r0:r0 + 128, :], in_=u2b[:]), r=[u2b_t], w=[C.u2rows_t])
                    for hf in range(2):
                        S.mm([lambda e, k=k: e.transpose(out=ps[hf][:, (k % 4) * 128:(k % 4 + 1) * 128], in_=u2[:, k * 128:(k + 1) * 128],
                                                          identity=K.ident_f[:]) for k in range(hf * 4, hf * 4 + 4)],
                             r=[u2_t, K.t], w=[pst[hf]])
                        S.op("act", lambda e, hf=hf: e.copy(out=u2Tf[:, hf * 4:(hf + 1) * 4, :].rearrange("p k t -> p (k t)"), in_=ps[hf][:, :]),
                             r=[pst[hf]], w=[u2T_t])
                        S.op("act", lambda e, hf=hf: e.copy(out=u2Tb[:, hf * 4:(hf + 1) * 4, :].rearrange("p k t -> p (k t)"), in_=ps[hf][:, :]),
                             r=[pst[hf]], w=[u2T_t])
                    S.mm([lambda e, k=k: e.matmul(ps[2][:, 0:NE], lhsT=u2Tf[:, k, :], rhs=wr[:, k, :], start=(k == 0), stop=(k == 7))
                          for k in range(8)], r=[u2T_t, tw], w=[pst[2]])
                    w_ = [rt]
                    TT = DBTT[q_]
                    t_sc, t_bi, t_m8g, t_g, t_mk, t_v8, t_idx, t_mask, t_posf, t_sel, t_pos = (TT[n_] for n_ in (
                        "sc", "bi", "m8g", "g", "mk", "v8", "idx", "mask", "posf", "sel", "pos"))
                    S.op("act", lambda e: e.activation(out=sc[:], in_=ps[2][:, 0:NE], func=AF.Sigmoid), r=[pst[2]], w=[t_sc])
                    S.op("dve", lambda e: e.tensor_tensor(out=bi[:], in0=sc[:], in1=rb_bc[:], op=ALU.add), r=[t_sc, tw], w=[t_bi])
                    S.mm([lambda e, k=k: e.matmul(ps[4][:, :], lhsT=u2Tb[:, k, :], rhs=wgus[:, k, :], start=(k == 0), stop=(k == 7))
                          for k in range(8)], r=[u2T_t, tw], w=[pst[4]])
                    S.op("act", lambda e: e.activation(out=sgl[:], in_=ps[4][:, 0:256], func=AF.Silu), r=[pst[4]], w=[sh_t])
                    S.op("dve", lambda e: e.tensor_tensor(out=hsb[:], in0=sgl[:], in1=ps[4][:, 256:512], op=ALU.mult), r=[pst[4], sh_t], w=[sh_t])
                    p5b = ps[5][:, :].bitcast(BF16)
                    S.mm([lambda e, f=f: e.transpose(out=p5b[:, f * 128:(f + 1) * 128], in_=hsb[:, f * 128:(f + 1) * 128],
                                                      identity=K.ident_b[:]) for f in range(2)], r=[sh_t, K.t], w=[pst[5]])
                    S.op("act", lambda e: e.copy(out=hsT[:].rearrange("p f t -> p (f t)"), in_=p5b[:, 0:256]), r=[pst[5]], w=[sh_t])
                    for hf in range(2):
                        S.mm([lambda e, f=f, hf=hf: e.matmul(ps[6 + hf][:, :], lhsT=hsT[:, f, :], rhs=wds[:, f, hf * 512:(hf + 1) * 512],
                                                              start=(f == 0), stop=(f == 1)) for f in range(2)], r=[sh_t, tw], w=[pst[6 + hf]])
                    S.op("act", lambda e: e.copy(out=ysh[:, 0:512], in_=ps[6][:, :]), r=[pst[6]], w=[sh_t])
                    S.op("act", lambda e: e.copy(out=ysh[:, 512:1024], in_=ps[7][:, :]), r=[pst[7]], w=[sh_t])
                    S.dma("pool", lambda e: e.dma_start(out=C.sh_out[r0:r0 + 128, :], in_=ysh[:]), r=[sh_t], w=[C.sh_out_t])

            def back_stage(ti):
                    gt = s_ * (SEQ // 128) + ti
                    r0 = gt * 128
                    h_, h_t = hin[gt % 2], hin_t[gt % 2]
                    q_ = gt % 2
                    ss = ss2[q_]
                    u2, u2b, u2Tf, u2Tb = DB["u2"][q_], DB["u2b"][q_], DB["u2Tf"][q_], DB["u2Tb"][q_]
                    sc, bi, mk, posf, mask8, rj = DB["sc"][q_], DB["bi"][q_], DB["mk"][q_], DB["posf"][q_], DB["mask8"][q_], DB["rj"][q_]
                    m8g, sm, smi, idx8 = DB["m8g"][q_], DB["sm"][q_], DB["smi"][q_], DB["idx8"][q_]
                    u2_t, u2b_t, u2T_t, rt = DBT["u2"][q_], DBT["u2b"][q_], DBT["u2T"][q_], DBT["rt"][q_]
                    rj2 = rj2s[q_]
                    w_ = [rt]
                    TT = DBTT[q_]
                    t_sc, t_bi, t_m8g, t_g, t_mk, t_v8, t_idx, t_mask, t_posf, t_sel, t_pos = (TT[n_] for n_ in (
                        "sc", "bi", "m8g", "g", "mk", "v8", "idx", "mask", "posf", "sel", "pos"))
                    for g in range(8):
                        S.op("dve", lambda e, g=g: e.max(out=m8g[:, g, :], in_=bi[:, g * 32:(g + 1) * 32]), r=[t_bi], w=[t_m8g])
                    S.op("dve", lambda e: e.tensor_tensor(out=sm[:, 0, :], in0=m8g[:, :, 0], in1=m8g[:, :, 1], op=ALU.add), r=[t_m8g], w=[t_g])
                    S.op("dve", lambda e: e.max(out=sm[:, 1, :], in_=sm[:, 0, :]), r=[t_g], w=[t_g])
                    S.op("dve", lambda e: e.tensor_scalar(out=sm[:, 2, :], in0=sm[:, 0, :], scalar1=sm[:, 1, 3:4], scalar2=None,
                                                          op0=ALU.is_ge), r=[t_g], w=[t_g])
                    S.op("dve", lambda e: e.tensor_scalar(out=sm[:, 3, :], in0=sm[:, 2, :], scalar1=100.0, scalar2=-100.0,
                                                          op0=ALU.mult, op1=ALU.add), r=[t_g], w=[t_g])
                    S.op("dve", lambda e: e.tensor_tensor(out=mk[:], in0=bi[:].rearrange("p (g c) -> p g c", g=8),
                                                          in1=sm[:, 2, :].unsqueeze(2).to_broadcast([128, 8, 32]), op=ALU.mult),
                         r=[t_g, t_bi], w=[t_mk])
                    S.op("dve", lambda e: e.tensor_tensor(out=mk[:], in0=mk[:], in1=sm[:, 3, :].unsqueeze(2).to_broadcast([128, 8, 32]),
                                                          op=ALU.add), r=[t_g, t_mk], w=[t_mk])
                    mkf = mk[:].rearrange("p g c -> p (g c)")
                    S.op("dve", lambda e: e.max(out=sm[:, 4, :], in_=mkf), r=[t_mk], w=[t_v8])
                    S.op("dve", lambda e: e.tensor_scalar(out=mask8[:], in0=mkf, scalar1=sm[:, 4, 7:8], scalar2=None, op0=ALU.is_ge),
                         r=[t_mk, t_v8], w=[t_mask])
                    S.op("dve", lambda e: e.tensor_tensor(out=rj[:], in0=sc[:], in1=mask8[:], op=ALU.mult), r=[t_sc, t_mask], w=[t_sel])
                    S.op("dve", lambda e: e.max(out=sm[:, 6, :], in_=rj[:]), r=[t_sel], w=[t_sel])
                    S.op("dve", lambda e: e.max_index(out=idx8[:], in_max=sm[:, 6, :], in_values=rj[:]), r=[t_sel], w=[t_idx])
                    S.op("dve", lambda e: e.tensor_copy(out=sm[:, 5, :], in_=idx8[:]), r=[t_idx], w=[t_idx])
                    S.mm([lambda e: e.matmul(ps[3][:, 0:NE], lhsT=K.cm_b[:, 5, :], rhs=mask8[:], start=True, stop=True),
                          lambda e: e.matmul(ps[3][:, NE:2 * NE], lhsT=K.ones_b[:], rhs=mask8[:], start=True, stop=True)],
                         r=[t_mask, K.t], w=[pst[3]])
                    S.op("dve", lambda e: e.tensor_tensor(out=posf[:], in0=ps[3][:, 0:NE], in1=base[:], op=ALU.add),
                         r=[pst[3], base_t], w=[t_posf])
                    S.op("dve", lambda e: e.tensor_tensor(out=base[:], in0=ps[3][:, NE:2 * NE], in1=base[:], op=ALU.add),
                         r=[pst[3], t_posf], w=[base_t])
                    for k in range(8):
                        S.op("dve", lambda e, k=k: e.scalar_tensor_tensor(out=rj2[:], in0=iota[:], scalar=sm[:, 5, k:k + 1], in1=posf[:],
                                                                          op0=ALU.is_equal, op1=ALU.mult, accum_out=sm[:, 7, k:k + 1]),
                             r=[t_idx, t_posf, tw], w=[t_pos])
                    w_ = [rt]
                    rr = w_ + [t_idx, t_sel, t_pos]
                    S.op("dve", lambda e: e.tensor_copy(out=smi[:, 0, :], in_=sm[:, 7, :]), r=rr, w=w_)
                    S.op("dve", lambda e: e.tensor_scalar(out=smi[:, 1, :], in0=smi[:, 0, :], scalar1=127, scalar2=None,
                                                          op0=ALU.bitwise_and), r=w_, w=w_)
                    S.op("dve", lambda e: e.tensor_scalar(out=smi[:, 2, :], in0=smi[:, 0, :], scalar1=7, scalar2=None,
                                                          op0=ALU.arith_shift_right), r=w_, w=w_)
                    S.op("dve", lambda e: e.tensor_copy(out=sm[:, 10, :], in_=smi[:, 1, :]), r=w_, w=w_)
                    S.op("dve", lambda e: e.tensor_copy(out=sm[:, 11, :], in_=smi[:, 2, :]), r=w_, w=w_)
                    S.op("dve", lambda e: e.scalar_tensor_tensor(out=sm[:, 8, :], in0=sm[:, 5, :], scalar=4.0, in1=sm[:, 11, :],
                                                                 op0=ALU.mult, op1=ALU.add), r=rr, w=w_)
                    S.op("dve", lambda e: e.scalar_tensor_tensor(out=sm[:, 8, :], in0=sm[:, 10, :], scalar=float(4 * NE), in1=sm[:, 8, :],
                                                                 op0=ALU.mult, op1=ALU.add), r=w_, w=w_)
                    S.op("dve", lambda e: e.tensor_scalar(out=sm[:, 9, :], in0=sm[:, 7, :], scalar1=float(CAP), scalar2=None,
                                                          op0=ALU.is_lt), r=rr, w=w_)
                    S.op("dve", lambda e: e.tensor_scalar(out=sm[:, 10, :], in0=sm[:, 9, :], scalar1=-1.0e9, scalar2=1.0e9,
                                                          op0=ALU.mult, op1=ALU.add), r=w_, w=w_)
                    S.op("dve", lambda e: e.tensor_tensor(out=sm[:, 8, :], in0=sm[:, 8, :], in1=sm[:, 10, :], op=ALU.add), r=w_, w=w_)
                    S.op("dve", lambda e: e.tensor_copy(out=smi[:, 3, :], in_=sm[:, 8, :]), r=w_, w=w_)
                    S.op("dve", lambda e: e.reduce_sum(out=sm[:, 11, 0:1], in_=sm[:, 6, :], axis=AX.X), r=rr, w=w_)
                    S.op("dve", lambda e: e.reciprocal(out=sm[:, 11, 0:1], in_=sm[:, 11, 0:1]), r=w_, w=w_)
                    S.op("dve", lambda e: e.tensor_scalar(out=sm[:, 6, :], in0=sm[:, 6, :], scalar1=sm[:, 11, 0:1], scalar2=2.5,
                                                          op0=ALU.mult, op1=ALU.mult), r=rr, w=w_ + [t_sel])
                    S.op("dve", lambda e: e.tensor_tensor(out=R.wsel[:, gt, :], in0=sm[:, 6, :], in1=sm[:, 9, :], op=ALU.mult),
                         r=w_ + [t_sel], w=[R.wsel_t])
                    for k in range(8):
                        S.dma("pool", lambda e, k=k: e.indirect_dma_start(
                            out=C.slot_info_flat, out_offset=bass.IndirectOffsetOnAxis(ap=smi[:, 3, k:k + 1], axis=0),
                            in_=aid[:, gt, k:k + 1], in_offset=None, bounds_check=R.reg_slot, oob_is_err=False),
                            r=[rt, tw, C.slot_t], w=[C.slot_sc])
                    if "route" in C.dbg:
                        dump(C, f"ridx{gt}", sm[:, 5, :], [128, 8], F32, [rt])
                        dump(C, f"rslot{gt}", sm[:, 8, :], [128, 8], F32, [rt])

            NT = SEQ // 128
            front_stage(0)
            for ti in range(NT):
                if ti + 1 < NT:
                    front_stage(ti + 1)
                back_stage(ti)
            S.barrier()


def phase_experts(C):
    nc, S, sb, I, K, R = C.nc, C.S, C.sb, C.I, C.K, C.R
    ps, pst = C.ps, C.pst
    NB = CAP // 128
    NBLK = NE * NB
    SI = sb("SI", [128, NE * NB], I32)
    TK = sb("TK", [128, NE * NB], I32)
    si_t = T("SI")
    S.dma("sp", lambda e: e.dma_start(out=SI[:], in_=C.slot_info), r=[C.slot_t, C.slot_sc], w=[si_t])
    S.op("dve", lambda e: e.tensor_scalar(out=TK[:], in0=SI[:], scalar1=3, scalar2=None, op0=ALU.arith_shift_right),
         r=[si_t], w=[si_t])
    NW = 3
    wg = [sb(f"wg{i}", [128, 8, 256], BF16) for i in range(NW)]
    wu = [sb(f"wu{i}", [128, 8, 256], BF16) for i in range(NW)]
    wd = [sb(f"wd{i}", [128, 2, D], BF16) for i in range(NW)]
    wg_t = [T(f"ewg{i}") for i in range(NW)]
    wu_t = [T(f"ewu{i}") for i in range(NW)]
    wd_t = [T(f"ewd{i}") for i in range(NW)]
    xg = [[sb(f"xg{i}_{b}", [128, D], BF16) for b in range(NB)] for i in range(2)]
    xg_t = [[T(f"xg{i}_{b}") for b in range(NB)] for i in range(2)]
    for i in range(2):
        for b in range(NB):
            S.op("dve", lambda e, i=i, b=b: e.memset(xg[i][b][:], 0.0), w=[xg_t[i][b]])
    xgT = [sb(f"xgT{i}", [128, 8, 128], BF16) for i in range(2)]
    xgT_t = [T(f"xgT{i}") for i in range(2)]
    sgl = [sb(f"esgl{i}", [128, 256]) for i in range(2)]
    sgl_t = [T(f"esgl{i}") for i in range(2)]
    hb = [sb(f"ehb{i}", [128, 256], BF16) for i in range(2)]
    hb_t = [T(f"ehb{i}") for i in range(2)]
    hT = [sb(f"ehT{i}", [128, 2, 128], BF16) for i in range(2)]
    hT_t = [T(f"ehT{i}") for i in range(2)]
    NY = 3
    yo = [sb(f"eyo{i}", [128, D]) for i in range(NY)]
    yo_t = [T(f"eyo{i}") for i in range(NY)]
    yo_t2 = [T(f"eyo{i}b") for i in range(NY)]
    p4b = ps[4][:, :].bitcast(BF16)
    pht_t = [T("psHT0"), T("psHT1")]

    def load_weights(e_):
        i = e_ % NW
        S.dma("pool", lambda e: e.dma_start(out=wg[i][:], in_=I.w_gate_e[e_].rearrange("(p k) n -> p k n", p=128)), w=[wg_t[i]])
        S.dma("pool", lambda e: e.dma_start(out=wu[i][:], in_=I.w_up_e[e_].rearrange("(p k) n -> p k n", p=128)), w=[wu_t[i]])
        S.dma("pool", lambda e: e.dma_start(out=wd[i][:], in_=I.w_down_e[e_].rearrange("(p k) n -> p k n", p=128)), w=[wd_t[i]])

    def gathers(e_):
        i = e_ % 2
        for b in range(NB):
            col = e_ * NB + b
            S.dma("pool", lambda e, b=b, col=col: e.indirect_dma_start(
                out=xg[i][b][:, :], out_offset=None, in_=C.u2_rows,
                in_offset=bass.IndirectOffsetOnAxis(ap=TK[:, col:col + 1], axis=0), bounds_check=R.reg_tok, oob_is_err=False),
                r=[si_t, C.u2rows_t], w=[xg_t[i][b]])

    xgTe = [sb(f"xgTe{i}", [128, 8, CAP], BF16) for i in range(2)]
    xgTe_t = [[T(f"xgTe{i}_{b}") for b in range(NB)] for i in range(2)]
    sge = [sb(f"sge{i}", [128, CAP]) for i in range(2)]
    sge_t = [T(f"sge{i}") for i in range(2)]
    hTe = [sb(f"hTe{i}", [128, 2, CAP], BF16) for i in range(2)]
    hTe_t = [[T(f"hTe{i}_{f}") for f in range(2)] for i in range(2)]

    def stA(e_, b):
        n = e_ * NB + b
        j = n % 2
        pxb = ps[j][:, :].bitcast(BF16)
        S.mm([lambda e, k=k: e.transpose(out=pxb[:, k * 128:(k + 1) * 128], in_=xg[e_ % 2][b][:, k::8],
                                          identity=K.ident_b[:]) for k in range(8)], r=[xg_t[e_ % 2][b], K.t], w=[pst[j]])
        dst = xgTe[e_ % 2][:, :, b * 128:(b + 1) * 128]
        src = pxb.rearrange("p (k t) -> p k t", k=8)
        if b % 2 == 0:
            S.op("dve", lambda e: e.tensor_copy(out=dst, in_=src), r=[pst[j]], w=[xgTe_t[e_ % 2][b]])
        else:
            S.op("act", lambda e: e.copy(out=dst, in_=src), r=[pst[j]], w=[xgTe_t[e_ % 2][b]])

    def stB(e_, c):
        w = e_ % NW
        i = e_ % 2
        ww, wt = (wg, wg_t) if c < 2 else (wu, wu_t)
        f = c % 2
        S.mm([lambda e, k=k: e.matmul(ps[2 + c][:, :], lhsT=ww[w][:, k, f::2], rhs=xgTe[i][:, k, :], start=(k == 0), stop=(k == 7))
              for k in range(8)], r=xgTe_t[i] + [wt[w]], w=[pst[2 + c]])
        if c < 2:
            S.op("act", lambda e: e.activation(out=sge[c][:], in_=ps[2 + c][:, :], func=AF.Silu), r=[pst[2 + c]], w=[sge_t[c]])
        else:
            S.op("dve", lambda e: e.tensor_tensor(out=hTe[i][:, f, :], in0=sge[f][:], in1=ps[2 + c][:, :], op=ALU.mult),
                 r=[pst[2 + c], sge_t[f]], w=[hTe_t[i][f]])

    def stD(e_, b):
        n = e_ * NB + b
        i = e_ % 2
        w = e_ % NW
        y = n % NY
        for hf in range(2):
            S.mm([lambda e, f=f, hf=hf: e.matmul(ps[6 + hf][:, :], lhsT=hTe[i][:, f, b * 128:(b + 1) * 128],
                                                  rhs=wd[w][:, f, hf * 512:(hf + 1) * 512], start=(f == 0), stop=(f == 1))
                  for f in range(2)], r=hTe_t[i] + [wd_t[w]], w=[pst[6 + hf]])
        S.op("act", lambda e: e.copy(out=yo[y][:, 0:512], in_=ps[6][:, :]), r=[pst[6]], w=[yo_t[y]])
        S.op("dve", lambda e: e.tensor_copy(out=yo[y][:, 512:1024], in_=ps[7][:, :]), r=[pst[7]], w=[yo_t2[y]])
        S.dma("pool", lambda e: e.indirect_dma_start(
            out=C.ye2, out_offset=bass.IndirectOffsetOnAxis(ap=SI[:, n:n + 1], axis=0), in_=yo[y][:, :], in_offset=None,
            bounds_check=R.reg_aid, oob_is_err=False), r=[yo_t[y], yo_t2[y], si_t], w=[C.ye2_t])

    load_weights(0)
    gathers(0)
    load_weights(1)
    gathers(1)
    for b in range(NB):
        stA(0, b)
    for e_ in range(NE + 1):
        if e_ + 2 < NE:
            gathers(e_ + 2)
        for b in range(NB):
            if e_ + 1 < NE:
                stA(e_ + 1, b)
            if e_ < NE:
                stB(e_, b)
            if e_ >= 1:
                stD(e_ - 1, b)
        if e_ + 2 < NE:
            load_weights(e_ + 2)


def phase_final(C):
    nc, S, sb, I, K, M, R = C.nc, C.S, C.sb, C.I, C.K, C.M, C.R
    M.g2_bc = [sb(f"bc_g2{b}", [128, D]) for b in range(NSEQ)]
    nfin_bc = sb("nfin_bc", [128, D])
    S.dma("sp", lambda e: e.dma_start(out=nfin_bc[:], in_=I.nfin_row.partition_broadcast(128)), w=[M.t])
    outer = C.scope
    with ExitStack() as loc:
        C.scope = loc
        phase_mod(C, "g2")
        S.barrier()
    C.scope = outer
    hin = [sb(f"fh{i}", [128, D]) for i in range(2)]
    hin_t = [T(f"fh{i}") for i in range(2)]
    shd = [sb(f"fsh{i}", [128, D]) for i in range(2)]
    shd_t = [T(f"fsh{i}") for i in range(2)]
    ye = [sb(f"fye{i}", [128, 8, D]) for i in range(2)]
    ye_t = [T(f"fye{i}") for i in range(2)]
    junk = sb("fjunk", [128, D])
    junk_t = T("fjunk")
    ss = [sb(f"fss{i}", [128, 2]) for i in range(2)]
    ob = [sb(f"fo{i}", [128, D]) for i in range(2)]
    ob_t = [T(f"fo{i}") for i in range(2)]
    def fin1(gt):
            s_ = gt // (SEQ // 128)
            r0 = gt * 128
            h_, h_t = hin[gt % 2], hin_t[gt % 2]
            a_, a_t = shd[gt % 2], shd_t[gt % 2]
            o_, o_t = ob[gt % 2], ob_t[gt % 2]
            y_, y_t = ye[gt % 2], ye_t[gt % 2]
            S.dma("sp", lambda e: e.dma_start(out=h_[:], in_=C.h_scr[r0:r0 + 128, :]), r=[C.h_scr_t], w=[h_t])
            S.dma("sp", lambda e: e.dma_start(out=a_[:], in_=C.sh_out[r0:r0 + 128, :]), r=[C.sh_out_t], w=[a_t])
            S.dma("sp", lambda e: e.dma_start(out=y_[:], in_=C.ye2[r0 * 8:(r0 + 128) * 8, :].rearrange("(t k) d -> t k d", k=8)),
                  r=[C.ye2_t], w=[y_t])
            for k in range(8):
                S.op("dve", lambda e, k=k: e.scalar_tensor_tensor(out=a_[:], in0=y_[:, k, :], scalar=R.wsel[:, gt, k:k + 1],
                                                                  in1=a_[:], op0=ALU.mult, op1=ALU.add),
                     r=[y_t, R.wsel_t, a_t], w=[a_t])
            S.op("dve", lambda e: e.tensor_tensor(out=a_[:], in0=a_[:], in1=M.g2_bc[s_][:], op=ALU.mult), r=[a_t, M.t], w=[a_t])
            S.op("dve", lambda e: e.tensor_tensor(out=a_[:], in0=a_[:], in1=h_[:], op=ALU.add), r=[a_t, h_t], w=[a_t])

    def fin2(gt):
            s_ = gt // (SEQ // 128)
            r0 = gt * 128
            h_, h_t = hin[gt % 2], hin_t[gt % 2]
            a_, a_t = shd[gt % 2], shd_t[gt % 2]
            o_, o_t = ob[gt % 2], ob_t[gt % 2]
            y_, y_t = ye[gt % 2], ye_t[gt % 2]
            rms_rstd(C, a_, a_t, ss[gt % 2], junk, junk_t, D)
            S.op("dve", lambda e: e.scalar_tensor_tensor(out=o_[:], in0=a_[:], scalar=ss[gt % 2][:, 1:2], in1=nfin_bc[:],
                                                         op0=ALU.mult, op1=ALU.mult), r=[a_t, junk_t, M.t], w=[o_t])
            S.dma("pool", lambda e: e.dma_start(out=C.out[r0:r0 + 128, :], in_=o_[:]), r=[o_t])


    NT_ = NTOK // 128
    fin1(0)
    for gt in range(NT_):
        if gt + 1 < NT_:
            fin1(gt + 1)
        fin2(gt)


def host_inputs(inputs):
    f = np.float32
    x = np.asarray(inputs["x"], f)
    c = np.asarray(inputs["c"], f)
    pos = np.asarray(inputs["positions"], np.int32)
    shared = {}
    shared["w_mod"] = np.ascontiguousarray(inputs["w_mod"][0], f)
    bm = np.asarray(inputs["b_mod"][0], f)
    shared["b_modT"] = np.ascontiguousarray(bm.reshape(48, 128).T)
    shared["b_mod_row"] = bm.reshape(1, -1)
    shared["nmwT"] = np.ascontiguousarray(np.asarray(inputs["norm_mix_w"][0], f).reshape(8, 128).T)
    shared["nfw_row"] = np.asarray(inputs["norm_ffn_w"][0], f).reshape(1, -1)
    shared["nfin_row"] = np.asarray(inputs["norm_final_w"], f).reshape(1, -1)
    shared["w_in"] = np.ascontiguousarray(inputs["w_in"][0], f)
    shared["ident"] = np.eye(128, dtype=f)
    shared["w_ba"] = np.ascontiguousarray(inputs["w_branch_attn"][0], f)
    shared["w_bs"] = np.ascontiguousarray(inputs["w_branch_ssm"][0], f)
    shared["w_out"] = np.ascontiguousarray(inputs["w_out"][0], f)
    cw = np.asarray(inputs["conv_w"][0], f)
    shared["conv_wT"] = np.ascontiguousarray(cw.reshape(4, 16, 128).transpose(2, 1, 0))
    shared["conv_bT"] = np.ascontiguousarray(np.asarray(inputs["conv_b"][0], f).reshape(16, 128).T)
    shared["snwT"] = np.ascontiguousarray(np.asarray(inputs["ssm_norm_w"][0], f).reshape(8, 128).T)
    shared["hrow"] = np.concatenate([np.asarray(inputs[k][0], f) for k in ("dt_bias", "a_log", "d_skip")]).reshape(1, 48)
    cm = np.zeros((128, 6, 128), f)
    for m in range(32):
        cm[(m + 16) % 32, 0, m] = 1.0
    kk, qq = np.meshgrid(np.arange(128), np.arange(128), indexing="ij")
    cm[:, 1, :] = np.where(kk <= qq, 0.0, NEG)
    cm[:, 2, :] = np.where(kk >= qq, 0.0, NEG)
    cm[:, 3, :] = (kk <= qq).astype(f)
    cm[:, 4, :] = (kk > qq).astype(f)
    cm[:, 5, :] = (kk < qq).astype(f)
    shared["w_router"] = np.ascontiguousarray(inputs["w_router"][0], f)
    shared["rbias_row"] = np.asarray(inputs["router_bias"][0], f).reshape(1, -1)
    shared["iota_row"] = np.arange(NE, dtype=f).reshape(1, -1)
    tokid = (np.arange(NTOK // 128)[None, :, None] * 128 + np.arange(128)[:, None, None])
    shared["aid"] = np.ascontiguousarray((tokid * 8 + np.arange(8)[None, None, :]).astype(np.int32))
    shared["bigtab"] = np.full((128, 4 * NE), 0x3FFFFFF8, np.int32)
    shared["w_gate_s"] = np.ascontiguousarray(inputs["w_gate_s"][0], f)
    shared["w_gate_e"] = np.ascontiguousarray(inputs["w_gate_e"][0], f)
    shared["w_up_e"] = np.ascontiguousarray(inputs["w_up_e"][0], f)
    shared["w_down_e"] = np.ascontiguousarray(inputs["w_down_e"][0], f)
    shared["w_up_s"] = np.ascontiguousarray(inputs["w_up_s"][0], f)
    shared["w_down_s"] = np.ascontiguousarray(inputs["w_down_s"][0], f)
    shared["cmat"] = cm
    rc = np.zeros((128, 2), f)
    half = 16
    invf = (500000.0 ** (-np.arange(half, dtype=np.float32) / half)).astype(f)
    rc[:32, 0] = np.concatenate([invf, invf])
    rc[:32, 1] = np.concatenate([-np.ones(16, f), np.ones(16, f)])
    shared["ropec"] = rc
    maps = []
    for cid in range(NCORES):
        m = dict(shared)
        m["x"] = np.ascontiguousarray(x[cid * NSEQ:(cid + 1) * NSEQ].reshape(NTOK, D))
        cc = c[cid * NSEQ:(cid + 1) * NSEQ]
        m["cT"] = np.ascontiguousarray(cc.reshape(NSEQ, 8, 128).transpose(2, 1, 0))
        m["pos"] = np.ascontiguousarray(pos[cid * NSEQ:(cid + 1) * NSEQ])
        maps.append(m)
    return maps


_CACHE = {}


def kernel(**inputs):
    if "nc" not in _CACHE:
        _CACHE["nc"] = build()
    nc, C = _CACHE["nc"]
    maps = host_inputs(inputs)
    res = run_bass_kernel_spmd(nc, maps, core_ids=list(range(NCORES)))
    out = np.concatenate([r["out"].reshape(NSEQ, SEQ, D) for r in res.results], axis=0)
    return out.astype(np.float32)
```

```python
import numpy as np
import ml_dtypes
from contextlib import ExitStack
import concourse.bass as bass
import concourse.mybir as mybir
from concourse.bass_utils import run_bass_kernel_spmd

F32 = mybir.dt.float32
BF16 = mybir.dt.bfloat16
I32 = mybir.dt.int32
U32 = mybir.dt.uint32
ALU = mybir.AluOpType
AF = mybir.ActivationFunctionType
AX = mybir.AxisListType

NCORES = 8
D = 1024
SEQ = 2048
NSEQ = 2
NTOK = NSEQ * SEQ
IN_DIM = 9744
Q0, K0, V0, Z0, X0, DT0, G0 = 0, 1536, 3072, 4608, 5632, 7680, 7696
NE = 256
CAP = 512
EPS = 1e-6
NEG = -30000.0


class Tok:
    __slots__ = ("key", "sem", "val")

    def __init__(self, key, sem, val):
        self.key, self.sem, self.val = key, sem, val


class T:
    __slots__ = ("name", "w", "r", "multi", "ws")

    def __init__(self, name="", multi=False):
        self.name, self.w, self.r, self.multi, self.ws = name, None, {}, multi, {}


class Sched:
    def __init__(self, nc, es, nslots=8):
        self.nc = nc
        self.eng = dict(pe=nc.tensor, act=nc.scalar, dve=nc.vector, pool=nc.gpsimd, sp=nc.sync)
        self.sem = {k: es.enter_context(nc.semaphore("sem_" + k)) for k in ("pe", "act", "dve", "pool")}
        self.cnt = dict.fromkeys(self.sem, 0)
        self.seen = {k: {} for k in self.eng}
        self.slots = {q: [[es.enter_context(nc.semaphore(f"dq_{q}{i}")), 0] for i in range(nslots)]
                      for q in ("sp", "pool", "act")}
        self.slot_i = dict.fromkeys(self.slots, 0)
        self.ninst = 0

    def _wait(self, en, toks):
        e, seen = self.eng[en], self.seen[en]
        for tk in toks:
            if tk is None or seen.get(tk.key, 0) >= tk.val:
                continue
            if tk.key == en == "pe":
                continue
            e.wait_ge(tk.sem, tk.val)
            seen[tk.key] = tk.val

    @staticmethod
    def _deps(r, w):
        toks = []
        for t in r:
            toks.append(t.w)
            if t.multi:
                toks.extend(t.ws.values())
        for t in w:
            if not t.multi:
                toks.append(t.w)
            toks.extend(t.r.values())
        return toks

    @staticmethod
    def _commit(tok, r, w):
        for t in w:
            if t.multi:
                t.ws[tok.key] = tok
                t.r = {}
            else:
                t.w, t.r = tok, {}
        for t in r:
            if t.w is not tok and t not in w:
                t.r[tok.key] = tok

    def op(self, en, fn, r=(), w=()):
        self._wait(en, self._deps(r, w))
        ins = fn(self.eng[en])
        self.cnt[en] += 1
        ins.then_inc(self.sem[en], 1)
        self._commit(Tok(en, self.sem[en], self.cnt[en]), r, w)
        self.ninst += 1

    def mm(self, fns, r=(), w=()):
        self._wait("pe", self._deps(r, w))
        ins = None
        for fn in fns:
            ins = fn(self.eng["pe"])
        self.cnt["pe"] += 1
        ins.then_inc(self.sem["pe"], 1)
        self._commit(Tok("pe", self.sem["pe"], self.cnt["pe"]), r, w)
        self.ninst += len(fns)

    def dma(self, q, fn, r=(), w=()):
        slots = self.slots[q]
        i = self.slot_i[q]
        self.slot_i[q] = (i + 1) % len(slots)
        sl = slots[i]
        key = f"{q}{i}"
        if sl[1] > 0:
            self._wait(q, [Tok(key, sl[0], sl[1])])
        self._wait(q, self._deps(r, w))
        ins = fn(self.eng[q])
        sl[1] += 16
        ins.then_inc(sl[0], 16)
        self._commit(Tok(key, sl[0], sl[1]), r, w)
        self.ninst += 1

    def all_toks(self):
        toks = []
        for q, slots in self.slots.items():
            for i, sl in enumerate(slots):
                if sl[1] > 0:
                    toks.append(Tok(f"{q}{i}", sl[0], sl[1]))
        for en in ("pe", "act", "dve", "pool"):
            if self.cnt[en] > 0:
                toks.append(Tok(en, self.sem[en], self.cnt[en]))
        return toks

    def barrier(self):
        toks = self.all_toks()
        for en in ("pe", "act", "dve", "pool", "sp"):
            self._wait(en, [t for t in toks if t.key != en])

    def finish(self):
        toks = []
        for q, slots in self.slots.items():
            for i, sl in enumerate(slots):
                if sl[1] > 0:
                    toks.append(Tok(f"{q}{i}", sl[0], sl[1]))
        for en in ("pe", "act", "dve", "pool"):
            if self.cnt[en] > 0:
                toks.append(Tok(en, self.sem[en], self.cnt[en]))
        self._wait("sp", toks)


class Ctx:
    pass


def build(dbg=()):
    nc = bass.Bass("TRN2", target_bir_lowering=False)
    es = ExitStack()
    S = Sched(nc, es)
    C = Ctx()
    C.nc, C.es, C.S, C.dbg, C.dbgout = nc, es, S, dbg, {}

    def din(name, shape, dt=F32):
        return nc.dram_tensor(name, list(shape), dt, kind="ExternalInput").ap()

    C.nalloc = 0

    def sb(name, shape, dt=F32, scope=None):
        C.nalloc += 1
        return (scope or C.scope).enter_context(nc.sbuf_tensor(f"s{C.nalloc}_{name}", list(shape), dt))

    C.scope = es

    C.din, C.sb = din, sb
    I = Ctx()
    C.I = I
    I.x = din("x", [NTOK, D])
    I.cT = din("cT", [128, 8, NSEQ])
    I.pos = din("pos", [NSEQ, SEQ], I32)
    I.w_mod = din("w_mod", [D, 6 * D])
    I.b_modT = din("b_modT", [128, 48])
    I.b_mod_row = din("b_mod_row", [1, 6 * D])
    I.nmwT = din("nmwT", [128, 8])
    I.nfw_row = din("nfw_row", [1, D])
    I.nfin_row = din("nfin_row", [1, D])
    I.w_in = din("w_in", [D, IN_DIM])
    I.ident = din("ident", [128, 128])
    I.w_ba = din("w_ba", [512, D])
    I.w_bs = din("w_bs", [D, D])
    I.w_out = din("w_out", [D, D])
    I.conv_wT = din("conv_wT", [128, 16, 4])
    I.conv_bT = din("conv_bT", [128, 16])
    I.snwT = din("snwT", [128, 8])
    I.hrow = din("hrow", [1, 48])
    C.h_scr = nc.dram_tensor("h_scr", [NTOK, D], F32, kind="Internal").ap()
    C.h_scr_t = T("h_scr", multi=True)
    I.w_router = din("w_router", [D, NE])
    I.rbias_row = din("rbias_row", [1, NE])
    I.iota_row = din("iota_row", [1, NE])
    I.aid = din("aid", [128, NTOK // 128, 8], I32)
    I.bigtab = din("bigtab", [128, 4 * NE], I32)
    I.w_gate_s = din("w_gate_s", [D, 256])
    I.w_up_s = din("w_up_s", [D, 256])
    I.w_down_s = din("w_down_s", [256, D])
    C.u2_rows = nc.dram_tensor("u2_rows", [NTOK, D], BF16, kind="Internal").ap()
    C.u2rows_t = T("u2_rows", multi=True)
    si_h = nc.dram_tensor("slot_info", [128 * 4 * NE, 1], I32, kind="Internal")
    C.slot_info_flat = si_h.ap()
    C.slot_info = si_h.ap().rearrange("(p n) o -> p (n o)", p=128)
    C.slot_t = T("slot_info")
    C.slot_sc = T("slot_scatter", multi=True)
    C.sh_out = nc.dram_tensor("sh_out", [NTOK, D], F32, kind="Internal").ap()
    C.ye2 = nc.dram_tensor("ye2", [NTOK * 8, D], F32, kind="Internal").ap()
    C.ye2_t = T("ye2", multi=True)
    I.w_gate_e = din("w_gate_e", [NE, D, 256])
    I.w_up_e = din("w_up_e", [NE, D, 256])
    I.w_down_e = din("w_down_e", [NE, 256, D])
    C.sh_out_t = T("sh_out", multi=True)
    C.out = nc.dram_tensor("out", [NTOK, D], F32, kind="ExternalOutput").ap()

    K = Ctx()
    C.K = K
    K.t = T("consts")
    K.ident_f = sb("ident_f", [128, 128], F32)
    K.ident_b = sb("ident_b", [128, 128], BF16)
    K.ones_f = sb("ones_f", [128, 128], F32)
    S.dma("sp", lambda e: e.dma_start(out=K.ident_f[:], in_=I.ident), w=[K.t])
    S.op("dve", lambda e: e.tensor_copy(out=K.ident_b[:], in_=K.ident_f[:]), r=[K.t], w=[K.t])
    S.op("dve", lambda e: e.memset(K.ones_f[:], 1.0), w=[K.t])
    K.ones_b = sb("ones_b", [128, 128], BF16)
    S.op("dve", lambda e: e.memset(K.ones_b[:], 1.0), w=[K.t])
    I.cmat = din("cmat", [128, 6, 128])
    I.ropec = din("ropec", [128, 2])
    cm_f = sb("cmat_f", [128, 6, 128], F32)
    K.cm_b = sb("cmat_b", [128, 6, 128], BF16)
    K.ropec = sb("ropec", [128, 2])
    S.dma("sp", lambda e: e.dma_start(out=cm_f[:], in_=I.cmat), w=[K.t])
    S.dma("sp", lambda e: e.dma_start(out=K.ropec[:], in_=I.ropec), w=[K.t])
    S.op("dve", lambda e: e.tensor_copy(out=K.cm_b[:], in_=cm_f[:]), r=[K.t], w=[K.t])
    K.cm_f = cm_f
    K.p32, K.mcur, K.mprev = K.cm_b[:, 0, :], K.cm_b[:, 1, :], K.cm_b[:, 2, :]

    C.ps = [es.enter_context(nc.psum_tensor(f"ps{i}", [128, 512], F32)) for i in range(8)]
    C.pst = [T(f"ps{i}") for i in range(8)]

    M = Ctx()
    C.M = M
    M.t = T("mod")
    M.modT = sb("modT", [128, 48, NSEQ])
    M.scale1T = sb("scale1T", [128, 8, NSEQ])
    with ExitStack() as loc:
        C.scope = loc
        phase_mod(C, None)
        S.barrier()
    for s in range(NSEQ):
        with ExitStack() as sq:
            C.scope = sq
            C.uT = sb("uT", [128, 8, SEQ], BF16)
            C.uT_t = [T(f"uT{g}") for g in range(4)]
            C.attnT = sb("attnT", [128, 4, SEQ], BF16)
            C.attnT_t = T("attnT")
            M.bc = {nm: sb(f"bc_{nm}", [128, D]) for nm in ("g1",)}
            with ExitStack() as loc:
                C.scope = loc
                phase_mod(C, s, which=("g1",))
                S.barrier()
            with ExitStack() as loc:
                C.scope = loc
                phase_norm1(C, s)
                S.barrier()
            if "uT" in dbg:
                dump(C, f"uT{s}", C.uT[:].rearrange("p k t -> p (k t)"), [128, 8 * SEQ], BF16, C.uT_t)
            with ExitStack() as loc:
                C.scope = loc
                phase_rope_tables(C, s)
                if s == 0:
                    zt = sb("zero_tile", [128, 512])
                    zt_t = T("zero_tile")
                    S.op("dve", lambda e: e.memset(zt[:], 0.0), w=[zt_t])
                    for zi in range(NTOK * 8 // 128):
                        for zh in range(2):
                            S.dma("sp", lambda e, zi=zi, zh=zh: e.dma_start(
                                out=C.ye2[zi * 128:(zi + 1) * 128, zh * 512:(zh + 1) * 512], in_=zt[:]), r=[zt_t], w=[C.ye2_t])
                phase_attn(C, s)
                if "attn" in dbg:
                    dump(C, f"attnT{s}", C.attnT[:].rearrange("p h t -> p (h t)"), [128, 4 * SEQ], BF16, [C.attnT_t])
                    dump(C, f"qk{s}", C.A.qk[1][1][:], [128, SEQ], BF16, [C.A.qk_t[1][1]])
                S.barrier()
            with ExitStack() as loc:
                C.scope = loc
                phase_ssd(C, s)
                S.barrier()
            S.barrier()
    C.scope = es
    R = Ctx()
    C.R = R
    R.reg_slot = nc.gpsimd.to_reg(128 * 4 * NE - 1)
    R.reg_tok = nc.gpsimd.to_reg(NTOK - 1)
    R.reg_aid = nc.gpsimd.to_reg(NTOK * 8 - 1)
    R.wsel = sb("wsel", [128, NTOK // 128, 8])
    R.wsel_t = T("wsel", multi=True)
    with ExitStack() as loc:
        C.scope = loc
        phase_route(C)
        S.barrier()
    C.scope = es
    with ExitStack() as loc:
        C.scope = loc
        phase_experts(C)
        S.barrier()
    with ExitStack() as loc:
        C.scope = loc
        phase_final(C)
        S.barrier()
    C.scope = es
    if "route" in dbg:
        dump(C, "wsel", R.wsel[:].rearrange("p g k -> p (g k)"), [128, NTOK // 128 * 8], F32, [R.wsel_t])
        dump(C, "slot_info", C.slot_info, [128, 4 * NE], I32, [C.slot_t, C.slot_sc])
        dump(C, "sh_out", C.sh_out, [NTOK, D], F32, [C.sh_out_t])
        dump(C, "u2_rows", C.u2_rows, [NTOK, D], BF16, [C.u2rows_t])
    if "h" in dbg:
        d_ = nc.dram_tensor("dbg_h", [NTOK, D], F32, kind="ExternalOutput").ap()
        S.dma("sp", lambda e: e.dma_start(out=d_, in_=C.h_scr), r=[C.h_scr_t])
        C.dbgout["h"] = "dbg_h"
    S.finish()
    return nc, C


def dump(C, name, ap, shape, dt, tiles):
    d = C.nc.dram_tensor("dbg_" + name, list(shape), dt, kind="ExternalOutput").ap()
    C.S.dma("sp", lambda e: e.dma_start(out=d, in_=ap), r=tiles)
    C.dbgout[name] = "dbg_" + name


def phase_mod(C, seq, which=("g1", "sh2", "sc2")):
    nc, S, sb, I, K, M = C.nc, C.S, C.sb, C.I, C.K, C.M
    condT = sb("condT", [128, 8, NSEQ])
    cond_bc = sb("cond_bc", [128, 8, NSEQ, 128], BF16)
    condb = sb("condb", [128, 8, NSEQ], BF16)
    bmod_row = sb("bmod_row", [1, 6 * D], BF16)
    tl = T("modload")
    S.dma("sp", lambda e: e.dma_start(out=condT[:], in_=I.cT), w=[tl])
    S.dma("pool", lambda e: e.dma_start(out=bmod_row[:], in_=I.b_mod_row), w=[tl])
    S.op("act", lambda e: e.activation(out=condT[:], in_=condT[:], func=AF.Silu), r=[tl], w=[tl])
    S.op("dve", lambda e: e.tensor_copy(out=cond_bc[:], in_=condT[:].unsqueeze(3).to_broadcast([128, 8, NSEQ, 128])),
         r=[tl], w=[tl])
    S.op("dve", lambda e: e.tensor_copy(out=condb[:], in_=condT[:]), r=[tl], w=[tl])
    wblk = [sb(f"wmod_blk{i}", [128, 8, 512], BF16) for i in range(2)]
    wblk_t = [T(f"wmod_blk{i}") for i in range(2)]
    psm, psm_t = C.ps[0], C.pst[0]
    if seq is None:
        b_modT = sb("b_modT", [128, 48])
        nmwT = sb("nmwT", [128, 8])
        S.dma("sp", lambda e: e.dma_start(out=b_modT[:], in_=I.b_modT), w=[tl])
        S.dma("sp", lambda e: e.dma_start(out=nmwT[:], in_=I.nmwT), w=[tl])
        blocks = list(range(12))
        names = {}
        bs = []
    elif seq == "g2":
        blocks = [10, 11]
        names = {10: "g2", 11: "g2"}
        bs = list(range(NSEQ))
    else:
        nfw_bc = sb("nfw_bc", [128, D])
        S.dma("sp", lambda e: e.dma_start(out=nfw_bc[:], in_=I.nfw_row.partition_broadcast(128)), w=[M.t])
        names = {jb: nm for jb, nm in {4: "g1", 5: "g1", 6: "sh2", 7: "sh2", 8: "sc2", 9: "sc2"}.items() if nm in which}
        blocks = sorted(names)
        bs = [seq]
    for ib, jb in enumerate(blocks):
        wb, wt = wblk[ib % 2], wblk_t[ib % 2]
        S.dma("pool", lambda e: e.dma_start(out=wb[:], in_=I.w_mod[:, jb * 512:(jb + 1) * 512]
                                          .rearrange("(k p) n -> p k n", p=128)), w=[wt])
        if seq is None:
            for j in range(4):
                jc = jb * 4 + j
                S.mm([lambda e, k=k: e.matmul(psm[:, jc * 2:jc * 2 + 2], lhsT=wb[:, k, j * 128:(j + 1) * 128],
                                               rhs=condb[:, k, :], start=(k == 0), stop=(k == 7)) for k in range(8)],
                     r=[wt, tl], w=[psm_t])
        if jb in names:
            half = jb % 2
            for b in bs:
                pb, pbt = C.ps[1 + b], C.pst[1 + b]
                fns = [lambda e, k=k: e.matmul(pb[:, :], lhsT=cond_bc[:, k, b, :], rhs=wb[:, k, :],
                                               start=(k == 0), stop=False) for k in range(8)]
                fns.append(lambda e: e.matmul(pb[:, :], lhsT=K.ones_b[0:1, :], rhs=bmod_row[0:1, jb * 512:(jb + 1) * 512],
                                              start=False, stop=True))
                S.mm(fns, r=[wt, tl, K.t], w=[pbt])
                dst = M.g2_bc[b] if names[jb] == "g2" else M.bc[names[jb]]
                S.op("act", lambda e: e.copy(out=dst[:, half * 512:(half + 1) * 512], in_=pb[:, :]), r=[pbt], w=[M.t])
    if seq is None:
        S.op("dve", lambda e: e.tensor_tensor(out=M.modT[:], in0=psm[:, 0:96].rearrange("p (j b) -> p j b", b=NSEQ),
                                              in1=b_modT[:].unsqueeze(2).to_broadcast([128, 48, NSEQ]), op=ALU.add),
             r=[psm_t, tl], w=[M.t])
        S.op("dve", lambda e: e.scalar_tensor_tensor(out=M.scale1T[:], in0=M.modT[:, 8:16, :], scalar=1.0,
                                                     in1=nmwT[:].unsqueeze(2).to_broadcast([128, 8, NSEQ]),
                                                     op0=ALU.add, op1=ALU.mult), r=[M.t, tl], w=[M.t])
    elif seq != "g2" and "sc2" in which:
        t = M.bc["sc2"]
        S.op("dve", lambda e: e.scalar_tensor_tensor(out=t[:], in0=t[:], scalar=1.0, in1=nfw_bc[:],
                                                     op0=ALU.add, op1=ALU.mult), r=[M.t], w=[M.t])


def phase_norm1(C, s):
    nc, S, sb, I, K, M = C.nc, C.S, C.sb, C.I, C.K, C.M
    if True:
        C.xin = [sb(f"xin{i}", [128, D]) for i in range(2)]
        C.xin_t = [T(f"xin{i}") for i in range(2)]
        C.xn = [sb(f"xn{i}", [128, D], BF16) for i in range(2)]
        C.xn_t = [T(f"xn{i}") for i in range(2)]
        C.sq = sb("sq_junk", [128, D])
        C.sq_t = T("sq")
        C.ss = [sb(f"ss{i}", [128, 2]) for i in range(2)]
        C.n1tmp = [sb(f"n1tmp{i}", [128, 8, 128]) for i in range(2)]
        C.n1tmp_t = [T(f"n1tmp{i}") for i in range(2)]
    for i in range(SEQ // 128):
        xt, xtt = C.xin[i % 2], C.xin_t[i % 2]
        xn, xnt = C.xn[i % 2], C.xn_t[i % 2]
        ss = C.ss[i % 2]
        r0 = s * SEQ + i * 128
        S.dma("sp", lambda e: e.dma_start(out=xt[:], in_=I.x[r0:r0 + 128, :]), w=[xtt])
        S.op("act", lambda e: e.activation(out=C.sq[:], in_=xt[:], func=AF.Square, accum_out=ss[:, 0:1]),
             r=[xtt], w=[C.sq_t, xnt])
        S.op("dve", lambda e: e.tensor_scalar(out=ss[:, 1:2], in0=ss[:, 0:1], scalar1=1.0 / D, scalar2=EPS,
                                              op0=ALU.mult, op1=ALU.add), r=[xnt], w=[xnt])
        S.op("act", lambda e: e.sqrt(out=ss[:, 1:2], in_=ss[:, 1:2]), r=[xnt], w=[xnt])
        S.op("dve", lambda e: e.reciprocal(out=ss[:, 1:2], in_=ss[:, 1:2]), r=[xnt], w=[xnt])
        S.op("dve", lambda e: e.tensor_scalar(out=xn[:], in0=xt[:], scalar1=ss[:, 1:2], scalar2=None,
                                              op0=ALU.mult), r=[xtt, xnt], w=[xnt])
        pb, pbt = C.ps[i % 2], C.pst[i % 2]
        pbb = pb[:, :].bitcast(BF16)
        S.mm([lambda e, k=k: e.transpose(out=pbb[:, k * 128:(k + 1) * 128], in_=xn[:, k * 128:(k + 1) * 128],
                                          identity=K.ident_b[:]) for k in range(8)], r=[xnt, K.t], w=[pbt])
        ut = C.uT_t[i // 4]
        tmp32, tmp32_t = C.n1tmp[i % 2], C.n1tmp_t[i % 2]
        S.op("dve", lambda e: e.tensor_tensor(out=tmp32[:], in0=pbb.rearrange("p (k t) -> p k t", k=8),
                                              in1=M.scale1T[:, :, s].unsqueeze(2).to_broadcast([128, 8, 128]), op=ALU.mult),
             r=[pbt, M.t], w=[tmp32_t])
        S.op("dve", lambda e: e.tensor_tensor(out=C.uT[:, :, i * 128:(i + 1) * 128], in0=tmp32[:],
                                              in1=M.modT[:, 0:8, s].unsqueeze(2).to_broadcast([128, 8, 128]), op=ALU.add),
             r=[tmp32_t, M.t], w=[ut])


def qsel(d, r, n):
    st = d * 128 * n + r
    return slice(st, st + d * 127 + 1, d)


def phase_rope_tables(C, s):
    nc, S, sb, I, K = C.nc, C.S, C.sb, C.I, C.K
    C.cosT = sb("cosT", [32, SEQ])
    C.sinT = sb("sinT", [32, SEQ])
    C.rope_t = T("rope")
    outer_scope = C.scope
    rloc = ExitStack()
    C.scope = rloc
    C.posi = sb("posi", [32, SEQ], I32)
    C.ang = sb("ang", [32, SEQ])
    S.dma("sp", lambda e: e.dma_start(out=C.posi[:], in_=I.pos[s:s + 1, :].partition_broadcast(32)), w=[C.rope_t])
    S.op("dve", lambda e: e.tensor_copy(out=C.ang[:], in_=C.posi[:]), r=[C.rope_t], w=[C.rope_t])
    S.op("dve", lambda e: e.tensor_scalar(out=C.ang[:], in0=C.ang[:], scalar1=K.ropec[0:32, 0:1], scalar2=None,
                                          op0=ALU.mult), r=[C.rope_t, K.t], w=[C.rope_t])
    PI = float(np.pi)
    TWO_PI = 2.0 * PI
    PI_LO = 3.1415925
    yy = C.sb("rope_y", [32, SEQ])
    for dst, sh in ((C.sinT, PI), (C.cosT, PI + PI / 2)):
        rt = [C.rope_t]
        S.op("dve", lambda e: e.tensor_scalar(out=yy[:], in0=C.ang[:], scalar1=sh, scalar2=None, op0=ALU.add), r=rt, w=rt)
        S.op("dve", lambda e: e.tensor_scalar(out=C.posi[:], in0=yy[:], scalar1=1.0 / TWO_PI, scalar2=None,
                                              op0=ALU.mult), r=rt, w=rt)
        S.op("dve", lambda e: e.tensor_copy(out=dst[:], in_=C.posi[:]), r=rt, w=rt)
        S.op("dve", lambda e: e.scalar_tensor_tensor(out=yy[:], in0=dst[:], scalar=-TWO_PI, in1=yy[:],
                                                     op0=ALU.mult, op1=ALU.add), r=rt, w=rt)
        S.op("dve", lambda e: e.tensor_scalar(out=dst[:], in0=yy[:], scalar1=0.0, scalar2=None, op0=ALU.is_lt), r=rt, w=rt)
        S.op("dve", lambda e: e.scalar_tensor_tensor(out=yy[:], in0=dst[:], scalar=TWO_PI, in1=yy[:],
                                                     op0=ALU.mult, op1=ALU.add), r=rt, w=rt)
        S.op("dve", lambda e: e.tensor_scalar(out=yy[:], in0=yy[:], scalar1=-PI, scalar2=None, op0=ALU.add), r=rt, w=rt)
        S.op("dve", lambda e: e.tensor_scalar(out=yy[:], in0=yy[:], scalar1=PI_LO, scalar2=-PI_LO,
                                              op0=ALU.min, op1=ALU.max), r=rt, w=rt)
        S.op("act", lambda e: e.activation(out=dst[:], in_=yy[:], func=AF.Sin), r=rt, w=rt)
    S.op("dve", lambda e: e.tensor_scalar(out=C.sinT[:], in0=C.sinT[:], scalar1=K.ropec[0:32, 1:2], scalar2=None,
                                          op0=ALU.mult), r=[C.rope_t, K.t], w=[C.rope_t])
    S.barrier()
    rloc.close()
    C.scope = outer_scope


def phase_attn(C, s):
    nc, S, sb, I, K = C.nc, C.S, C.sb, C.I, C.K
    A = Ctx()
    C.A = A
    A.w = [[sb(f"aw{b}_{i}", [128, 8, 128], BF16) for i in range(9)] for b in range(2)]
    A.w_t = [[T(f"aw{b}_{i}") for i in range(9)] for b in range(2)]
    A.qk = [[sb(f"qk{b}_{i}", [128, SEQ], BF16) for i in range(6)] for b in range(2)]
    A.qk_t = [[T(f"qk{b}_{i}") for i in range(6)] for b in range(2)]
    A.v = [[sb(f"v{b}_{i}", [128, 16, 128], BF16) for i in range(3)] for b in range(2)]
    A.v_t = [[T(f"v{b}_{i}") for i in range(3)] for b in range(2)]
    A.acc = sb("attacc", [128, 2, SEQ])
    A.acc_t = T("attacc")
    A.pT = [sb(f"pT{i}", [128, 512], BF16) for i in range(2)]
    A.pT_t = [T(f"pT{i}") for i in range(2)]
    A.rt = [sb(f"ropetmp{i}", [32, 512]) for i in range(2)]
    A.rt_t = T("ropetmp")
    A.ni = 0
    A.nb = 0
    scale = 1.0 / float(np.sqrt(128.0))

    def gen_inproj(hs, bs):
        W, W_t, QK, QK_t, V, V_t = A.w[bs], A.w_t[bs], A.qk[bs], A.qk_t[bs], A.v[bs], A.v_t[bs]
        for i in range(9):
            base = (Q0, K0, V0)[i // 3] + ((i % 3) * 4 + hs) * 128
            S.dma("pool", lambda e, i=i, base=base: e.dma_start(
                out=W[i][:], in_=I.w_in[:, base:base + 128].rearrange("(k p) n -> p k n", p=128)), w=[W_t[i]])
        for i in range(6):
            for tg in range(4):
                A.ni += 1
                pq, pqt = C.ps[A.ni % 2], C.pst[A.ni % 2]
                psw, pswt = C.ps[2 + A.ni % 2], C.pst[2 + A.ni % 2]
                tsl = slice(tg * 512, (tg + 1) * 512)
                S.mm([lambda e, k=k: e.matmul(pq[:, :], lhsT=W[i][:, k, :], rhs=C.uT[:, k, tsl],
                                               start=(k == 0), stop=(k == 7)) for k in range(8)],
                     r=[W_t[i], C.uT_t[tg]], w=[pqt])
                S.op("act", lambda e: e.copy(out=QK[i][:, tsl], in_=pq[:, :]), r=[pqt], w=[QK_t[i]])
                S.mm([lambda e: e.matmul(psw[0:32, :], lhsT=K.p32[0:32, 0:32], rhs=QK[i][0:32, tsl],
                                         start=True, stop=True)], r=[QK_t[i], K.t], w=[pswt])
                t0, t1 = A.rt
                S.op("dve", lambda e: e.tensor_tensor(out=t0[:], in0=psw[0:32, :], in1=C.sinT[:, tsl], op=ALU.mult),
                     r=[pswt, C.rope_t], w=[A.rt_t])
                S.op("dve", lambda e: e.tensor_tensor(out=t1[:], in0=pq[0:32, :], in1=C.cosT[:, tsl], op=ALU.mult),
                     r=[pqt, C.rope_t], w=[A.rt_t])
                S.op("dve", lambda e: e.tensor_tensor(out=QK[i][0:32, tsl], in0=t0[:], in1=t1[:], op=ALU.add),
                     r=[A.rt_t], w=[QK_t[i], A.rt_t])
                yield
        for g, d in enumerate((1, 4, 16)):
            nb = 16 // d
            for j0 in range(0, 16, 4):
                A.ni += 1
                pv, pvt = C.ps[A.ni % 2], C.pst[A.ni % 2]
                for jj in range(4):
                    j = j0 + jj
                    r_, n_ = j // nb, j % nb
                    sel = qsel(d, r_, n_)
                    S.mm([lambda e, k=k: e.matmul(pv[:, jj * 128:(jj + 1) * 128], lhsT=C.uT[:, k, sel],
                                                   rhs=W[6 + g][:, k, :], start=(k == 0), stop=(k == 7))
                          for k in range(8)], r=[W_t[6 + g]] + C.uT_t, w=[pvt])
                S.op("act", lambda e: e.copy(out=V[g][:, j0:j0 + 4, :].rearrange("p j e -> p (j e)"), in_=pv[:, :]),
                     r=[pvt], w=[V_t[g]])
                yield

    def gen_blocks(hs, bs):
        QK, QK_t, V, V_t = A.qk[bs], A.qk_t[bs], A.v[bs], A.v_t[bs]
        blocks = []
        for g, d in enumerate((1, 4, 16)):
            nb = 16 // d
            for r_ in range(d):
                for n_ in range(nb):
                    blocks.append((g, d, nb, r_, n_))
        for b0 in range(0, len(blocks), 2):
            pair = blocks[b0:b0 + 2]
            g = pair[0][0]
            assert all(p[0] == g for p in pair)
            qT, kT, qt_, kt_ = QK[g], QK[3 + g], QK_t[g], QK_t[3 + g]
            A.nb += 1
            ps_s, ps_st = C.ps[4 + A.nb % 2], C.pst[4 + A.nb % 2]
            ps_o, ps_ot = C.ps[6 + A.nb % 2], C.pst[6 + A.nb % 2]
            pT, pTt = A.pT[A.nb % 2], A.pT_t[A.nb % 2]
            fns = []
            metas = []
            for pi, (g_, d, nb, r_, n_) in enumerate(pair):
                qs = qsel(d, r_, n_)
                kbs = [kb for kb in (n_ - 1, n_) if kb >= 0]
                c0 = pi * 256 + 256 - 128 * len(kbs)
                for ci, kb in enumerate(kbs):
                    cs = slice(c0 + ci * 128, c0 + (ci + 1) * 128)
                    ks = qsel(d, r_, kb)
                    msk = K.mcur if kb == n_ else K.mprev
                    fns.append(lambda e, cs=cs, ks=ks, qs=qs: e.matmul(ps_s[:, cs], lhsT=kT[:, ks], rhs=qT[:, qs],
                                                                       start=True, stop=False))
                    fns.append(lambda e, cs=cs, msk=msk: e.matmul(ps_s[:, cs], lhsT=K.ident_b[:], rhs=msk[:],
                                                                  start=False, stop=True))
                metas.append((d, nb, r_, n_, qs, kbs, c0))
            S.mm(fns, r=[qt_, kt_, K.t], w=[ps_st])
            lo0, hi0 = metas[0][6], 256
            lo1, hi1 = metas[1][6], 512
            rngs = [(lo0, hi1)] if lo1 == 256 else [(lo0, hi0), (lo1, hi1)]
            for lo, hi in rngs:
                S.op("act", lambda e, lo=lo, hi=hi: e.activation(out=pT[:, lo:hi], in_=ps_s[:, lo:hi], func=AF.Exp, scale=scale),
                     r=[ps_st], w=[pTt])
            fns = []
            for pi, (d, nb, r_, n_, qs, kbs, c0) in enumerate(metas):
                ob = pi * 256
                for ci, kb in enumerate(kbs):
                    cs = slice(c0 + ci * 128, c0 + (ci + 1) * 128)
                    vj = r_ * nb + kb
                    fns.append(lambda e, cs=cs, vj=vj, ci=ci, ob=ob, nk=len(kbs): e.matmul(
                        ps_o[:, ob:ob + 128], lhsT=V[g][:, vj, :], rhs=pT[:, cs], start=(ci == 0), stop=(ci == nk - 1)))
                for ci, kb in enumerate(kbs):
                    cs = slice(c0 + ci * 128, c0 + (ci + 1) * 128)
                    fns.append(lambda e, cs=cs, ci=ci, ob=ob, nk=len(kbs): e.matmul(
                        ps_o[:, ob + 128:ob + 256], lhsT=K.ones_b[:], rhs=pT[:, cs], start=(ci == 0), stop=(ci == nk - 1)))
            S.mm(fns, r=[pTt, V_t[g], K.t], w=[ps_ot])
            for pi, (d, nb, r_, n_, qs, kbs, c0) in enumerate(metas):
                src = ps_o[:, pi * 256:(pi + 1) * 256].rearrange("p (a q) -> p a q", a=2)
                if g == 0:
                    S.op("act", lambda e, src=src, qs=qs: e.copy(out=A.acc[:, :, qs], in_=src), r=[ps_ot], w=[A.acc_t])
                else:
                    S.op("dve", lambda e, src=src, qs=qs: e.tensor_tensor(out=A.acc[:, :, qs], in0=src, in1=A.acc[:, :, qs], op=ALU.add),
                         r=[ps_ot, A.acc_t], w=[A.acc_t])
            yield
        S.op("dve", lambda e: e.reciprocal(out=A.acc[:, 1, :], in_=A.acc[:, 1, :]), r=[A.acc_t], w=[A.acc_t])
        S.op("dve", lambda e: e.tensor_tensor(out=C.attnT[:, hs, :], in0=A.acc[:, 0, :], in1=A.acc[:, 1, :], op=ALU.mult),
             r=[A.acc_t], w=[C.attnT_t])

    for _ in gen_inproj(0, 0):
        pass
    for hs in range(4):
        gb = gen_blocks(hs, hs % 2)
        gi = gen_inproj(hs + 1, (hs + 1) % 2) if hs + 1 < 4 else iter(())
        done_b = done_i = False
        step = 0
        while not (done_b and done_i):
            step += 1
            if not done_b:
                try:
                    next(gb)
                except StopIteration:
                    done_b = True
            for _rep in range(2 if step % 2 == 0 else 1):
                if not done_i:
                    try:
                        next(gi)
                    except StopIteration:
                        done_i = True


def phase_ssd(C, s):
    nc, S, sb, I, K, M = C.nc, C.S, C.sb, C.I, C.K, C.M
    ps, pst = C.ps, C.pst
    tri_f, sl_f = K.cm_f[:, 3, :], K.cm_f[:, 4, :]
    tw = T("ssd_w", multi=True)
    wdt = sb("wdt", [128, 8, 16], BF16)
    S.dma("pool", lambda e: e.dma_start(out=wdt[:], in_=I.w_in[:, DT0:DT0 + 16].rearrange("(k p) n -> p k n", p=128)), w=[tw])
    tc = T("ssd_c")
    convw = sb("convw", [128, 16, 4])
    convb = sb("convb", [128, 16])
    snwT = sb("snwT", [128, 8])
    hb16 = sb("hb16", [128, 3, 16])
    D_bc = sb("D_bc", [128, 16, 64])
    S.dma("sp", lambda e: e.dma_start(out=convw[:], in_=I.conv_wT), w=[tc])
    S.dma("sp", lambda e: e.dma_start(out=convb[:], in_=I.conv_bT), w=[tc])
    S.dma("sp", lambda e: e.dma_start(out=snwT[:], in_=I.snwT), w=[tc])
    S.dma("sp", lambda e: e.dma_start(out=hb16[:].rearrange("p a h -> p (a h)"), in_=I.hrow.partition_broadcast(128)), w=[tc])
    S.op("act", lambda e: e.activation(out=hb16[:, 1, :], in_=hb16[:, 1, :], func=AF.Exp), r=[tc], w=[tc])
    S.op("dve", lambda e: e.tensor_scalar(out=hb16[:, 1, :], in0=hb16[:, 1, :], scalar1=-1.0, scalar2=None, op0=ALU.mult),
         r=[tc], w=[tc])
    S.op("dve", lambda e: e.tensor_copy(out=D_bc[:], in_=hb16[:, 2, :].unsqueeze(2).to_broadcast([128, 16, 64])), r=[tc], w=[tc])
    nws = [0]
    halo = sb("halo", [128, 16, 3], BF16)
    halo_t = T("halo")
    S.op("dve", lambda e: e.memset(halo[:], 0.0), w=[halo_t])
    cdiag = sb("cdiag", [128, 16, 4, 128], BF16)
    cdiag_t = T("cdiag", multi=True)
    for c in range(16):
        for j in range(4):
            S.op("dve", lambda e, c=c, j=j: e.tensor_scalar(out=cdiag[:, c, j, :], in0=K.ident_f[:], scalar1=convw[:, c, j:j + 1],
                                                            scalar2=None, op0=ALU.mult), r=[tc, K.t], w=[cdiag_t])
    Hs = sb("Hs", [128, 16, 64])
    Hb = sb("Hb", [128, 16, 64], BF16)
    H_t = T("H")
    xbc = sb("xbc", [128, 16, 512], BF16)
    xbc_t = T("xbc")
    yT = sb("yT", [128, 8, 512], BF16)
    yT_t = T("yT")
    nb = [0]
    ssd_scope = C.scope
    W = {}

    def load_w(col0):
        i = nws[0] % 2
        nws[0] += 1
        wst, wst_t = W["wst"], W["wst_t"]
        S.dma("pool", lambda e: e.dma_start(out=wst[i][:], in_=I.w_in[:, col0:col0 + 512].rearrange("(k p) n -> p k n", p=128)),
              w=[wst_t[i]])
        return wst[i], wst_t[i]

    for tg in range(4):
        tsl = slice(tg * 512, (tg + 1) * 512)
        ut = C.uT_t[tg]
        loc = ExitStack()
        C.scope = loc
        W["wst"] = [sb(f"wst{i}", [128, 8, 512], BF16) for i in range(2)]
        W["wst_t"] = [T(f"wst{i}") for i in range(2)]
        pre = [sb(f"pre{i}", [128, 516], BF16) for i in range(2)]
        pre_t = [T(f"pre{i}") for i in range(2)]
        for c in range(16):
            if c % 4 == 0:
                wx, wxt = load_w(X0 + c * 128)
            nb[0] += 1
            pq, pqt = ps[nb[0] % 2], pst[nb[0] % 2]
            pc, pct = ps[2 + nb[0] % 2], pst[2 + nb[0] % 2]
            pr, prt = pre[nb[0] % 2], pre_t[nb[0] % 2]
            S.mm([lambda e, k=k: e.matmul(pq[:, :], lhsT=wx[:, k, (c % 4) * 128:(c % 4 + 1) * 128], rhs=C.uT[:, k, tsl],
                                           start=(k == 0), stop=(k == 7)) for k in range(8)], r=[wxt, ut], w=[pqt])
            S.op("act", lambda e: e.copy(out=pr[:, 3:515], in_=pq[:, :]), r=[pqt], w=[prt])
            S.op("act", lambda e: e.copy(out=pr[:, 0:3], in_=halo[:, c, :]), r=[halo_t], w=[prt])
            S.mm([lambda e, j=j: e.matmul(pc[:, :], lhsT=cdiag[:, c, j, :], rhs=pr[:, j:j + 512], start=(j == 0), stop=(j == 3))
                  for j in range(4)], r=[prt, cdiag_t], w=[pct])
            S.op("act", lambda e: e.copy(out=halo[:, c, :], in_=pr[:, 512:515]), r=[prt], w=[halo_t])
            S.op("act", lambda e: e.activation(out=xbc[:, c, :], in_=pc[:, :], func=AF.Silu, bias=convb[:, c:c + 1]),
                 r=[pct, tc], w=[xbc_t])
        S.barrier()
        loc.close()
        loc = ExitStack()
        C.scope = loc
        wz = sb("wz", [128, 8, D], BF16)
        twz = T("wz")
        S.dma("pool", lambda e: e.dma_start(out=wz[:], in_=I.w_in[:, Z0:Z0 + D].rearrange("(k p) n -> p k n", p=128)), w=[twz])
        dts = sb("dts", [128, 4, 4, 16])
        dts_t = T("dts")
        sm2 = [sb(f"ssd_small{i}", [128, 6, 16]) for i in range(2)]
        sm2_t = [T(f"ssd_small{i}") for i in range(2)]
        Lf = sb("Lf", [128, 16, 128])
        Lf_t = T("Lf")
        cbTm = sb("cbTm", [128, 4, 128])
        cbTm_t = T("cbTm")
        dec = [sb(f"dec{i}", [128, 4, 128], BF16) for i in range(2)]
        dec_t = [T(f"dec{i}") for i in range(2)]
        MT2 = [sb(f"MT{i}", [128, 16, 128], BF16) for i in range(2)]
        MT2_t = [T(f"MT{i}") for i in range(2)]
        xdt2 = [sb(f"xdt{i}", [128, 16, 64], BF16) for i in range(2)]
        xsD2 = [sb(f"xsD{i}", [128, 16, 64]) for i in range(2)]
        xdd2 = [sb(f"xdd{i}", [128, 16, 64], BF16) for i in range(2)]
        xd2_t = [T(f"xd{i}") for i in range(2)]
        Bt2 = [sb(f"Bt{i}", [128, 4, 128], BF16) for i in range(2)]
        Bt2_t = [T(f"Bt{i}") for i in range(2)]
        t1 = sb("t1", [128, 16, 64])
        t1_t = T("t1")
        yb = sb("yb", [128, D])
        yb_t = T("yb")
        sz2 = [sb(f"sz{i}", [128, D]) for i in range(2)]
        sz2_t = [T(f"sz{i}") for i in range(2)]
        ysq = sb("ysq", [128, 2, 4])
        yn = sb("yn", [128, D], BF16)
        yn_t = T("yn")
        pd, pdt = ps[2], pst[2]
        for ti in range(4):
            tok = slice(tg * 512 + ti * 128, tg * 512 + (ti + 1) * 128)
            S.mm([lambda e, k=k: e.matmul(pd[:, ti * 16:(ti + 1) * 16], lhsT=C.uT[:, k, tok], rhs=wdt[:, k, :],
                                           start=(k == 0), stop=(k == 7)) for k in range(8)], r=[tw, ut], w=[pdt])
        dt4 = [dts_t]
        S.op("dve", lambda e: e.tensor_tensor(out=dts[:, :, 0, :], in0=pd[:, 0:64].rearrange("p (t h) -> p t h", h=16),
                                              in1=hb16[:, 0, :].unsqueeze(1).to_broadcast([128, 4, 16]), op=ALU.add),
             r=[pdt, tc], w=dt4)
        S.op("act", lambda e: e.activation(out=dts[:, :, 1, :], in_=dts[:, :, 0, :], func=AF.Abs), r=dt4, w=dt4)
        S.op("act", lambda e: e.activation(out=dts[:, :, 1, :], in_=dts[:, :, 1, :], func=AF.Exp, scale=-1.0), r=dt4, w=dt4)
        S.op("dve", lambda e: e.tensor_scalar(out=dts[:, :, 1, :], in0=dts[:, :, 1, :], scalar1=1.0, scalar2=None, op0=ALU.add),
             r=dt4, w=dt4)
        S.op("act", lambda e: e.activation(out=dts[:, :, 1, :], in_=dts[:, :, 1, :], func=AF.Ln), r=dt4, w=dt4)
        S.op("dve", lambda e: e.scalar_tensor_tensor(out=dts[:, :, 2, :], in0=dts[:, :, 0, :], scalar=0.0, in1=dts[:, :, 1, :],
                                                     op0=ALU.max, op1=ALU.add), r=dt4, w=dt4)
        S.op("dve", lambda e: e.tensor_tensor(out=dts[:, :, 3, :], in0=dts[:, :, 2, :],
                                              in1=hb16[:, 1, :].unsqueeze(1).to_broadcast([128, 4, 16]), op=ALU.mult),
             r=dt4 + [tc], w=dt4)
        def chunk_front(ci):
                csl = slice(ci * 128, (ci + 1) * 128)
                tok = slice(tg * 512 + ci * 128, tg * 512 + (ci + 1) * 128)
                first = (tg == 0 and ci == 0)
                a_ = dts[:, ci, 3, :]
                dt_ = dts[:, ci, 2, :]
                par = ci % 2
                sm, sm_t = sm2[par], sm2_t[par]
                MT, MT_t = MT2[par], MT2_t[par]
                xdt, xsD, xdd, xd_t = xdt2[par], xsD2[par], xdd2[par], xd2_t[par]
                Bt, Bt_t = Bt2[par], Bt2_t[par]
                sz, sz_t = sz2[par], sz2_t[par]
                smt = [sm_t]
                xdw = [xd_t]
                p0, p0t = ps[0], pst[0]
                S.mm([lambda e: e.matmul(p0[:, 0:16], lhsT=tri_f, rhs=a_, start=True, stop=True),
                      lambda e: e.matmul(p0[:, 16:32], lhsT=K.ones_f[:], rhs=a_, start=True, stop=True)],
                     r=[K.t, dts_t], w=[p0t])
                smt = [sm_t]
                S.op("act", lambda e: e.copy(out=sm[:, 0, :], in_=p0[:, 0:16]), r=[p0t], w=smt)
                S.op("dve", lambda e: e.tensor_tensor(out=sm[:, 1, :], in0=p0[:, 16:32], in1=sm[:, 0, :], op=ALU.subtract),
                     r=[p0t] + smt, w=smt)
                S.op("act", lambda e: e.activation(out=sm[:, 2, :], in_=sm[:, 0, :], func=AF.Exp), r=smt, w=smt)
                S.op("act", lambda e: e.activation(out=sm[:, 3, :], in_=sm[:, 1, :], func=AF.Exp), r=smt, w=smt)
                S.op("act", lambda e: e.activation(out=sm[:, 4, :], in_=p0[:, 16:32], func=AF.Exp), r=[p0t] + smt, w=smt)
                S.op("dve", lambda e: e.tensor_tensor(out=Lf[:], in0=sl_f.unsqueeze(1).to_broadcast([128, 16, 128]),
                                                      in1=a_.unsqueeze(2).to_broadcast([128, 16, 128]), op=ALU.mult),
                     r=[K.t, dts_t], w=[Lf_t])
                p1, p1t = ps[1], pst[1]
                S.mm([lambda e, g=g: e.matmul(p1[:, g * 128:(g + 1) * 128], lhsT=xbc[:, 8 + g, csl], rhs=xbc[:, 12 + g, csl],
                                               start=True, stop=True) for g in range(4)], r=[xbc_t], w=[p1t])
                S.op("dve", lambda e: e.tensor_tensor(out=cbTm[:], in0=p1[:, :].rearrange("p (g l) -> p g l", g=4),
                                                      in1=tri_f.unsqueeze(1).to_broadcast([128, 4, 128]), op=ALU.mult),
                     r=[p1t, K.t], w=[cbTm_t])
                p2, p2t = ps[2], pst[2]
                p3, p3t = ps[3], pst[3]
                p2b = p2[:, :].bitcast(BF16)
                p3b = p3[:, :].bitcast(BF16)
                S.mm([lambda e, k=k: e.transpose(out=p2b[:, k * 128:(k + 1) * 128], in_=xbc[:, k, csl], identity=K.ident_b[:])
                      for k in range(8)], r=[xbc_t, K.t], w=[p2t])
                S.mm([lambda e, g=g: e.transpose(out=p3b[:, g * 128:(g + 1) * 128], in_=xbc[:, 8 + g, csl], identity=K.ident_b[:])
                      for g in range(4)], r=[xbc_t, K.t], w=[p3t])
                xsT = p2b.rearrange("p (h e) -> p h e", h=16)
                xdw = [xd_t]
                S.op("dve", lambda e: e.tensor_tensor(out=xdt[:], in0=xsT, in1=dt_.unsqueeze(2).to_broadcast([128, 16, 64]),
                                                      op=ALU.mult), r=[p2t, dts_t], w=xdw)
                S.op("dve", lambda e: e.tensor_tensor(out=xsD[:], in0=xsT, in1=D_bc[:], op=ALU.mult), r=[p2t, tc], w=xdw)
                S.op("dve", lambda e: e.tensor_tensor(out=xdd[:], in0=xdt[:], in1=sm[:, 3, :].unsqueeze(2).to_broadcast([128, 16, 64]),
                                                      op=ALU.mult), r=xdw + smt, w=xdw)
                S.op("act", lambda e: e.copy(out=Bt[:].rearrange("p g n -> p (g n)"), in_=p3b[:, 0:512]), r=[p3t], w=[Bt_t])
                for g in range(4):
                    pdx, pdxt = ps[4 + g % 2], pst[4 + g % 2]
                    S.mm([lambda e, hl=hl: e.matmul(pdx[:, hl * 128:(hl + 1) * 128], lhsT=Lf[:, g * 4 + hl, :], rhs=tri_f,
                                                     start=True, stop=True) for hl in range(4)], r=[Lf_t, K.t], w=[pdxt])
                    dc, dct = dec[g % 2], dec_t[g % 2]
                    S.op("act", lambda e: e.activation(out=dc[:].rearrange("p h l -> p (h l)"), in_=pdx[:, :], func=AF.Exp),
                         r=[pdxt], w=[dct])
                    S.op("dve", lambda e: e.tensor_tensor(out=MT[:, g * 4:(g + 1) * 4, :], in0=dc[:],
                                                          in1=cbTm[:, g, :].unsqueeze(1).to_broadcast([128, 4, 128]), op=ALU.mult),
                         r=[dct, cbTm_t], w=[MT_t])

                pz = (ps[6], ps[7])
                pzt = [pst[6], pst[7]]
                for hf in range(2):
                    S.mm([lambda e, k=k, hf=hf: e.matmul(pz[hf][:, :], lhsT=C.uT[:, k, tok], rhs=wz[:, k, hf * 512:(hf + 1) * 512],
                                                          start=(k == 0), stop=(k == 7)) for k in range(8)], r=[twz, ut], w=[pzt[hf]])
                    S.op("act", lambda e, hf=hf: e.activation(out=sz[:, hf * 512:(hf + 1) * 512], in_=pz[hf][:, :], func=AF.Silu),
                         r=[pzt[hf]], w=[sz_t])

        def chunk_back(ci):
                csl = slice(ci * 128, (ci + 1) * 128)
                tok = slice(tg * 512 + ci * 128, tg * 512 + (ci + 1) * 128)
                first = (tg == 0 and ci == 0)
                a_ = dts[:, ci, 3, :]
                dt_ = dts[:, ci, 2, :]
                par = ci % 2
                sm, sm_t = sm2[par], sm2_t[par]
                MT, MT_t = MT2[par], MT2_t[par]
                xdt, xsD, xdd, xd_t = xdt2[par], xsD2[par], xdd2[par], xd2_t[par]
                Bt, Bt_t = Bt2[par], Bt2_t[par]
                sz, sz_t = sz2[par], sz2_t[par]
                smt = [sm_t]
                xdw = [xd_t]
                py = (ps[6], ps[7])
                pyt = [pst[6], pst[7]]
                S.mm([lambda e, h=h: e.matmul(py[h // 8][:, (h % 8) * 64:(h % 8 + 1) * 64], lhsT=MT[:, h, :], rhs=xdt[:, h, :],
                                               start=True, stop=True) for h in range(16)], r=[MT_t, xd_t], w=pyt)
                po = (ps[0], ps[1])
                pot = [pst[0], pst[1]]
                if not first:
                    S.mm([lambda e, g=g: e.matmul(po[g // 2][:, (g % 2) * 256:(g % 2 + 1) * 256], lhsT=xbc[:, 12 + g, csl],
                                                   rhs=Hb[:, g * 4:(g + 1) * 4, :].rearrange("p h e -> p (h e)"),
                                                   start=True, stop=True) for g in range(4)], r=[xbc_t, H_t], w=pot)
                    for hf in range(2):
                        S.op("dve", lambda e, hf=hf: e.tensor_tensor(
                            out=t1[:, hf * 8:(hf + 1) * 8, :], in0=po[hf][:, :].rearrange("p (h e) -> p h e", h=8),
                            in1=sm[:, 2, hf * 8:(hf + 1) * 8].unsqueeze(2).to_broadcast([128, 8, 64]), op=ALU.mult),
                            r=[pot[hf]] + smt, w=[t1_t])
                    S.op("dve", lambda e: e.tensor_tensor(out=t1[:], in0=t1[:], in1=xsD[:], op=ALU.add), r=[t1_t, xd_t], w=[t1_t])
                    tsrc = t1
                else:
                    tsrc = xsD
                for hf in range(2):
                    S.op("dve", lambda e, hf=hf: e.tensor_tensor(
                        out=yb[:, hf * 512:(hf + 1) * 512], in0=py[hf][:, :],
                        in1=tsrc[:, hf * 8:(hf + 1) * 8, :].rearrange("p h e -> p (h e)"), op=ALU.add),
                        r=[pyt[hf], t1_t, xd_t], w=[yb_t])
                pS = (ps[2], ps[3])
                pSt = [pst[2], pst[3]]
                S.mm([lambda e, g=g: e.matmul(pS[g // 2][:, (g % 2) * 256:(g % 2 + 1) * 256], lhsT=Bt[:, g, :],
                                               rhs=xdd[:, g * 4:(g + 1) * 4, :].rearrange("p h e -> p (h e)"),
                                               start=True, stop=True) for g in range(4)], r=[Bt_t, xd_t], w=pSt)
                if not first:
                    S.op("dve", lambda e: e.tensor_tensor(out=Hs[:], in0=Hs[:], in1=sm[:, 4, :].unsqueeze(2).to_broadcast([128, 16, 64]),
                                                          op=ALU.mult), r=smt + [H_t], w=[H_t])
                    for hf in range(2):
                        S.op("dve", lambda e, hf=hf: e.tensor_tensor(
                            out=Hs[:, hf * 8:(hf + 1) * 8, :], in0=pS[hf][:, :].rearrange("p (h e) -> p h e", h=8),
                            in1=Hs[:, hf * 8:(hf + 1) * 8, :], op=ALU.add), r=[pSt[hf], H_t], w=[H_t])
                else:
                    for hf in range(2):
                        S.op("act", lambda e, hf=hf: e.copy(out=Hs[:, hf * 8:(hf + 1) * 8, :].rearrange("p h e -> p (h e)"),
                                                            in_=pS[hf][:, :]), r=[pSt[hf]], w=[H_t])
                S.op("act", lambda e: e.copy(out=Hb[:], in_=Hs[:]), r=[H_t], w=[H_t])
                S.op("dve", lambda e: e.tensor_tensor(out=yb[:], in0=yb[:], in1=sz[:], op=ALU.mult), r=[yb_t, sz_t], w=[yb_t])
                for g in range(4):
                    S.op("act", lambda e, g=g: e.activation(out=sz[:, g * 256:(g + 1) * 256], in_=yb[:, g * 256:(g + 1) * 256],
                                                            func=AF.Square, accum_out=ysq[:, 0, g:g + 1]), r=[yb_t], w=[sz_t])
                S.op("dve", lambda e: e.tensor_scalar(out=ysq[:, 1, :], in0=ysq[:, 0, :], scalar1=1.0 / 256, scalar2=EPS,
                                                      op0=ALU.mult, op1=ALU.add), r=[sz_t], w=[sz_t])
                S.op("act", lambda e: e.sqrt(out=ysq[:, 1, :], in_=ysq[:, 1, :]), r=[sz_t], w=[sz_t])
                S.op("dve", lambda e: e.reciprocal(out=ysq[:, 1, :], in_=ysq[:, 1, :]), r=[sz_t], w=[sz_t])
                S.op("dve", lambda e: e.tensor_tensor(out=yn[:].rearrange("p (g c) -> p g c", g=4),
                                                      in0=yb[:].rearrange("p (g c) -> p g c", g=4),
                                                      in1=ysq[:, 1, :].unsqueeze(2).to_broadcast([128, 4, 256]), op=ALU.mult),
                     r=[yb_t, sz_t], w=[yn_t])
                pT_, pTt = ps[0], pst[0]
                pTb = pT_[:, :].bitcast(BF16)
                S.mm([lambda e, k=k: e.transpose(out=pTb[:, k * 128:(k + 1) * 128], in_=yn[:, k * 128:(k + 1) * 128],
                                                  identity=K.ident_b[:]) for k in range(8)], r=[yn_t, K.t], w=[pTt])
                for k in range(8):
                    S.op("act", lambda e, k=k: e.activation(out=yT[:, k, csl], in_=pTb[:, k * 128:(k + 1) * 128], func=AF.Copy,
                                                            scale=snwT[:, k:k + 1]), r=[pTt, tc], w=[yT_t])

        chunk_front(0)
        for ci in range(4):
            if ci + 1 < 4:
                chunk_front(ci + 1)
            chunk_back(ci)
        if "ssm" in C.dbg:
            dump(C, f"yT{s}_{tg}", yT[:].rearrange("p k t -> p (k t)"), [128, 8 * 512], BF16, [yT_t])
            dump(C, f"xbc{s}_{tg}", xbc[:].rearrange("p k t -> p (k t)"), [128, 16 * 512], BF16, [xbc_t])
        S.barrier()
        loc.close()
        loc = ExitStack()
        C.scope = loc
        W["wst"] = [sb(f"wst{i}", [128, 8, 512], BF16) for i in range(2)]
        W["wst_t"] = [T(f"wst{i}") for i in range(2)]
        wba = sb("wba", [128, 4, D], BF16)
        wbs = sb("wbs", [128, 8, D], BF16)
        wout = sb("wout", [128, 8, D], BF16)
        gpre = [load_w(G0), load_w(G0 + 512)]
        S.dma("pool", lambda e: e.dma_start(out=wba[:], in_=I.w_ba.rearrange("(k p) n -> p k n", p=128)), w=[tw])
        S.dma("pool", lambda e: e.dma_start(out=wbs[:], in_=I.w_bs.rearrange("(k p) n -> p k n", p=128)), w=[tw])
        S.dma("pool", lambda e: e.dma_start(out=wout[:], in_=I.w_out.rearrange("(k p) n -> p k n", p=128)), w=[tw])
        sg = sb("sg", [128, 16, 512], BF16)
        sg_t = T("sg")
        m1 = [sb(f"m1{i}", [128, 512]) for i in range(1)]
        m1_t = [T(f"m1{i}") for i in range(1)]
        mgT = sb("mgT", [128, 8, 512], BF16)
        mgT_t = T("mgT")
        xr = [sb(f"xr{i}", [128, D]) for i in range(2)]
        xr_t = [T(f"xr{i}") for i in range(2)]
        hh = [sb(f"hh{i}", [128, D]) for i in range(2)]
        hh_t = [T(f"hh{i}") for i in range(2)]
        for c in range(16):
            if c % 4 == 0:
                wg, wgt = gpre[c // 4]
            nb[0] += 1
            pq, pqt = ps[4 + nb[0] % 2], pst[4 + nb[0] % 2]
            S.mm([lambda e, k=k: e.matmul(pq[:, :], lhsT=wg[:, k, (c % 4) * 128:(c % 4 + 1) * 128], rhs=C.uT[:, k, tsl],
                                           start=(k == 0), stop=(k == 7)) for k in range(8)], r=[wgt, ut], w=[pqt])
            S.op("act", lambda e: e.activation(out=sg[:, c, :], in_=pq[:, :], func=AF.Sigmoid), r=[pqt], w=[sg_t])
            if c % 4 == 3 and c // 4 + 2 < 4:
                gpre.append(load_w(G0 + (c // 4 + 2) * 512))
        for dc in range(8):
            nb[0] += 1
            pa, pat = ps[nb[0] % 2], pst[nb[0] % 2]
            pb_, pbt = ps[2 + nb[0] % 2], pst[2 + nb[0] % 2]
            mm1, mm1t = m1[0], m1_t[0]
            S.mm([lambda e, k=k: e.matmul(pa[:, :], lhsT=wba[:, k, dc * 128:(dc + 1) * 128], rhs=C.attnT[:, k, tsl],
                                           start=(k == 0), stop=(k == 3)) for k in range(4)], r=[tw, C.attnT_t], w=[pat])
            S.mm([lambda e, k=k: e.matmul(pb_[:, :], lhsT=wbs[:, k, dc * 128:(dc + 1) * 128], rhs=yT[:, k, :],
                                           start=(k == 0), stop=(k == 7)) for k in range(8)], r=[tw, yT_t], w=[pbt])
            S.op("dve", lambda e: e.tensor_tensor(out=mm1[:], in0=pa[:, :], in1=sg[:, dc, :], op=ALU.mult),
                 r=[pat, sg_t], w=[mm1t])
            S.op("dve", lambda e: e.tensor_tensor(out=mgT[:, dc, :], in0=pb_[:, :], in1=sg[:, 8 + dc, :], op=ALU.mult),
                 r=[pbt, sg_t], w=[mgT_t])
            S.op("dve", lambda e: e.tensor_tensor(out=mgT[:, dc, :], in0=mgT[:, dc, :], in1=mm1[:], op=ALU.add),
                 r=[mm1t, mgT_t], w=[mgT_t])
        if "mg" in C.dbg:
            dump(C, f"mgT{s}_{tg}", mgT[:].rearrange("p k t -> p (k t)"), [128, 8 * 512], BF16, [mgT_t])
        rx = s * SEQ + tg * 512
        S.dma("sp", lambda e: e.dma_start(out=xr[0][:], in_=I.x[rx:rx + 128, :]), w=[xr_t[0]])
        for ti in range(4):
            nb[0] += 1
            r0 = s * SEQ + tg * 512 + ti * 128
            x_, x_t = xr[ti % 2], xr_t[ti % 2]
            h_, h_t = hh[ti % 2], hh_t[ti % 2]
            if ti + 1 < 4:
                S.dma("sp", lambda e: e.dma_start(out=xr[(ti + 1) % 2][:], in_=I.x[r0 + 128:r0 + 256, :]), w=[xr_t[(ti + 1) % 2]])
            ph = (ps[6], ps[7])
            pht = [pst[6], pst[7]]
            for hf in range(2):
                S.mm([lambda e, k=k, hf=hf: e.matmul(ph[hf][:, :], lhsT=mgT[:, k, ti * 128:(ti + 1) * 128],
                                                      rhs=wout[:, k, hf * 512:(hf + 1) * 512], start=(k == 0), stop=(k == 7))
                      for k in range(8)], r=[mgT_t, tw], w=[pht[hf]])
                S.op("dve", lambda e, hf=hf: e.tensor_tensor(out=h_[:, hf * 512:(hf + 1) * 512], in0=ph[hf][:, :],
                                                             in1=M.bc["g1"][:, hf * 512:(hf + 1) * 512], op=ALU.mult),
                     r=[pht[hf], M.t], w=[h_t])
            S.op("dve", lambda e: e.tensor_tensor(out=h_[:], in0=h_[:], in1=x_[:], op=ALU.add), r=[h_t, x_t], w=[h_t])
            phase_post_h(C, s, tg * 4 + ti, h_, h_t)
        S.barrier()
        loc.close()
        C.scope = ssd_scope


def phase_post_h(C, s, ti, h_, h_t):
    S = C.S
    r0 = s * SEQ + ti * 128
    S.dma("sp", lambda e: e.dma_start(out=C.h_scr[r0:r0 + 128, :], in_=h_[:]), r=[h_t], w=[C.h_scr_t])


def rms_rstd(C, src, src_t, ss, junk, junk_t, dim):
    S = C.S
    S.op("act", lambda e: e.activation(out=junk[:], in_=src[:], func=AF.Square, accum_out=ss[:, 0:1]), r=[src_t], w=[junk_t])
    S.op("dve", lambda e: e.tensor_scalar(out=ss[:, 1:2], in0=ss[:, 0:1], scalar1=1.0 / dim, scalar2=EPS,
                                          op0=ALU.mult, op1=ALU.add), r=[junk_t], w=[junk_t])
    S.op("act", lambda e: e.sqrt(out=ss[:, 1:2], in_=ss[:, 1:2]), r=[junk_t], w=[junk_t])
    S.op("dve", lambda e: e.reciprocal(out=ss[:, 1:2], in_=ss[:, 1:2]), r=[junk_t], w=[junk_t])


def phase_route(C):
    nc, S, sb, I, K, M = C.nc, C.S, C.sb, C.I, C.K, C.M
    ps, pst = C.ps, C.pst
    R = C.R
    tw = T("route_w", multi=True)
    wr = sb("wr", [128, 8, NE])
    wgus = sb("wgus", [128, 8, 512], BF16)
    wds = sb("wds", [128, 2, D], BF16)
    rb_bc = sb("rb_bc", [128, NE])
    iota = sb("iota", [128, NE])
    aid = sb("aid", [128, NTOK // 128, 8], I32)
    big = sb("big", [128, 4 * NE], I32)
    S.dma("sp", lambda e: e.dma_start(out=wr[:], in_=I.w_router.rearrange("(k p) n -> p k n", p=128)), w=[tw])
    S.dma("pool", lambda e: e.dma_start(out=wgus[:, :, 0:256], in_=I.w_gate_s.rearrange("(k p) n -> p k n", p=128)), w=[tw])
    S.dma("pool", lambda e: e.dma_start(out=wgus[:, :, 256:512], in_=I.w_up_s.rearrange("(k p) n -> p k n", p=128)), w=[tw])
    S.dma("pool", lambda e: e.dma_start(out=wds[:], in_=I.w_down_s.rearrange("(k p) n -> p k n", p=128)), w=[tw])
    S.dma("sp", lambda e: e.dma_start(out=rb_bc[:], in_=I.rbias_row.partition_broadcast(128)), w=[tw])
    S.dma("sp", lambda e: e.dma_start(out=iota[:], in_=I.iota_row.partition_broadcast(128)), w=[tw])
    S.dma("sp", lambda e: e.dma_start(out=aid[:], in_=I.aid), w=[tw])
    S.dma("sp", lambda e: e.dma_start(out=big[:], in_=I.bigtab), w=[tw])
    S.dma("sp", lambda e: e.dma_start(out=C.slot_info, in_=big[:]), r=[tw], w=[C.slot_t])
    base = sb("cnt_base", [128, NE])
    base_t = T("cnt_base")
    S.op("dve", lambda e: e.memset(base[:], 0.0), w=[base_t])
    hin = [sb(f"hin{i}", [128, D]) for i in range(2)]
    hin_t = [T(f"hin{i}") for i in range(2)]
    junk = sb("rjunk", [128, D])
    junk_t = T("rjunk")
    ss2 = [sb(f"rss{i}", [128, 2]) for i in range(2)]
    DB = {}
    for nm, shp, dt_ in (("u2", [128, D], F32), ("u2b", [128, D], BF16), ("u2Tf", [128, 8, 128], F32), ("u2Tb", [128, 8, 128], BF16),
                         ("sc", [128, NE], F32), ("bi", [128, NE], F32), ("mk", [128, 8, 32], F32), ("posf", [128, NE], F32),
                         ("mask8", [128, NE], BF16), ("rj", [128, NE], F32), ("m8g", [128, 8, 8], F32), ("sm", [128, 12, 8], F32),
                         ("smi", [128, 4, 8], I32), ("idx8", [128, 8], U32)):
        DB[nm] = [sb(f"r_{nm}{i}", shp, dt_) for i in range(2)]
    DBT = {nm: [T(f"r_{nm}{i}") for i in range(2)] for nm in ("u2", "u2b", "u2T", "rt")}
    DBTT = [{n_: T(f"rr_{n_}{i}", multi=(n_ in ("m8g", "sel", "pos"))) for n_ in
             ("sc", "bi", "m8g", "g", "mk", "v8", "idx", "mask", "posf", "sel", "pos")} for i in range(2)]
    rj2s = [sb(f"r_rj2_{i}", [128, NE]) for i in range(2)]
    sgl = sb("s_sgl", [128, 256])
    hsb = sb("s_hsb", [128, 256], BF16)
    hsT = sb("s_hsT", [128, 2, 128], BF16)
    ysh = sb("s_ysh", [128, D])
    sh_t = T("shared_tmp")
    for s_ in range(NSEQ):
        with ExitStack() as loc:
            C.scope = loc
            M.bc = {nm: sb(f"bc_{nm}", [128, D]) for nm in ("sh2", "sc2")}
            with ExitStack() as loc2:
                C.scope = loc2
                phase_mod(C, s_, which=("sh2", "sc2"))
                S.barrier()
            C.scope = loc
            def bind(ti):
                gt = s_ * (SEQ // 128) + ti
                r0 = gt * 128
                q_ = gt % 2
                return dict(gt=gt, r0=r0, q_=q_)

            def front_stage(ti):
                    gt = s_ * (SEQ // 128) + ti
                    r0 = gt * 128
                    h_, h_t = hin[gt % 2], hin_t[gt % 2]
                    q_ = gt % 2
                    ss = ss2[q_]
                    u2, u2b, u2Tf, u2Tb = DB["u2"][q_], DB["u2b"][q_], DB["u2Tf"][q_], DB["u2Tb"][q_]
                    sc, bi, mk, posf, mask8, rj = DB["sc"][q_], DB["bi"][q_], DB["mk"][q_], DB["posf"][q_], DB["mask8"][q_], DB["rj"][q_]
                    m8g, sm, smi, idx8 = DB["m8g"][q_], DB["sm"][q_], DB["smi"][q_], DB["idx8"][q_]
                    u2_t, u2b_t, u2T_t, rt = DBT["u2"][q_], DBT["u2b"][q_], DBT["u2T"][q_], DBT["rt"][q_]
                    rj2 = rj2s[q_]
                    S.dma("sp", lambda e: e.dma_start(out=h_[:], in_=C.h_scr[r0:r0 + 128, :]), r=[C.h_scr_t], w=[h_t])
                    rms_rstd(C, h_, h_t, ss, junk, junk_t, D)
                    S.op("dve", lambda e: e.scalar_tensor_tensor(out=u2[:], in0=h_[:], scalar=ss[:, 1:2], in1=M.bc["sc2"][:],
                                                                 op0=ALU.mult, op1=ALU.mult), r=[h_t, junk_t, M.t], w=[u2_t])
                    S.op("dve", lambda e: e.tensor_tensor(out=u2[:], in0=u2[:], in1=M.bc["sh2"][:], op=ALU.add), r=[u2_t, M.t], w=[u2_t])
                    S.op("act", lambda e: e.copy(out=u2b[:], in_=u2[:]), r=[u2_t], w=[u2b_t])
                    S.dma("pool", lambda e: e.dma_start(out=C.u2_rows[r0:r0 + 128, :], in_=u2b[:]), r=[u2b_t], w=[C.u2rows_t])
                    for hf in range(2):
                        S.mm([lambda e, k=k: e.transpose(out=ps[hf][:, (k % 4) * 128:(k % 4 + 1) * 128], in_=u2[:, k * 128:(k + 1) * 128],
                                                          identity=K.ident_f[:]) for k in range(hf * 4, hf * 4 + 4)],
                             r=[u2_t, K.t], w=[pst[hf]])
                        S.op("act", lambda e, hf=hf: e.copy(out=u2Tf[:, hf * 4:(hf + 1) * 4, :].rearrange("p k t -> p (k t)"), in_=ps[hf][:, :]),
                             r=[pst[hf]], w=[u2T_t])
                        S.op("act", lambda e, hf=hf: e.copy(out=u2Tb[:, hf * 4:(hf + 1) * 4, :].rearrange("p k t -> p (k t)"), in_=ps[hf][:, :]),
                             r=[pst[hf]], w=[u2T_t])
                    S.mm([lambda e, k=k: e.matmul(ps[2][:, 0:NE], lhsT=u2Tf[:, k, :], rhs=wr[:, k, :], start=(k == 0), stop=(k == 7))
                          for k in range(8)], r=[u2T_t, tw], w=[pst[2]])
                    w_ = [rt]
                    TT = DBTT[q_]
                    t_sc, t_bi, t_m8g, t_g, t_mk, t_v8, t_idx, t_mask, t_posf, t_sel, t_pos = (TT[n_] for n_ in (
                        "sc", "bi", "m8g", "g", "mk", "v8", "idx", "mask", "posf", "sel", "pos"))
                    S.op("act", lambda e: e.activation(out=sc[:], in_=ps[2][:, 0:NE], func=AF.Sigmoid), r=[pst[2]], w=[t_sc])
                    S.op("dve", lambda e: e.tensor_tensor(out=bi[:], in0=sc[:], in1=rb_bc[:], op=ALU.add), r=[t_sc, tw], w=[t_bi])
                    S.mm([lambda e, k=k: e.matmul(ps[4][:, :], lhsT=u2Tb[:, k, :], rhs=wgus[:, k, :], start=(k == 0), stop=(k == 7))
                          for k in range(8)], r=[u2T_t, tw], w=[pst[4]])
                    S.op("act", lambda e: e.activation(out=sgl[:], in_=ps[4][:, 0:256], func=AF.Silu), r=[pst[4]], w=[sh_t])
                    S.op("dve", lambda e: e.tensor_tensor(out=hsb[:], in0=sgl[:], in1=ps[4][:, 256:512], op=ALU.mult), r=[pst[4], sh_t], w=[sh_t])
                    p5b = ps[5][:, :].bitcast(BF16)
                    S.mm([lambda e, f=f: e.transpose(out=p5b[:, f * 128:(f + 1) * 128], in_=hsb[:, f * 128:(f + 1) * 128],
                                                      identity=K.ident_b[:]) for f in range(2)], r=[sh_t, K.t], w=[pst[5]])
                    S.op("act", lambda e: e.copy(out=hsT[:].rearrange("p f t -> p (f t)"), in_=p5b[:, 0:256]), r=[pst[5]], w=[sh_t])
                    for hf in range(2):
                        S.mm([lambda e, f=f, hf=hf: e.matmul(ps[6 + hf][:, :], lhsT=hsT[:, f, :], rhs=wds[:, f, hf * 512:(hf + 1) * 512],
                                                              start=(f == 0), stop=(f == 1)) for f in range(2)], r=[sh_t, tw], w=[pst[6 + hf]])
                    S.op("act", lambda e: e.copy(out=ysh[:, 0:512], in_=ps[6][:, :]), r=[pst[6]], w=[sh_t])
                    S.op("act", lambda e: e.copy(out=ysh[:, 512:1024], in_=ps[7][:, :]), r=[pst[7]], w=[sh_t])
                    S.dma("pool", lambda e: e.dma_start(out=C.sh_out[r0:r0 + 128, :], in_=ysh[:]), r=[sh_t], w=[C.sh_out_t])

            def back_stage(ti):
                    gt = s_ * (SEQ // 128) + ti
                    r0 = gt * 128
                    h_, h_t = hin[gt % 2], hin_t[gt % 2]
                    q_ = gt % 2
                    ss = ss2[q_]
                    u2, u2b, u2Tf, u2Tb = DB["u2"][q_], DB["u2b"][q_], DB["u2Tf"][q_], DB["u2Tb"][q_]
                    sc, bi, mk, posf, mask8, rj = DB["sc"][q_], DB["bi"][q_], DB["mk"][q_], DB["posf"][q_], DB["mask8"][q_], DB["rj"][q_]
                    m8g, sm, smi, idx8 = DB["m8g"][q_], DB["sm"][q_], DB["smi"][q_], DB["idx8"][q_]
                    u2_t, u2b_t, u2T_t, rt = DBT["u2"][q_], DBT["u2b"][q_], DBT["u2T"][q_], DBT["rt"][q_]
                    rj2 = rj2s[q_]
                    w_ = [rt]
                    TT = DBTT[q_]
                    t_sc, t_bi, t_m8g, t_g, t_mk, t_v8, t_idx, t_mask, t_posf, t_sel, t_pos = (TT[n_] for n_ in (
                        "sc", "bi", "m8g", "g", "mk", "v8", "idx", "mask", "posf", "sel", "pos"))
                    for g in range(8):
                        S.op("dve", lambda e, g=g: e.max(out=m8g[:, g, :], in_=bi[:, g * 32:(g + 1) * 32]), r=[t_bi], w=[t_m8g])
                    S.op("dve", lambda e: e.tensor_tensor(out=sm[:, 0, :], in0=m8g[:, :, 0], in1=m8g[:, :, 1], op=ALU.add), r=[t_m8g], w=[t_g])
                    S.op("dve", lambda e: e.max(out=sm[:, 1, :], in_=sm[:, 0, :]), r=[t_g], w=[t_g])
                    S.op("dve", lambda e: e.tensor_scalar(out=sm[:, 2, :], in0=sm[:, 0, :], scalar1=sm[:, 1, 3:4], scalar2=None,
                                                          op0=ALU.is_ge), r=[t_g], w=[t_g])
                    S.op("dve", lambda e: e.tensor_scalar(out=sm[:, 3, :], in0=sm[:, 2, :], scalar1=100.0, scalar2=-100.0,
                                                          op0=ALU.mult, op1=ALU.add), r=[t_g], w=[t_g])
                    S.op("dve", lambda e: e.tensor_tensor(out=mk[:], in0=bi[:].rearrange("p (g c) -> p g c", g=8),
                                                          in1=sm[:, 2, :].unsqueeze(2).to_broadcast([128, 8, 32]), op=ALU.mult),
                         r=[t_g, t_bi], w=[t_mk])
                    S.op("dve", lambda e: e.tensor_tensor(out=mk[:], in0=mk[:], in1=sm[:, 3, :].unsqueeze(2).to_broadcast([128, 8, 32]),
                                                          op=ALU.add), r=[t_g, t_mk], w=[t_mk])
                    mkf = mk[:].rearrange("p g c -> p (g c)")
                    S.op("dve", lambda e: e.max(out=sm[:, 4, :], in_=mkf), r=[t_mk], w=[t_v8])
                    S.op("dve", lambda e: e.tensor_scalar(out=mask8[:], in0=mkf, scalar1=sm[:, 4, 7:8], scalar2=None, op0=ALU.is_ge),
                         r=[t_mk, t_v8], w=[t_mask])
                    S.op("dve", lambda e: e.tensor_tensor(out=rj[:], in0=sc[:], in1=mask8[:], op=ALU.mult), r=[t_sc, t_mask], w=[t_sel])
                    S.op("dve", lambda e: e.max(out=sm[:, 6, :], in_=rj[:]), r=[t_sel], w=[t_sel])
                    S.op("dve", lambda e: e.max_index(out=idx8[:], in_max=sm[:, 6, :], in_values=rj[:]), r=[t_sel], w=[t_idx])
                    S.op("dve", lambda e: e.tensor_copy(out=sm[:, 5, :], in_=idx8[:]), r=[t_idx], w=[t_idx])
                    S.mm([lambda e: e.matmul(ps[3][:, 0:NE], lhsT=K.cm_b[:, 5, :], rhs=mask8[:], start=True, stop=True),
                          lambda e: e.matmul(ps[3][:, NE:2 * NE], lhsT=K.ones_b[:], rhs=mask8[:], start=True, stop=True)],
                         r=[t_mask, K.t], w=[pst[3]])
                    S.op("dve", lambda e: e.tensor_tensor(out=posf[:], in0=ps[3][:, 0:NE], in1=base[:], op=ALU.add),
                         r=[pst[3], base_t], w=[t_posf])
                    S.op("dve", lambda e: e.tensor_tensor(out=base[:], in0=ps[3][:, NE:2 * NE], in1=base[:], op=ALU.add),
                         r=[pst[3], t_posf], w=[base_t])
                    for k in range(8):
                        S.op("dve", lambda e, k=k: e.scalar_tensor_tensor(out=rj2[:], in0=iota[:], scalar=sm[:, 5, k:k + 1], in1=posf[:],
                                                                          op0=ALU.is_equal, op1=ALU.mult, accum_out=sm[:, 7, k:k + 1]),
                             r=[t_idx, t_posf, tw], w=[t_pos])
                    w_ = [rt]
                    rr = w_ + [t_idx, t_sel, t_pos]
                    S.op("dve", lambda e: e.tensor_copy(out=smi[:, 0, :], in_=sm[:, 7, :]), r=rr, w=w_)
                    S.op("dve", lambda e: e.tensor_scalar(out=smi[:, 1, :], in0=smi[:, 0, :], scalar1=127, scalar2=None,
                                                          op0=ALU.bitwise_and), r=w_, w=w_)
                    S.op("dve", lambda e: e.tensor_scalar(out=smi[:, 2, :], in0=smi[:, 0, :], scalar1=7, scalar2=None,
                                                          op0=ALU.arith_shift_right), r=w_, w=w_)
                    S.op("dve", lambda e: e.tensor_copy(out=sm[:, 10, :], in_=smi[:, 1, :]), r=w_, w=w_)
                    S.op("dve", lambda e: e.tensor_copy(out=sm[:, 11, :], in_=smi[:, 2, :]), r=w_, w=w_)
                    S.op("dve", lambda e: e.scalar_tensor_tensor(out=sm[:, 8, :], in0=sm[:, 5, :], scalar=4.0, in1=sm[:, 11, :],
                                                                 op0=ALU.mult, op1=ALU.add), r=rr, w=w_)
                    S.op("dve", lambda e: e.scalar_tensor_tensor(out=sm[:, 8, :], in0=sm[:, 10, :], scalar=float(4 * NE), in1=sm[:, 8, :],
                                                                 op0=ALU.mult, op1=ALU.add), r=w_, w=w_)
                    S.op("dve", lambda e: e.tensor_scalar(out=sm[:, 9, :], in0=sm[:, 7, :], scalar1=float(CAP), scalar2=None,
                                                          op0=ALU.is_lt), r=rr, w=w_)
                    S.op("dve", lambda e: e.tensor_scalar(out=sm[:, 10, :], in0=sm[:, 9, :], scalar1=-1.0e9, scalar2=1.0e9,
                                                          op0=ALU.mult, op1=ALU.add), r=w_, w=w_)
                    S.op("dve", lambda e: e.tensor_tensor(out=sm[:, 8, :], in0=sm[:, 8, :], in1=sm[:, 10, :], op=ALU.add), r=w_, w=w_)
                    S.op("dve", lambda e: e.tensor_copy(out=smi[:, 3, :], in_=sm[:, 8, :]), r=w_, w=w_)
                    S.op("dve", lambda e: e.reduce_sum(out=sm[:, 11, 0:1], in_=sm[:, 6, :], axis=AX.X), r=rr, w=w_)
                    S.op("dve", lambda e: e.reciprocal(out=sm[:, 11, 0:1], in_=sm[:, 11, 0:1]), r=w_, w=w_)
                    S.op("dve", lambda e: e.tensor_scalar(out=sm[:, 6, :], in0=sm[:, 6, :], scalar1=sm[:, 11, 0:1], scalar2=2.5,
                                                          op0=ALU.mult, op1=ALU.mult), r=rr, w=w_ + [t_sel])
                    S.op("dve", lambda e: e.tensor_tensor(out=R.wsel[:, gt, :], in0=sm[:, 6, :], in1=sm[:, 9, :], op=ALU.mult),
                         r=w_ + [t_sel], w=[R.wsel_t])
                    for k in range(8):
                        S.dma("pool", lambda e, k=k: e.indirect_dma_start(
                            out=C.slot_info_flat, out_offset=bass.IndirectOffsetOnAxis(ap=smi[:, 3, k:k + 1], axis=0),
                            in_=aid[:, gt, k:k + 1], in_offset=None, bounds_check=R.reg_slot, oob_is_err=False),
                            r=[rt, tw, C.slot_t], w=[C.slot_sc])
                    if "route" in C.dbg:
                        dump(C, f"ridx{gt}", sm[:, 5, :], [128, 8], F32, [rt])
                        dump(C, f"rslot{gt}", sm[:, 8, :], [128, 8], F32, [rt])

            NT = SEQ // 128
            front_stage(0)
            for ti in range(NT):
                if ti + 1 < NT:
                    front_stage(ti + 1)
                back_stage(ti)
            S.barrier()


def phase_experts(C):
    nc, S, sb, I, K, R = C.nc, C.S, C.sb, C.I, C.K, C.R
    ps, pst = C.ps, C.pst
    NB = CAP // 128
    NBLK = NE * NB
    SI = sb("SI", [128, NE * NB], I32)
    TK = sb("TK", [128, NE * NB], I32)
    si_t = T("SI")
    S.dma("sp", lambda e: e.dma_start(out=SI[:], in_=C.slot_info), r=[C.slot_t, C.slot_sc], w=[si_t])
    S.op("dve", lambda e: e.tensor_scalar(out=TK[:], in0=SI[:], scalar1=3, scalar2=None, op0=ALU.arith_shift_right),
         r=[si_t], w=[si_t])
    NW = 3
    wg = [sb(f"wg{i}", [128, 8, 256], BF16) for i in range(NW)]
    wu = [sb(f"wu{i}", [128, 8, 256], BF16) for i in range(NW)]
    wd = [sb(f"wd{i}", [128, 2, D], BF16) for i in range(NW)]
    wg_t = [T(f"ewg{i}") for i in range(NW)]
    wu_t = [T(f"ewu{i}") for i in range(NW)]
    wd_t = [T(f"ewd{i}") for i in range(NW)]
    xg = [[sb(f"xg{i}_{b}", [128, D], BF16) for b in range(NB)] for i in range(2)]
    xg_t = [[T(f"xg{i}_{b}") for b in range(NB)] for i in range(2)]
    for i in range(2):
        for b in range(NB):
            S.op("dve", lambda e, i=i, b=b: e.memset(xg[i][b][:], 0.0), w=[xg_t[i][b]])
    xgT = [sb(f"xgT{i}", [128, 8, 128], BF16) for i in range(2)]
    xgT_t = [T(f"xgT{i}") for i in range(2)]
    sgl = [sb(f"esgl{i}", [128, 256]) for i in range(2)]
    sgl_t = [T(f"esgl{i}") for i in range(2)]
    hb = [sb(f"ehb{i}", [128, 256], BF16) for i in range(2)]
    hb_t = [T(f"ehb{i}") for i in range(2)]
    hT = [sb(f"ehT{i}", [128, 2, 128], BF16) for i in range(2)]
    hT_t = [T(f"ehT{i}") for i in range(2)]
    NY = 3
    yo = [sb(f"eyo{i}", [128, D]) for i in range(NY)]
    yo_t = [T(f"eyo{i}") for i in range(NY)]
    yo_t2 = [T(f"eyo{i}b") for i in range(NY)]
    p4b = ps[4][:, :].bitcast(BF16)
    pht_t = [T("psHT0"), T("psHT1")]

    def load_weights(e_):
        i = e_ % NW
        S.dma("pool", lambda e: e.dma_start(out=wg[i][:], in_=I.w_gate_e[e_].rearrange("(p k) n -> p k n", p=128)), w=[wg_t[i]])
        S.dma("pool", lambda e: e.dma_start(out=wu[i][:], in_=I.w_up_e[e_].rearrange("(p k) n -> p k n", p=128)), w=[wu_t[i]])
        S.dma("pool", lambda e: e.dma_start(out=wd[i][:], in_=I.w_down_e[e_].rearrange("(p k) n -> p k n", p=128)), w=[wd_t[i]])

    def gathers(e_):
        i = e_ % 2
        for b in range(NB):
            col = e_ * NB + b
            S.dma("pool", lambda e, b=b, col=col: e.indirect_dma_start(
                out=xg[i][b][:, :], out_offset=None, in_=C.u2_rows,
                in_offset=bass.IndirectOffsetOnAxis(ap=TK[:, col:col + 1], axis=0), bounds_check=R.reg_tok, oob_is_err=False),
                r=[si_t, C.u2rows_t], w=[xg_t[i][b]])

    xgTe = [sb(f"xgTe{i}", [128, 8, CAP], BF16) for i in range(2)]
    xgTe_t = [[T(f"xgTe{i}_{b}") for b in range(NB)] for i in range(2)]
    sge = [sb(f"sge{i}", [128, CAP]) for i in range(2)]
    sge_t = [T(f"sge{i}") for i in range(2)]
    hTe = [sb(f"hTe{i}", [128, 2, CAP], BF16) for i in range(2)]
    hTe_t = [[T(f"hTe{i}_{f}") for f in range(2)] for i in range(2)]

    def stA(e_, b):
        n = e_ * NB + b
        j = n % 2
        pxb = ps[j][:, :].bitcast(BF16)
        S.mm([lambda e, k=k: e.transpose(out=pxb[:, k * 128:(k + 1) * 128], in_=xg[e_ % 2][b][:, k::8],
                                          identity=K.ident_b[:]) for k in range(8)], r=[xg_t[e_ % 2][b], K.t], w=[pst[j]])
        dst = xgTe[e_ % 2][:, :, b * 128:(b + 1) * 128]
        src = pxb.rearrange("p (k t) -> p k t", k=8)
        if b % 2 == 0:
            S.op("dve", lambda e: e.tensor_copy(out=dst, in_=src), r=[pst[j]], w=[xgTe_t[e_ % 2][b]])
        else:
            S.op("act", lambda e: e.copy(out=dst, in_=src), r=[pst[j]], w=[xgTe_t[e_ % 2][b]])

    def stB(e_, c):
        w = e_ % NW
        i = e_ % 2
        ww, wt = (wg, wg_t) if c < 2 else (wu, wu_t)
        f = c % 2
        S.mm([lambda e, k=k: e.matmul(ps[2 + c][:, :], lhsT=ww[w][:, k, f::2], rhs=xgTe[i][:, k, :], start=(k == 0), stop=(k == 7))
              for k in range(8)], r=xgTe_t[i] + [wt[w]], w=[pst[2 + c]])
        if c < 2:
            S.op("act", lambda e: e.activation(out=sge[c][:], in_=ps[2 + c][:, :], func=AF.Silu), r=[pst[2 + c]], w=[sge_t[c]])
        else:
            S.op("dve", lambda e: e.tensor_tensor(out=hTe[i][:, f, :], in0=sge[f][:], in1=ps[2 + c][:, :], op=ALU.mult),
                 r=[pst[2 + c], sge_t[f]], w=[hTe_t[i][f]])

    def stD(e_, b):
        n = e_ * NB + b
        i = e_ % 2
        w = e_ % NW
        y = n % NY
        for hf in range(2):
            S.mm([lambda e, f=f, hf=hf: e.matmul(ps[6 + hf][:, :], lhsT=hTe[i][:, f, b * 128:(b + 1) * 128],
                                                  rhs=wd[w][:, f, hf * 512:(hf + 1) * 512], start=(f == 0), stop=(f == 1))
                  for f in range(2)], r=hTe_t[i] + [wd_t[w]], w=[pst[6 + hf]])
        S.op("act", lambda e: e.copy(out=yo[y][:, 0:512], in_=ps[6][:, :]), r=[pst[6]], w=[yo_t[y]])
        S.op("dve", lambda e: e.tensor_copy(out=yo[y][:, 512:1024], in_=ps[7][:, :]), r=[pst[7]], w=[yo_t2[y]])
        S.dma("pool", lambda e: e.indirect_dma_start(
            out=C.ye2, out_offset=bass.IndirectOffsetOnAxis(ap=SI[:, n:n + 1], axis=0), in_=yo[y][:, :], in_offset=None,
            bounds_check=R.reg_aid, oob_is_err=False), r=[yo_t[y], yo_t2[y], si_t], w=[C.ye2_t])

    load_weights(0)
    gathers(0)
    load_weights(1)
    gathers(1)
    for b in range(NB):
        stA(0, b)
    for e_ in range(NE + 1):
        if e_ + 2 < NE:
            gathers(e_ + 2)
        for b in range(NB):
            if e_ + 1 < NE:
                stA(e_ + 1, b)
            if e_ < NE:
                stB(e_, b)
            if e_ >= 1:
                stD(e_ - 1, b)
        if e_ + 2 < NE:
            load_weights(e_ + 2)


def phase_final(C):
    nc, S, sb, I, K, M, R = C.nc, C.S, C.sb, C.I, C.K, C.M, C.R
    M.g2_bc = [sb(f"bc_g2{b}", [128, D]) for b in range(NSEQ)]
    nfin_bc = sb("nfin_bc", [128, D])
    S.dma("sp", lambda e: e.dma_start(out=nfin_bc[:], in_=I.nfin_row.partition_broadcast(128)), w=[M.t])
    outer = C.scope
    with ExitStack() as loc:
        C.scope = loc
        phase_mod(C, "g2")
        S.barrier()
    C.scope = outer
    hin = [sb(f"fh{i}", [128, D]) for i in range(2)]
    hin_t = [T(f"fh{i}") for i in range(2)]
    shd = [sb(f"fsh{i}", [128, D]) for i in range(2)]
    shd_t = [T(f"fsh{i}") for i in range(2)]
    ye = [sb(f"fye{i}", [128, 8, D]) for i in range(2)]
    ye_t = [T(f"fye{i}") for i in range(2)]
    junk = sb("fjunk", [128, D])
    junk_t = T("fjunk")
    ss = [sb(f"fss{i}", [128, 2]) for i in range(2)]
    ob = [sb(f"fo{i}", [128, D]) for i in range(2)]
    ob_t = [T(f"fo{i}") for i in range(2)]
    for gt in range(NTOK // 128):
        s_ = gt // (SEQ // 128)
        r0 = gt * 128
        h_, h_t = hin[gt % 2], hin_t[gt % 2]
        a_, a_t = shd[gt % 2], shd_t[gt % 2]
        o_, o_t = ob[gt % 2], ob_t[gt % 2]
        y_, y_t = ye[gt % 2], ye_t[gt % 2]
        S.dma("sp", lambda e: e.dma_start(out=h_[:], in_=C.h_scr[r0:r0 + 128, :]), r=[C.h_scr_t], w=[h_t])
        S.dma("sp", lambda e: e.dma_start(out=a_[:], in_=C.sh_out[r0:r0 + 128, :]), r=[C.sh_out_t], w=[a_t])
        S.dma("sp", lambda e: e.dma_start(out=y_[:], in_=C.ye2[r0 * 8:(r0 + 128) * 8, :].rearrange("(t k) d -> t k d", k=8)),
              r=[C.ye2_t], w=[y_t])
        for k in range(8):
            S.op("dve", lambda e, k=k: e.scalar_tensor_tensor(out=a_[:], in0=y_[:, k, :], scalar=R.wsel[:, gt, k:k + 1],
                                                              in1=a_[:], op0=ALU.mult, op1=ALU.add),
                 r=[y_t, R.wsel_t, a_t], w=[a_t])
        S.op("dve", lambda e: e.tensor_tensor(out=a_[:], in0=a_[:], in1=M.g2_bc[s_][:], op=ALU.mult), r=[a_t, M.t], w=[a_t])
        S.op("dve", lambda e: e.tensor_tensor(out=a_[:], in0=a_[:], in1=h_[:], op=ALU.add), r=[a_t, h_t], w=[a_t])
        rms_rstd(C, a_, a_t, ss[gt % 2], junk, junk_t, D)
        S.op("dve", lambda e: e.scalar_tensor_tensor(out=o_[:], in0=a_[:], scalar=ss[gt % 2][:, 1:2], in1=nfin_bc[:],
                                                     op0=ALU.mult, op1=ALU.mult), r=[a_t, junk_t, M.t], w=[o_t])
        S.dma("pool", lambda e: e.dma_start(out=C.out[r0:r0 + 128, :], in_=o_[:]), r=[o_t])


def host_inputs(inputs):
    f = np.float32
    x = np.asarray(inputs["x"], f)
    c = np.asarray(inputs["c"], f)
    pos = np.asarray(inputs["positions"], np.int32)
    shared = {}
    shared["w_mod"] = np.ascontiguousarray(inputs["w_mod"][0], f)
    bm = np.asarray(inputs["b_mod"][0], f)
    shared["b_modT"] = np.ascontiguousarray(bm.reshape(48, 128).T)
    shared["b_mod_row"] = bm.reshape(1, -1)
    shared["nmwT"] = np.ascontiguousarray(np.asarray(inputs["norm_mix_w"][0], f).reshape(8, 128).T)
    shared["nfw_row"] = np.asarray(inputs["norm_ffn_w"][0], f).reshape(1, -1)
    shared["nfin_row"] = np.asarray(inputs["norm_final_w"], f).reshape(1, -1)
    shared["w_in"] = np.ascontiguousarray(inputs["w_in"][0], f)
    shared["ident"] = np.eye(128, dtype=f)
    shared["w_ba"] = np.ascontiguousarray(inputs["w_branch_attn"][0], f)
    shared["w_bs"] = np.ascontiguousarray(inputs["w_branch_ssm"][0], f)
    shared["w_out"] = np.ascontiguousarray(inputs["w_out"][0], f)
    cw = np.asarray(inputs["conv_w"][0], f)
    shared["conv_wT"] = np.ascontiguousarray(cw.reshape(4, 16, 128).transpose(2, 1, 0))
    shared["conv_bT"] = np.ascontiguousarray(np.asarray(inputs["conv_b"][0], f).reshape(16, 128).T)
    shared["snwT"] = np.ascontiguousarray(np.asarray(inputs["ssm_norm_w"][0], f).reshape(8, 128).T)
    shared["hrow"] = np.concatenate([np.asarray(inputs[k][0], f) for k in ("dt_bias", "a_log", "d_skip")]).reshape(1, 48)
    cm = np.zeros((128, 6, 128), f)
    for m in range(32):
        cm[(m + 16) % 32, 0, m] = 1.0
    kk, qq = np.meshgrid(np.arange(128), np.arange(128), indexing="ij")
    cm[:, 1, :] = np.where(kk <= qq, 0.0, NEG)
    cm[:, 2, :] = np.where(kk >= qq, 0.0, NEG)
    cm[:, 3, :] = (kk <= qq).astype(f)
    cm[:, 4, :] = (kk > qq).astype(f)
    cm[:, 5, :] = (kk < qq).astype(f)
    shared["w_router"] = np.ascontiguousarray(inputs["w_router"][0], f)
    shared["rbias_row"] = np.asarray(inputs["router_bias"][0], f).reshape(1, -1)
    shared["iota_row"] = np.arange(NE, dtype=f).reshape(1, -1)
    tokid = (np.arange(NTOK // 128)[None, :, None] * 128 + np.arange(128)[:, None, None])
    shared["aid"] = np.ascontiguousarray((tokid * 8 + np.arange(8)[None, None, :]).astype(np.int32))
    shared["bigtab"] = np.full((128, 4 * NE), 0x3FFFFFF8, np.int32)
    shared["w_gate_s"] = np.ascontiguousarray(inputs["w_gate_s"][0], f)
    shared["w_gate_e"] = np.ascontiguousarray(inputs["w_gate_e"][0], f)
    shared["w_up_e"] = np.ascontiguousarray(inputs["w_up_e"][0], f)
    shared["w_down_e"] = np.ascontiguousarray(inputs["w_down_e"][0], f)
    shared["w_up_s"] = np.ascontiguousarray(inputs["w_up_s"][0], f)
    shared["w_down_s"] = np.ascontiguousarray(inputs["w_down_s"][0], f)
    shared["cmat"] = cm
    rc = np.zeros((128, 2), f)
    half = 16
    invf = (500000.0 ** (-np.arange(half, dtype=np.float32) / half)).astype(f)
    rc[:32, 0] = np.concatenate([invf, invf])
    rc[:32, 1] = np.concatenate([-np.ones(16, f), np.ones(16, f)])
    shared["ropec"] = rc
    maps = []
    for cid in range(NCORES):
        m = dict(shared)
        m["x"] = np.ascontiguousarray(x[cid * NSEQ:(cid + 1) * NSEQ].reshape(NTOK, D))
        cc = c[cid * NSEQ:(cid + 1) * NSEQ]
        m["cT"] = np.ascontiguousarray(cc.reshape(NSEQ, 8, 128).transpose(2, 1, 0))
        m["pos"] = np.ascontiguousarray(pos[cid * NSEQ:(cid + 1) * NSEQ])
        maps.append(m)
    return maps


_CACHE = {}


def kernel(**inputs):
    if "nc" not in _CACHE:
        _CACHE["nc"] = build()
    nc, C = _CACHE["nc"]
    maps = host_inputs(inputs)
    res = run_bass_kernel_spmd(nc, maps, core_ids=list(range(NCORES)))
    out = np.concatenate([r["out"].reshape(NSEQ, SEQ, D) for r in res.results], axis=0)
    return out.astype(np.float32)
```

```python
import numpy as np
import ml_dtypes
from contextlib import ExitStack
import concourse.bass as bass
import concourse.mybir as mybir
from concourse.bass_utils import run_bass_kernel_spmd

F32 = mybir.dt.float32
BF16 = mybir.dt.bfloat16
I32 = mybir.dt.int32
U32 = mybir.dt.uint32
ALU = mybir.AluOpType
AF = mybir.ActivationFunctionType
AX = mybir.AxisListType

NCORES = 8
D = 1024
SEQ = 2048
NSEQ = 2
NTOK = NSEQ * SEQ
IN_DIM = 9744
Q0, K0, V0, Z0, X0, DT0, G0 = 0, 1536, 3072, 4608, 5632, 7680, 7696
NE = 256
CAP = 512
EPS = 1e-6
NEG = -30000.0


class Tok:
    __slots__ = ("key", "sem", "val")

    def __init__(self, key, sem, val):
        self.key, self.sem, self.val = key, sem, val


class T:
    __slots__ = ("name", "w", "r", "multi", "ws")

    def __init__(self, name="", multi=False):
        self.name, self.w, self.r, self.multi, self.ws = name, None, {}, multi, {}


class Sched:
    def __init__(self, nc, es, nslots=8):
        self.nc = nc
        self.eng = dict(pe=nc.tensor, act=nc.scalar, dve=nc.vector, pool=nc.gpsimd, sp=nc.sync)
        self.sem = {k: es.enter_context(nc.semaphore("sem_" + k)) for k in ("pe", "act", "dve", "pool")}
        self.cnt = dict.fromkeys(self.sem, 0)
        self.seen = {k: {} for k in self.eng}
        self.slots = {q: [[es.enter_context(nc.semaphore(f"dq_{q}{i}")), 0] for i in range(nslots)]
                      for q in ("sp", "pool", "act")}
        self.slot_i = dict.fromkeys(self.slots, 0)
        self.ninst = 0

    def _wait(self, en, toks):
        e, seen = self.eng[en], self.seen[en]
        for tk in toks:
            if tk is None or seen.get(tk.key, 0) >= tk.val:
                continue
            if tk.key == en == "pe":
                continue
            e.wait_ge(tk.sem, tk.val)
            seen[tk.key] = tk.val

    @staticmethod
    def _deps(r, w):
        toks = []
        for t in r:
            toks.append(t.w)
            if t.multi:
                toks.extend(t.ws.values())
        for t in w:
            if not t.multi:
                toks.append(t.w)
            toks.extend(t.r.values())
        return toks

    @staticmethod
    def _commit(tok, r, w):
        for t in w:
            if t.multi:
                t.ws[tok.key] = tok
                t.r = {}
            else:
                t.w, t.r = tok, {}
        for t in r:
            if t.w is not tok and t not in w:
                t.r[tok.key] = tok

    def op(self, en, fn, r=(), w=()):
        self._wait(en, self._deps(r, w))
        ins = fn(self.eng[en])
        self.cnt[en] += 1
        ins.then_inc(self.sem[en], 1)
        self._commit(Tok(en, self.sem[en], self.cnt[en]), r, w)
        self.ninst += 1

    def mm(self, fns, r=(), w=()):
        self._wait("pe", self._deps(r, w))
        ins = None
        for fn in fns:
            ins = fn(self.eng["pe"])
        self.cnt["pe"] += 1
        ins.then_inc(self.sem["pe"], 1)
        self._commit(Tok("pe", self.sem["pe"], self.cnt["pe"]), r, w)
        self.ninst += len(fns)

    def dma(self, q, fn, r=(), w=()):
        slots = self.slots[q]
        i = self.slot_i[q]
        self.slot_i[q] = (i + 1) % len(slots)
        sl = slots[i]
        key = f"{q}{i}"
        if sl[1] > 0:
            self._wait(q, [Tok(key, sl[0], sl[1])])
        self._wait(q, self._deps(r, w))
        ins = fn(self.eng[q])
        sl[1] += 16
        ins.then_inc(sl[0], 16)
        self._commit(Tok(key, sl[0], sl[1]), r, w)
        self.ninst += 1

    def all_toks(self):
        toks = []
        for q, slots in self.slots.items():
            for i, sl in enumerate(slots):
                if sl[1] > 0:
                    toks.append(Tok(f"{q}{i}", sl[0], sl[1]))
        for en in ("pe", "act", "dve", "pool"):
            if self.cnt[en] > 0:
                toks.append(Tok(en, self.sem[en], self.cnt[en]))
        return toks

    def barrier(self):
        toks = self.all_toks()
        for en in ("pe", "act", "dve", "pool", "sp"):
            self._wait(en, [t for t in toks if t.key != en])

    def finish(self):
        toks = []
        for q, slots in self.slots.items():
            for i, sl in enumerate(slots):
                if sl[1] > 0:
                    toks.append(Tok(f"{q}{i}", sl[0], sl[1]))
        for en in ("pe", "act", "dve", "pool"):
            if self.cnt[en] > 0:
                toks.append(Tok(en, self.sem[en], self.cnt[en]))
        self._wait("sp", toks)


class Ctx:
    pass


def build(dbg=()):
    nc = bass.Bass("TRN2", target_bir_lowering=False)
    es = ExitStack()
    S = Sched(nc, es)
    C = Ctx()
    C.nc, C.es, C.S, C.dbg, C.dbgout = nc, es, S, dbg, {}

    def din(name, shape, dt=F32):
        return nc.dram_tensor(name, list(shape), dt, kind="ExternalInput").ap()

    C.nalloc = 0

    def sb(name, shape, dt=F32, scope=None):
        C.nalloc += 1
        return (scope or C.scope).enter_context(nc.sbuf_tensor(f"s{C.nalloc}_{name}", list(shape), dt))

    C.scope = es

    C.din, C.sb = din, sb
    I = Ctx()
    C.I = I
    I.x = din("x", [NTOK, D])
    I.cT = din("cT", [128, 8, NSEQ])
    I.pos = din("pos", [NSEQ, SEQ], I32)
    I.w_mod = din("w_mod", [D, 6 * D])
    I.b_modT = din("b_modT", [128, 48])
    I.b_mod_row = din("b_mod_row", [1, 6 * D])
    I.nmwT = din("nmwT", [128, 8])
    I.nfw_row = din("nfw_row", [1, D])
    I.nfin_row = din("nfin_row", [1, D])
    I.w_in = din("w_in", [D, IN_DIM])
    I.ident = din("ident", [128, 128])
    I.w_ba = din("w_ba", [512, D])
    I.w_bs = din("w_bs", [D, D])
    I.w_out = din("w_out", [D, D])
    I.conv_wT = din("conv_wT", [128, 16, 4])
    I.conv_bT = din("conv_bT", [128, 16])
    I.snwT = din("snwT", [128, 8])
    I.hrow = din("hrow", [1, 48])
    C.h_scr = nc.dram_tensor("h_scr", [NTOK, D], F32, kind="Internal").ap()
    C.h_scr_t = T("h_scr", multi=True)
    I.w_router = din("w_router", [D, NE])
    I.rbias_row = din("rbias_row", [1, NE])
    I.iota_row = din("iota_row", [1, NE])
    I.aid = din("aid", [128, NTOK // 128, 8], I32)
    I.bigtab = din("bigtab", [128, 4 * NE], I32)
    I.w_gate_s = din("w_gate_s", [D, 256])
    I.w_up_s = din("w_up_s", [D, 256])
    I.w_down_s = din("w_down_s", [256, D])
    C.u2_rows = nc.dram_tensor("u2_rows", [NTOK, D], BF16, kind="Internal").ap()
    C.u2rows_t = T("u2_rows", multi=True)
    si_h = nc.dram_tensor("slot_info", [128 * 4 * NE, 1], I32, kind="Internal")
    C.slot_info_flat = si_h.ap()
    C.slot_info = si_h.ap().rearrange("(p n) o -> p (n o)", p=128)
    C.slot_t = T("slot_info")
    C.slot_sc = T("slot_scatter", multi=True)
    C.sh_out = nc.dram_tensor("sh_out", [NTOK, D], F32, kind="Internal").ap()
    C.ye2 = nc.dram_tensor("ye2", [NTOK * 8, D], F32, kind="Internal").ap()
    C.ye2_t = T("ye2", multi=True)
    I.w_gate_e = din("w_gate_e", [NE, D, 256])
    I.w_up_e = din("w_up_e", [NE, D, 256])
    I.w_down_e = din("w_down_e", [NE, 256, D])
    C.sh_out_t = T("sh_out", multi=True)
    C.out = nc.dram_tensor("out", [NTOK, D], F32, kind="ExternalOutput").ap()

    K = Ctx()
    C.K = K
    K.t = T("consts")
    K.ident_f = sb("ident_f", [128, 128], F32)
    K.ident_b = sb("ident_b", [128, 128], BF16)
    K.ones_f = sb("ones_f", [128, 128], F32)
    S.dma("sp", lambda e: e.dma_start(out=K.ident_f[:], in_=I.ident), w=[K.t])
    S.op("dve", lambda e: e.tensor_copy(out=K.ident_b[:], in_=K.ident_f[:]), r=[K.t], w=[K.t])
    S.op("dve", lambda e: e.memset(K.ones_f[:], 1.0), w=[K.t])
    K.ones_b = sb("ones_b", [128, 128], BF16)
    S.op("dve", lambda e: e.memset(K.ones_b[:], 1.0), w=[K.t])
    I.cmat = din("cmat", [128, 6, 128])
    I.ropec = din("ropec", [128, 2])
    cm_f = sb("cmat_f", [128, 6, 128], F32)
    K.cm_b = sb("cmat_b", [128, 6, 128], BF16)
    K.ropec = sb("ropec", [128, 2])
    S.dma("sp", lambda e: e.dma_start(out=cm_f[:], in_=I.cmat), w=[K.t])
    S.dma("sp", lambda e: e.dma_start(out=K.ropec[:], in_=I.ropec), w=[K.t])
    S.op("dve", lambda e: e.tensor_copy(out=K.cm_b[:], in_=cm_f[:]), r=[K.t], w=[K.t])
    K.cm_f = cm_f
    K.p32, K.mcur, K.mprev = K.cm_b[:, 0, :], K.cm_b[:, 1, :], K.cm_b[:, 2, :]

    C.ps = [es.enter_context(nc.psum_tensor(f"ps{i}", [128, 512], F32)) for i in range(8)]
    C.pst = [T(f"ps{i}") for i in range(8)]

    M = Ctx()
    C.M = M
    M.t = T("mod")
    M.modT = sb("modT", [128, 48, NSEQ])
    M.scale1T = sb("scale1T", [128, 8, NSEQ])
    with ExitStack() as loc:
        C.scope = loc
        phase_mod(C, None)
        S.barrier()
    for s in range(NSEQ):
        with ExitStack() as sq:
            C.scope = sq
            C.uT = sb("uT", [128, 8, SEQ], BF16)
            C.uT_t = [T(f"uT{g}") for g in range(4)]
            C.attnT = sb("attnT", [128, 4, SEQ], BF16)
            C.attnT_t = T("attnT")
            M.bc = {nm: sb(f"bc_{nm}", [128, D]) for nm in ("g1",)}
            with ExitStack() as loc:
                C.scope = loc
                phase_mod(C, s, which=("g1",))
                S.barrier()
            with ExitStack() as loc:
                C.scope = loc
                phase_norm1(C, s)
                S.barrier()
            if "uT" in dbg:
                dump(C, f"uT{s}", C.uT[:].rearrange("p k t -> p (k t)"), [128, 8 * SEQ], BF16, C.uT_t)
            with ExitStack() as loc:
                C.scope = loc
                phase_rope_tables(C, s)
                if s == 0:
                    zt = sb("zero_tile", [128, 512])
                    zt_t = T("zero_tile")
                    S.op("dve", lambda e: e.memset(zt[:], 0.0), w=[zt_t])
                    for zi in range(NTOK * 8 // 128):
                        for zh in range(2):
                            S.dma("sp", lambda e, zi=zi, zh=zh: e.dma_start(
                                out=C.ye2[zi * 128:(zi + 1) * 128, zh * 512:(zh + 1) * 512], in_=zt[:]), r=[zt_t], w=[C.ye2_t])
                phase_attn(C, s)
                if "attn" in dbg:
                    dump(C, f"attnT{s}", C.attnT[:].rearrange("p h t -> p (h t)"), [128, 4 * SEQ], BF16, [C.attnT_t])
                    dump(C, f"qk{s}", C.A.qk[1][1][:], [128, SEQ], BF16, [C.A.qk_t[1][1]])
                S.barrier()
            with ExitStack() as loc:
                C.scope = loc
                phase_ssd(C, s)
                S.barrier()
            S.barrier()
    C.scope = es
    R = Ctx()
    C.R = R
    R.reg_slot = nc.gpsimd.to_reg(128 * 4 * NE - 1)
    R.reg_tok = nc.gpsimd.to_reg(NTOK - 1)
    R.reg_aid = nc.gpsimd.to_reg(NTOK * 8 - 1)
    R.wsel = sb("wsel", [128, NTOK // 128, 8])
    R.wsel_t = T("wsel", multi=True)
    with ExitStack() as loc:
        C.scope = loc
        phase_route(C)
        S.barrier()
    C.scope = es
    with ExitStack() as loc:
        C.scope = loc
        phase_experts(C)
        S.barrier()
    with ExitStack() as loc:
        C.scope = loc
        phase_final(C)
        S.barrier()
    C.scope = es
    if "route" in dbg:
        dump(C, "wsel", R.wsel[:].rearrange("p g k -> p (g k)"), [128, NTOK // 128 * 8], F32, [R.wsel_t])
        dump(C, "slot_info", C.slot_info, [128, 4 * NE], I32, [C.slot_t, C.slot_sc])
        dump(C, "sh_out", C.sh_out, [NTOK, D], F32, [C.sh_out_t])
        dump(C, "u2_rows", C.u2_rows, [NTOK, D], BF16, [C.u2rows_t])
    if "h" in dbg:
        d_ = nc.dram_tensor("dbg_h", [NTOK, D], F32, kind="ExternalOutput").ap()
        S.dma("sp", lambda e: e.dma_start(out=d_, in_=C.h_scr), r=[C.h_scr_t])
        C.dbgout["h"] = "dbg_h"
    S.finish()
    return nc, C


def dump(C, name, ap, shape, dt, tiles):
    d = C.nc.dram_tensor("dbg_" + name, list(shape), dt, kind="ExternalOutput").ap()
    C.S.dma("sp", lambda e: e.dma_start(out=d, in_=ap), r=tiles)
    C.dbgout[name] = "dbg_" + name


def phase_mod(C, seq, which=("g1", "sh2", "sc2")):
    nc, S, sb, I, K, M = C.nc, C.S, C.sb, C.I, C.K, C.M
    condT = sb("condT", [128, 8, NSEQ])
    cond_bc = sb("cond_bc", [128, 8, NSEQ, 128], BF16)
    condb = sb("condb", [128, 8, NSEQ], BF16)
    bmod_row = sb("bmod_row", [1, 6 * D], BF16)
    tl = T("modload")
    S.dma("sp", lambda e: e.dma_start(out=condT[:], in_=I.cT), w=[tl])
    S.dma("pool", lambda e: e.dma_start(out=bmod_row[:], in_=I.b_mod_row), w=[tl])
    S.op("act", lambda e: e.activation(out=condT[:], in_=condT[:], func=AF.Silu), r=[tl], w=[tl])
    S.op("dve", lambda e: e.tensor_copy(out=cond_bc[:], in_=condT[:].unsqueeze(3).to_broadcast([128, 8, NSEQ, 128])),
         r=[tl], w=[tl])
    S.op("dve", lambda e: e.tensor_copy(out=condb[:], in_=condT[:]), r=[tl], w=[tl])
    wblk = [sb(f"wmod_blk{i}", [128, 8, 512], BF16) for i in range(2)]
    wblk_t = [T(f"wmod_blk{i}") for i in range(2)]
    psm, psm_t = C.ps[0], C.pst[0]
    if seq is None:
        b_modT = sb("b_modT", [128, 48])
        nmwT = sb("nmwT", [128, 8])
        S.dma("sp", lambda e: e.dma_start(out=b_modT[:], in_=I.b_modT), w=[tl])
        S.dma("sp", lambda e: e.dma_start(out=nmwT[:], in_=I.nmwT), w=[tl])
        blocks = list(range(12))
        names = {}
        bs = []
    elif seq == "g2":
        blocks = [10, 11]
        names = {10: "g2", 11: "g2"}
        bs = list(range(NSEQ))
    else:
        nfw_bc = sb("nfw_bc", [128, D])
        S.dma("sp", lambda e: e.dma_start(out=nfw_bc[:], in_=I.nfw_row.partition_broadcast(128)), w=[M.t])
        names = {jb: nm for jb, nm in {4: "g1", 5: "g1", 6: "sh2", 7: "sh2", 8: "sc2", 9: "sc2"}.items() if nm in which}
        blocks = sorted(names)
        bs = [seq]
    for ib, jb in enumerate(blocks):
        wb, wt = wblk[ib % 2], wblk_t[ib % 2]
        S.dma("pool", lambda e: e.dma_start(out=wb[:], in_=I.w_mod[:, jb * 512:(jb + 1) * 512]
                                          .rearrange("(k p) n -> p k n", p=128)), w=[wt])
        if seq is None:
            for j in range(4):
                jc = jb * 4 + j
                S.mm([lambda e, k=k: e.matmul(psm[:, jc * 2:jc * 2 + 2], lhsT=wb[:, k, j * 128:(j + 1) * 128],
                                               rhs=condb[:, k, :], start=(k == 0), stop=(k == 7)) for k in range(8)],
                     r=[wt, tl], w=[psm_t])
        if jb in names:
            half = jb % 2
            for b in bs:
                pb, pbt = C.ps[1 + b], C.pst[1 + b]
                fns = [lambda e, k=k: e.matmul(pb[:, :], lhsT=cond_bc[:, k, b, :], rhs=wb[:, k, :],
                                               start=(k == 0), stop=False) for k in range(8)]
                fns.append(lambda e: e.matmul(pb[:, :], lhsT=K.ones_b[0:1, :], rhs=bmod_row[0:1, jb * 512:(jb + 1) * 512],
                                              start=False, stop=True))
                S.mm(fns, r=[wt, tl, K.t], w=[pbt])
                dst = M.g2_bc[b] if names[jb] == "g2" else M.bc[names[jb]]
                S.op("act", lambda e: e.copy(out=dst[:, half * 512:(half + 1) * 512], in_=pb[:, :]), r=[pbt], w=[M.t])
    if seq is None:
        S.op("dve", lambda e: e.tensor_tensor(out=M.modT[:], in0=psm[:, 0:96].rearrange("p (j b) -> p j b", b=NSEQ),
                                              in1=b_modT[:].unsqueeze(2).to_broadcast([128, 48, NSEQ]), op=ALU.add),
             r=[psm_t, tl], w=[M.t])
        S.op("dve", lambda e: e.scalar_tensor_tensor(out=M.scale1T[:], in0=M.modT[:, 8:16, :], scalar=1.0,
                                                     in1=nmwT[:].unsqueeze(2).to_broadcast([128, 8, NSEQ]),
                                                     op0=ALU.add, op1=ALU.mult), r=[M.t, tl], w=[M.t])
    elif seq != "g2" and "sc2" in which:
        t = M.bc["sc2"]
        S.op("dve", lambda e: e.scalar_tensor_tensor(out=t[:], in0=t[:], scalar=1.0, in1=nfw_bc[:],
                                                     op0=ALU.add, op1=ALU.mult), r=[M.t], w=[M.t])


def phase_norm1(C, s):
    nc, S, sb, I, K, M = C.nc, C.S, C.sb, C.I, C.K, C.M
    if True:
        C.xin = [sb(f"xin{i}", [128, D]) for i in range(2)]
        C.xin_t = [T(f"xin{i}") for i in range(2)]
        C.xn = [sb(f"xn{i}", [128, D], BF16) for i in range(2)]
        C.xn_t = [T(f"xn{i}") for i in range(2)]
        C.sq = sb("sq_junk", [128, D])
        C.sq_t = T("sq")
        C.ss = [sb(f"ss{i}", [128, 2]) for i in range(2)]
        C.n1tmp = [sb(f"n1tmp{i}", [128, 8, 128]) for i in range(2)]
        C.n1tmp_t = [T(f"n1tmp{i}") for i in range(2)]
    for i in range(SEQ // 128):
        xt, xtt = C.xin[i % 2], C.xin_t[i % 2]
        xn, xnt = C.xn[i % 2], C.xn_t[i % 2]
        ss = C.ss[i % 2]
        r0 = s * SEQ + i * 128
        S.dma("sp", lambda e: e.dma_start(out=xt[:], in_=I.x[r0:r0 + 128, :]), w=[xtt])
        S.op("act", lambda e: e.activation(out=C.sq[:], in_=xt[:], func=AF.Square, accum_out=ss[:, 0:1]),
             r=[xtt], w=[C.sq_t, xnt])
        S.op("dve", lambda e: e.tensor_scalar(out=ss[:, 1:2], in0=ss[:, 0:1], scalar1=1.0 / D, scalar2=EPS,
                                              op0=ALU.mult, op1=ALU.add), r=[xnt], w=[xnt])
        S.op("act", lambda e: e.sqrt(out=ss[:, 1:2], in_=ss[:, 1:2]), r=[xnt], w=[xnt])
        S.op("dve", lambda e: e.reciprocal(out=ss[:, 1:2], in_=ss[:, 1:2]), r=[xnt], w=[xnt])
        S.op("dve", lambda e: e.tensor_scalar(out=xn[:], in0=xt[:], scalar1=ss[:, 1:2], scalar2=None,
                                              op0=ALU.mult), r=[xtt, xnt], w=[xnt])
        pb, pbt = C.ps[i % 2], C.pst[i % 2]
        pbb = pb[:, :].bitcast(BF16)
        S.mm([lambda e, k=k: e.transpose(out=pbb[:, k * 128:(k + 1) * 128], in_=xn[:, k * 128:(k + 1) * 128],
                                          identity=K.ident_b[:]) for k in range(8)], r=[xnt, K.t], w=[pbt])
        ut = C.uT_t[i // 4]
        tmp32, tmp32_t = C.n1tmp[i % 2], C.n1tmp_t[i % 2]
        S.op("dve", lambda e: e.tensor_tensor(out=tmp32[:], in0=pbb.rearrange("p (k t) -> p k t", k=8),
                                              in1=M.scale1T[:, :, s].unsqueeze(2).to_broadcast([128, 8, 128]), op=ALU.mult),
             r=[pbt, M.t], w=[tmp32_t])
        S.op("dve", lambda e: e.tensor_tensor(out=C.uT[:, :, i * 128:(i + 1) * 128], in0=tmp32[:],
                                              in1=M.modT[:, 0:8, s].unsqueeze(2).to_broadcast([128, 8, 128]), op=ALU.add),
             r=[tmp32_t, M.t], w=[ut])


def qsel(d, r, n):
    st = d * 128 * n + r
    return slice(st, st + d * 127 + 1, d)


def phase_rope_tables(C, s):
    nc, S, sb, I, K = C.nc, C.S, C.sb, C.I, C.K
    C.cosT = sb("cosT", [32, SEQ])
    C.sinT = sb("sinT", [32, SEQ])
    C.rope_t = T("rope")
    outer_scope = C.scope
    rloc = ExitStack()
    C.scope = rloc
    C.posi = sb("posi", [32, SEQ], I32)
    C.ang = sb("ang", [32, SEQ])
    S.dma("sp", lambda e: e.dma_start(out=C.posi[:], in_=I.pos[s:s + 1, :].partition_broadcast(32)), w=[C.rope_t])
    S.op("dve", lambda e: e.tensor_copy(out=C.ang[:], in_=C.posi[:]), r=[C.rope_t], w=[C.rope_t])
    S.op("dve", lambda e: e.tensor_scalar(out=C.ang[:], in0=C.ang[:], scalar1=K.ropec[0:32, 0:1], scalar2=None,
                                          op0=ALU.mult), r=[C.rope_t, K.t], w=[C.rope_t])
    PI = float(np.pi)
    TWO_PI = 2.0 * PI
    PI_LO = 3.1415925
    yy = C.sb("rope_y", [32, SEQ])
    for dst, sh in ((C.sinT, PI), (C.cosT, PI + PI / 2)):
        rt = [C.rope_t]
        S.op("dve", lambda e: e.tensor_scalar(out=yy[:], in0=C.ang[:], scalar1=sh, scalar2=None, op0=ALU.add), r=rt, w=rt)
        S.op("dve", lambda e: e.tensor_scalar(out=C.posi[:], in0=yy[:], scalar1=1.0 / TWO_PI, scalar2=None,
                                              op0=ALU.mult), r=rt, w=rt)
        S.op("dve", lambda e: e.tensor_copy(out=dst[:], in_=C.posi[:]), r=rt, w=rt)
        S.op("dve", lambda e: e.scalar_tensor_tensor(out=yy[:], in0=dst[:], scalar=-TWO_PI, in1=yy[:],
                                                     op0=ALU.mult, op1=ALU.add), r=rt, w=rt)
        S.op("dve", lambda e: e.tensor_scalar(out=dst[:], in0=yy[:], scalar1=0.0, scalar2=None, op0=ALU.is_lt), r=rt, w=rt)
        S.op("dve", lambda e: e.scalar_tensor_tensor(out=yy[:], in0=dst[:], scalar=TWO_PI, in1=yy[:],
                                                     op0=ALU.mult, op1=ALU.add), r=rt, w=rt)
        S.op("dve", lambda e: e.tensor_scalar(out=yy[:], in0=yy[:], scalar1=-PI, scalar2=None, op0=ALU.add), r=rt, w=rt)
        S.op("dve", lambda e: e.tensor_scalar(out=yy[:], in0=yy[:], scalar1=PI_LO, scalar2=-PI_LO,
                                              op0=ALU.min, op1=ALU.max), r=rt, w=rt)
        S.op("act", lambda e: e.activation(out=dst[:], in_=yy[:], func=AF.Sin), r=rt, w=rt)
    S.op("dve", lambda e: e.tensor_scalar(out=C.sinT[:], in0=C.sinT[:], scalar1=K.ropec[0:32, 1:2], scalar2=None,
                                          op0=ALU.mult), r=[C.rope_t, K.t], w=[C.rope_t])
    S.barrier()
    rloc.close()
    C.scope = outer_scope


def phase_attn(C, s):
    nc, S, sb, I, K = C.nc, C.S, C.sb, C.I, C.K
    A = Ctx()
    C.A = A
    A.w = [[sb(f"aw{b}_{i}", [128, 8, 128], BF16) for i in range(9)] for b in range(2)]
    A.w_t = [[T(f"aw{b}_{i}") for i in range(9)] for b in range(2)]
    A.qk = [[sb(f"qk{b}_{i}", [128, SEQ], BF16) for i in range(6)] for b in range(2)]
    A.qk_t = [[T(f"qk{b}_{i}") for i in range(6)] for b in range(2)]
    A.v = [[sb(f"v{b}_{i}", [128, 16, 128], BF16) for i in range(3)] for b in range(2)]
    A.v_t = [[T(f"v{b}_{i}") for i in range(3)] for b in range(2)]
    A.acc = sb("attacc", [128, 2, SEQ])
    A.acc_t = T("attacc")
    A.pT = [sb(f"pT{i}", [128, 512], BF16) for i in range(2)]
    A.pT_t = [T(f"pT{i}") for i in range(2)]
    A.rt = [sb(f"ropetmp{i}", [32, 512]) for i in range(2)]
    A.rt_t = T("ropetmp")
    A.ni = 0
    A.nb = 0
    scale = 1.0 / float(np.sqrt(128.0))

    def gen_inproj(hs, bs):
        W, W_t, QK, QK_t, V, V_t = A.w[bs], A.w_t[bs], A.qk[bs], A.qk_t[bs], A.v[bs], A.v_t[bs]
        for i in range(9):
            base = (Q0, K0, V0)[i // 3] + ((i % 3) * 4 + hs) * 128
            S.dma("pool", lambda e, i=i, base=base: e.dma_start(
                out=W[i][:], in_=I.w_in[:, base:base + 128].rearrange("(k p) n -> p k n", p=128)), w=[W_t[i]])
        for i in range(6):
            for tg in range(4):
                A.ni += 1
                pq, pqt = C.ps[A.ni % 2], C.pst[A.ni % 2]
                psw, pswt = C.ps[2 + A.ni % 2], C.pst[2 + A.ni % 2]
                tsl = slice(tg * 512, (tg + 1) * 512)
                S.mm([lambda e, k=k: e.matmul(pq[:, :], lhsT=W[i][:, k, :], rhs=C.uT[:, k, tsl],
                                               start=(k == 0), stop=(k == 7)) for k in range(8)],
                     r=[W_t[i], C.uT_t[tg]], w=[pqt])
                S.op("act", lambda e: e.copy(out=QK[i][:, tsl], in_=pq[:, :]), r=[pqt], w=[QK_t[i]])
                S.mm([lambda e: e.matmul(psw[0:32, :], lhsT=K.p32[0:32, 0:32], rhs=QK[i][0:32, tsl],
                                         start=True, stop=True)], r=[QK_t[i], K.t], w=[pswt])
                t0, t1 = A.rt
                S.op("dve", lambda e: e.tensor_tensor(out=t0[:], in0=psw[0:32, :], in1=C.sinT[:, tsl], op=ALU.mult),
                     r=[pswt, C.rope_t], w=[A.rt_t])
                S.op("dve", lambda e: e.tensor_tensor(out=t1[:], in0=pq[0:32, :], in1=C.cosT[:, tsl], op=ALU.mult),
                     r=[pqt, C.rope_t], w=[A.rt_t])
                S.op("dve", lambda e: e.tensor_tensor(out=QK[i][0:32, tsl], in0=t0[:], in1=t1[:], op=ALU.add),
                     r=[A.rt_t], w=[QK_t[i], A.rt_t])
                yield
        for g, d in enumerate((1, 4, 16)):
            nb = 16 // d
            for j0 in range(0, 16, 4):
                A.ni += 1
                pv, pvt = C.ps[A.ni % 2], C.pst[A.ni % 2]
                for jj in range(4):
                    j = j0 + jj
                    r_, n_ = j // nb, j % nb
                    sel = qsel(d, r_, n_)
                    S.mm([lambda e, k=k: e.matmul(pv[:, jj * 128:(jj + 1) * 128], lhsT=C.uT[:, k, sel],
                                                   rhs=W[6 + g][:, k, :], start=(k == 0), stop=(k == 7))
                          for k in range(8)], r=[W_t[6 + g]] + C.uT_t, w=[pvt])
                S.op("act", lambda e: e.copy(out=V[g][:, j0:j0 + 4, :].rearrange("p j e -> p (j e)"), in_=pv[:, :]),
                     r=[pvt], w=[V_t[g]])
                yield

    def gen_blocks(hs, bs):
        QK, QK_t, V, V_t = A.qk[bs], A.qk_t[bs], A.v[bs], A.v_t[bs]
        blocks = []
        for g, d in enumerate((1, 4, 16)):
            nb = 16 // d
            for r_ in range(d):
                for n_ in range(nb):
                    blocks.append((g, d, nb, r_, n_))
        for b0 in range(0, len(blocks), 2):
            pair = blocks[b0:b0 + 2]
            g = pair[0][0]
            assert all(p[0] == g for p in pair)
            qT, kT, qt_, kt_ = QK[g], QK[3 + g], QK_t[g], QK_t[3 + g]
            A.nb += 1
            ps_s, ps_st = C.ps[4 + A.nb % 2], C.pst[4 + A.nb % 2]
            ps_o, ps_ot = C.ps[6 + A.nb % 2], C.pst[6 + A.nb % 2]
            pT, pTt = A.pT[A.nb % 2], A.pT_t[A.nb % 2]
            fns = []
            metas = []
            for pi, (g_, d, nb, r_, n_) in enumerate(pair):
                qs = qsel(d, r_, n_)
                kbs = [kb for kb in (n_ - 1, n_) if kb >= 0]
                c0 = pi * 256 + 256 - 128 * len(kbs)
                for ci, kb in enumerate(kbs):
                    cs = slice(c0 + ci * 128, c0 + (ci + 1) * 128)
                    ks = qsel(d, r_, kb)
                    msk = K.mcur if kb == n_ else K.mprev
                    fns.append(lambda e, cs=cs, ks=ks, qs=qs: e.matmul(ps_s[:, cs], lhsT=kT[:, ks], rhs=qT[:, qs],
                                                                       start=True, stop=False))
                    fns.append(lambda e, cs=cs, msk=msk: e.matmul(ps_s[:, cs], lhsT=K.ident_b[:], rhs=msk[:],
                                                                  start=False, stop=True))
                metas.append((d, nb, r_, n_, qs, kbs, c0))
            S.mm(fns, r=[qt_, kt_, K.t], w=[ps_st])
            lo0, hi0 = metas[0][6], 256
            lo1, hi1 = metas[1][6], 512
            rngs = [(lo0, hi1)] if lo1 == 256 else [(lo0, hi0), (lo1, hi1)]
            for lo, hi in rngs:
                S.op("act", lambda e, lo=lo, hi=hi: e.activation(out=pT[:, lo:hi], in_=ps_s[:, lo:hi], func=AF.Exp, scale=scale),
                     r=[ps_st], w=[pTt])
            fns = []
            for pi, (d, nb, r_, n_, qs, kbs, c0) in enumerate(metas):
                ob = pi * 256
                for ci, kb in enumerate(kbs):
                    cs = slice(c0 + ci * 128, c0 + (ci + 1) * 128)
                    vj = r_ * nb + kb
                    fns.append(lambda e, cs=cs, vj=vj, ci=ci, ob=ob, nk=len(kbs): e.matmul(
                        ps_o[:, ob:ob + 128], lhsT=V[g][:, vj, :], rhs=pT[:, cs], start=(ci == 0), stop=(ci == nk - 1)))
                for ci, kb in enumerate(kbs):
                    cs = slice(c0 + ci * 128, c0 + (ci + 1) * 128)
                    fns.append(lambda e, cs=cs, ci=ci, ob=ob, nk=len(kbs): e.matmul(
                        ps_o[:, ob + 128:ob + 256], lhsT=K.ones_b[:], rhs=pT[:, cs], start=(ci == 0), stop=(ci == nk - 1)))
            S.mm(fns, r=[pTt, V_t[g], K.t], w=[ps_ot])
            for pi, (d, nb, r_, n_, qs, kbs, c0) in enumerate(metas):
                src = ps_o[:, pi * 256:(pi + 1) * 256].rearrange("p (a q) -> p a q", a=2)
                if g == 0:
                    S.op("act", lambda e, src=src, qs=qs: e.copy(out=A.acc[:, :, qs], in_=src), r=[ps_ot], w=[A.acc_t])
                else:
                    S.op("dve", lambda e, src=src, qs=qs: e.tensor_tensor(out=A.acc[:, :, qs], in0=src, in1=A.acc[:, :, qs], op=ALU.add),
                         r=[ps_ot, A.acc_t], w=[A.acc_t])
            yield
        S.op("dve", lambda e: e.reciprocal(out=A.acc[:, 1, :], in_=A.acc[:, 1, :]), r=[A.acc_t], w=[A.acc_t])
        S.op("dve", lambda e: e.tensor_tensor(out=C.attnT[:, hs, :], in0=A.acc[:, 0, :], in1=A.acc[:, 1, :], op=ALU.mult),
             r=[A.acc_t], w=[C.attnT_t])

    for _ in gen_inproj(0, 0):
        pass
    for hs in range(4):
        gb = gen_blocks(hs, hs % 2)
        gi = gen_inproj(hs + 1, (hs + 1) % 2) if hs + 1 < 4 else iter(())
        done_b = done_i = False
        step = 0
        while not (done_b and done_i):
            step += 1
            if not done_b:
                try:
                    next(gb)
                except StopIteration:
                    done_b = True
            for _rep in range(2 if step % 2 == 0 else 1):
                if not done_i:
                    try:
                        next(gi)
                    except StopIteration:
                        done_i = True


def phase_ssd(C, s):
    nc, S, sb, I, K, M = C.nc, C.S, C.sb, C.I, C.K, C.M
    ps, pst = C.ps, C.pst
    tri_f, sl_f = K.cm_f[:, 3, :], K.cm_f[:, 4, :]
    tw = T("ssd_w", multi=True)
    wdt = sb("wdt", [128, 8, 16], BF16)
    S.dma("pool", lambda e: e.dma_start(out=wdt[:], in_=I.w_in[:, DT0:DT0 + 16].rearrange("(k p) n -> p k n", p=128)), w=[tw])
    tc = T("ssd_c")
    convw = sb("convw", [128, 16, 4])
    convb = sb("convb", [128, 16])
    snwT = sb("snwT", [128, 8])
    hb16 = sb("hb16", [128, 3, 16])
    D_bc = sb("D_bc", [128, 16, 64])
    S.dma("sp", lambda e: e.dma_start(out=convw[:], in_=I.conv_wT), w=[tc])
    S.dma("sp", lambda e: e.dma_start(out=convb[:], in_=I.conv_bT), w=[tc])
    S.dma("sp", lambda e: e.dma_start(out=snwT[:], in_=I.snwT), w=[tc])
    S.dma("sp", lambda e: e.dma_start(out=hb16[:].rearrange("p a h -> p (a h)"), in_=I.hrow.partition_broadcast(128)), w=[tc])
    S.op("act", lambda e: e.activation(out=hb16[:, 1, :], in_=hb16[:, 1, :], func=AF.Exp), r=[tc], w=[tc])
    S.op("dve", lambda e: e.tensor_scalar(out=hb16[:, 1, :], in0=hb16[:, 1, :], scalar1=-1.0, scalar2=None, op0=ALU.mult),
         r=[tc], w=[tc])
    S.op("dve", lambda e: e.tensor_copy(out=D_bc[:], in_=hb16[:, 2, :].unsqueeze(2).to_broadcast([128, 16, 64])), r=[tc], w=[tc])
    nws = [0]
    halo = sb("halo", [128, 16, 3], BF16)
    halo_t = T("halo")
    S.op("dve", lambda e: e.memset(halo[:], 0.0), w=[halo_t])
    cdiag = sb("cdiag", [128, 16, 4, 128], BF16)
    cdiag_t = T("cdiag", multi=True)
    for c in range(16):
        for j in range(4):
            S.op("dve", lambda e, c=c, j=j: e.tensor_scalar(out=cdiag[:, c, j, :], in0=K.ident_f[:], scalar1=convw[:, c, j:j + 1],
                                                            scalar2=None, op0=ALU.mult), r=[tc, K.t], w=[cdiag_t])
    Hs = sb("Hs", [128, 16, 64])
    Hb = sb("Hb", [128, 16, 64], BF16)
    H_t = T("H")
    xbc = sb("xbc", [128, 16, 512], BF16)
    xbc_t = T("xbc")
    yT = sb("yT", [128, 8, 512], BF16)
    yT_t = T("yT")
    nb = [0]
    ssd_scope = C.scope
    W = {}

    def load_w(col0):
        i = nws[0] % 2
        nws[0] += 1
        wst, wst_t = W["wst"], W["wst_t"]
        S.dma("pool", lambda e: e.dma_start(out=wst[i][:], in_=I.w_in[:, col0:col0 + 512].rearrange("(k p) n -> p k n", p=128)),
              w=[wst_t[i]])
        return wst[i], wst_t[i]

    for tg in range(4):
        tsl = slice(tg * 512, (tg + 1) * 512)
        ut = C.uT_t[tg]
        loc = ExitStack()
        C.scope = loc
        W["wst"] = [sb(f"wst{i}", [128, 8, 512], BF16) for i in range(2)]
        W["wst_t"] = [T(f"wst{i}") for i in range(2)]
        pre = [sb(f"pre{i}", [128, 516], BF16) for i in range(2)]
        pre_t = [T(f"pre{i}") for i in range(2)]
        for c in range(16):
            if c % 4 == 0:
                wx, wxt = load_w(X0 + c * 128)
            nb[0] += 1
            pq, pqt = ps[nb[0] % 2], pst[nb[0] % 2]
            pc, pct = ps[2 + nb[0] % 2], pst[2 + nb[0] % 2]
            pr, prt = pre[nb[0] % 2], pre_t[nb[0] % 2]
            S.mm([lambda e, k=k: e.matmul(pq[:, :], lhsT=wx[:, k, (c % 4) * 128:(c % 4 + 1) * 128], rhs=C.uT[:, k, tsl],
                                           start=(k == 0), stop=(k == 7)) for k in range(8)], r=[wxt, ut], w=[pqt])
            S.op("act", lambda e: e.copy(out=pr[:, 3:515], in_=pq[:, :]), r=[pqt], w=[prt])
            S.op("act", lambda e: e.copy(out=pr[:, 0:3], in_=halo[:, c, :]), r=[halo_t], w=[prt])
            S.mm([lambda e, j=j: e.matmul(pc[:, :], lhsT=cdiag[:, c, j, :], rhs=pr[:, j:j + 512], start=(j == 0), stop=(j == 3))
                  for j in range(4)], r=[prt, cdiag_t], w=[pct])
            S.op("act", lambda e: e.copy(out=halo[:, c, :], in_=pr[:, 512:515]), r=[prt], w=[halo_t])
            S.op("act", lambda e: e.activation(out=xbc[:, c, :], in_=pc[:, :], func=AF.Silu, bias=convb[:, c:c + 1]),
                 r=[pct, tc], w=[xbc_t])
        S.barrier()
        loc.close()
        loc = ExitStack()
        C.scope = loc
        wz = sb("wz", [128, 8, D], BF16)
        twz = T("wz")
        S.dma("pool", lambda e: e.dma_start(out=wz[:], in_=I.w_in[:, Z0:Z0 + D].rearrange("(k p) n -> p k n", p=128)), w=[twz])
        dts = sb("dts", [128, 4, 4, 16])
        dts_t = T("dts")
        sm2 = [sb(f"ssd_small{i}", [128, 6, 16]) for i in range(2)]
        sm2_t = [T(f"ssd_small{i}") for i in range(2)]
        Lf = sb("Lf", [128, 16, 128])
        Lf_t = T("Lf")
        cbTm = sb("cbTm", [128, 4, 128])
        cbTm_t = T("cbTm")
        dec = [sb(f"dec{i}", [128, 4, 128], BF16) for i in range(2)]
        dec_t = [T(f"dec{i}") for i in range(2)]
        MT2 = [sb(f"MT{i}", [128, 16, 128], BF16) for i in range(2)]
        MT2_t = [T(f"MT{i}") for i in range(2)]
        xdt2 = [sb(f"xdt{i}", [128, 16, 64], BF16) for i in range(2)]
        xsD2 = [sb(f"xsD{i}", [128, 16, 64]) for i in range(2)]
        xdd2 = [sb(f"xdd{i}", [128, 16, 64], BF16) for i in range(2)]
        xd2_t = [T(f"xd{i}") for i in range(2)]
        Bt2 = [sb(f"Bt{i}", [128, 4, 128], BF16) for i in range(2)]
        Bt2_t = [T(f"Bt{i}") for i in range(2)]
        t1 = sb("t1", [128, 16, 64])
        t1_t = T("t1")
        yb = sb("yb", [128, D])
        yb_t = T("yb")
        sz2 = [sb(f"sz{i}", [128, D]) for i in range(2)]
        sz2_t = [T(f"sz{i}") for i in range(2)]
        ysq = sb("ysq", [128, 2, 4])
        yn = sb("yn", [128, D], BF16)
        yn_t = T("yn")
        pd, pdt = ps[2], pst[2]
        for ti in range(4):
            tok = slice(tg * 512 + ti * 128, tg * 512 + (ti + 1) * 128)
            S.mm([lambda e, k=k: e.matmul(pd[:, ti * 16:(ti + 1) * 16], lhsT=C.uT[:, k, tok], rhs=wdt[:, k, :],
                                           start=(k == 0), stop=(k == 7)) for k in range(8)], r=[tw, ut], w=[pdt])
        dt4 = [dts_t]
        S.op("dve", lambda e: e.tensor_tensor(out=dts[:, :, 0, :], in0=pd[:, 0:64].rearrange("p (t h) -> p t h", h=16),
                                              in1=hb16[:, 0, :].unsqueeze(1).to_broadcast([128, 4, 16]), op=ALU.add),
             r=[pdt, tc], w=dt4)
        S.op("act", lambda e: e.activation(out=dts[:, :, 1, :], in_=dts[:, :, 0, :], func=AF.Abs), r=dt4, w=dt4)
        S.op("act", lambda e: e.activation(out=dts[:, :, 1, :], in_=dts[:, :, 1, :], func=AF.Exp, scale=-1.0), r=dt4, w=dt4)
        S.op("dve", lambda e: e.tensor_scalar(out=dts[:, :, 1, :], in0=dts[:, :, 1, :], scalar1=1.0, scalar2=None, op0=ALU.add),
             r=dt4, w=dt4)
        S.op("act", lambda e: e.activation(out=dts[:, :, 1, :], in_=dts[:, :, 1, :], func=AF.Ln), r=dt4, w=dt4)
        S.op("dve", lambda e: e.scalar_tensor_tensor(out=dts[:, :, 2, :], in0=dts[:, :, 0, :], scalar=0.0, in1=dts[:, :, 1, :],
                                                     op0=ALU.max, op1=ALU.add), r=dt4, w=dt4)
        S.op("dve", lambda e: e.tensor_tensor(out=dts[:, :, 3, :], in0=dts[:, :, 2, :],
                                              in1=hb16[:, 1, :].unsqueeze(1).to_broadcast([128, 4, 16]), op=ALU.mult),
             r=dt4 + [tc], w=dt4)
        def chunk_front(ci):
                csl = slice(ci * 128, (ci + 1) * 128)
                tok = slice(tg * 512 + ci * 128, tg * 512 + (ci + 1) * 128)
                first = (tg == 0 and ci == 0)
                a_ = dts[:, ci, 3, :]
                dt_ = dts[:, ci, 2, :]
                par = ci % 2
                sm, sm_t = sm2[par], sm2_t[par]
                MT, MT_t = MT2[par], MT2_t[par]
                xdt, xsD, xdd, xd_t = xdt2[par], xsD2[par], xdd2[par], xd2_t[par]
                Bt, Bt_t = Bt2[par], Bt2_t[par]
                sz, sz_t = sz2[par], sz2_t[par]
                smt = [sm_t]
                xdw = [xd_t]
                p0, p0t = ps[0], pst[0]
                S.mm([lambda e: e.matmul(p0[:, 0:16], lhsT=tri_f, rhs=a_, start=True, stop=True),
                      lambda e: e.matmul(p0[:, 16:32], lhsT=K.ones_f[:], rhs=a_, start=True, stop=True)],
                     r=[K.t, dts_t], w=[p0t])
                smt = [sm_t]
                S.op("act", lambda e: e.copy(out=sm[:, 0, :], in_=p0[:, 0:16]), r=[p0t], w=smt)
                S.op("dve", lambda e: e.tensor_tensor(out=sm[:, 1, :], in0=p0[:, 16:32], in1=sm[:, 0, :], op=ALU.subtract),
                     r=[p0t] + smt, w=smt)
                S.op("act", lambda e: e.activation(out=sm[:, 2, :], in_=sm[:, 0, :], func=AF.Exp), r=smt, w=smt)
                S.op("act", lambda e: e.activation(out=sm[:, 3, :], in_=sm[:, 1, :], func=AF.Exp), r=smt, w=smt)
                S.op("act", lambda e: e.activation(out=sm[:, 4, :], in_=p0[:, 16:32], func=AF.Exp), r=[p0t] + smt, w=smt)
                S.op("dve", lambda e: e.tensor_tensor(out=Lf[:], in0=sl_f.unsqueeze(1).to_broadcast([128, 16, 128]),
                                                      in1=a_.unsqueeze(2).to_broadcast([128, 16, 128]), op=ALU.mult),
                     r=[K.t, dts_t], w=[Lf_t])
                p1, p1t = ps[1], pst[1]
                S.mm([lambda e, g=g: e.matmul(p1[:, g * 128:(g + 1) * 128], lhsT=xbc[:, 8 + g, csl], rhs=xbc[:, 12 + g, csl],
                                               start=True, stop=True) for g in range(4)], r=[xbc_t], w=[p1t])
                S.op("dve", lambda e: e.tensor_tensor(out=cbTm[:], in0=p1[:, :].rearrange("p (g l) -> p g l", g=4),
                                                      in1=tri_f.unsqueeze(1).to_broadcast([128, 4, 128]), op=ALU.mult),
                     r=[p1t, K.t], w=[cbTm_t])
                p2, p2t = ps[2], pst[2]
                p3, p3t = ps[3], pst[3]
                p2b = p2[:, :].bitcast(BF16)
                p3b = p3[:, :].bitcast(BF16)
                S.mm([lambda e, k=k: e.transpose(out=p2b[:, k * 128:(k + 1) * 128], in_=xbc[:, k, csl], identity=K.ident_b[:])
                      for k in range(8)], r=[xbc_t, K.t], w=[p2t])
                S.mm([lambda e, g=g: e.transpose(out=p3b[:, g * 128:(g + 1) * 128], in_=xbc[:, 8 + g, csl], identity=K.ident_b[:])
                      for g in range(4)], r=[xbc_t, K.t], w=[p3t])
                xsT = p2b.rearrange("p (h e) -> p h e", h=16)
                xdw = [xd_t]
                S.op("dve", lambda e: e.tensor_tensor(out=xdt[:], in0=xsT, in1=dt_.unsqueeze(2).to_broadcast([128, 16, 64]),
                                                      op=ALU.mult), r=[p2t, dts_t], w=xdw)
                S.op("dve", lambda e: e.tensor_tensor(out=xsD[:], in0=xsT, in1=D_bc[:], op=ALU.mult), r=[p2t, tc], w=xdw)
                S.op("dve", lambda e: e.tensor_tensor(out=xdd[:], in0=xdt[:], in1=sm[:, 3, :].unsqueeze(2).to_broadcast([128, 16, 64]),
                                                      op=ALU.mult), r=xdw + smt, w=xdw)
                S.op("act", lambda e: e.copy(out=Bt[:].rearrange("p g n -> p (g n)"), in_=p3b[:, 0:512]), r=[p3t], w=[Bt_t])
                for g in range(4):
                    pdx, pdxt = ps[4 + g % 2], pst[4 + g % 2]
                    S.mm([lambda e, hl=hl: e.matmul(pdx[:, hl * 128:(hl + 1) * 128], lhsT=Lf[:, g * 4 + hl, :], rhs=tri_f,
                                                     start=True, stop=True) for hl in range(4)], r=[Lf_t, K.t], w=[pdxt])
                    dc, dct = dec[g % 2], dec_t[g % 2]
                    S.op("act", lambda e: e.activation(out=dc[:].rearrange("p h l -> p (h l)"), in_=pdx[:, :], func=AF.Exp),
                         r=[pdxt], w=[dct])
                    S.op("dve", lambda e: e.tensor_tensor(out=MT[:, g * 4:(g + 1) * 4, :], in0=dc[:],
                                                          in1=cbTm[:, g, :].unsqueeze(1).to_broadcast([128, 4, 128]), op=ALU.mult),
                         r=[dct, cbTm_t], w=[MT_t])

                pz = (ps[6], ps[7])
                pzt = [pst[6], pst[7]]
                for hf in range(2):
                    S.mm([lambda e, k=k, hf=hf: e.matmul(pz[hf][:, :], lhsT=C.uT[:, k, tok], rhs=wz[:, k, hf * 512:(hf + 1) * 512],
                                                          start=(k == 0), stop=(k == 7)) for k in range(8)], r=[twz, ut], w=[pzt[hf]])
                    S.op("act", lambda e, hf=hf: e.activation(out=sz[:, hf * 512:(hf + 1) * 512], in_=pz[hf][:, :], func=AF.Silu),
                         r=[pzt[hf]], w=[sz_t])

        def chunk_back(ci):
                csl = slice(ci * 128, (ci + 1) * 128)
                tok = slice(tg * 512 + ci * 128, tg * 512 + (ci + 1) * 128)
                first = (tg == 0 and ci == 0)
                a_ = dts[:, ci, 3, :]
                dt_ = dts[:, ci, 2, :]
                par = ci % 2
                sm, sm_t = sm2[par], sm2_t[par]
                MT, MT_t = MT2[par], MT2_t[par]
                xdt, xsD, xdd, xd_t = xdt2[par], xsD2[par], xdd2[par], xd2_t[par]
                Bt, Bt_t = Bt2[par], Bt2_t[par]
                sz, sz_t = sz2[par], sz2_t[par]
                smt = [sm_t]
                xdw = [xd_t]
                py = (ps[6], ps[7])
                pyt = [pst[6], pst[7]]
                S.mm([lambda e, h=h: e.matmul(py[h // 8][:, (h % 8) * 64:(h % 8 + 1) * 64], lhsT=MT[:, h, :], rhs=xdt[:, h, :],
                                               start=True, stop=True) for h in range(16)], r=[MT_t, xd_t], w=pyt)
                po = (ps[0], ps[1])
                pot = [pst[0], pst[1]]
                if not first:
                    S.mm([lambda e, g=g: e.matmul(po[g // 2][:, (g % 2) * 256:(g % 2 + 1) * 256], lhsT=xbc[:, 12 + g, csl],
                                                   rhs=Hb[:, g * 4:(g + 1) * 4, :].rearrange("p h e -> p (h e)"),
                                                   start=True, stop=True) for g in range(4)], r=[xbc_t, H_t], w=pot)
                    for hf in range(2):
                        S.op("dve", lambda e, hf=hf: e.tensor_tensor(
                            out=t1[:, hf * 8:(hf + 1) * 8, :], in0=po[hf][:, :].rearrange("p (h e) -> p h e", h=8),
                            in1=sm[:, 2, hf * 8:(hf + 1) * 8].unsqueeze(2).to_broadcast([128, 8, 64]), op=ALU.mult),
                            r=[pot[hf]] + smt, w=[t1_t])
                    S.op("dve", lambda e: e.tensor_tensor(out=t1[:], in0=t1[:], in1=xsD[:], op=ALU.add), r=[t1_t, xd_t], w=[t1_t])
                    tsrc = t1
                else:
                    tsrc = xsD
                for hf in range(2):
                    S.op("dve", lambda e, hf=hf: e.tensor_tensor(
                        out=yb[:, hf * 512:(hf + 1) * 512], in0=py[hf][:, :],
                        in1=tsrc[:, hf * 8:(hf + 1) * 8, :].rearrange("p h e -> p (h e)"), op=ALU.add),
                        r=[pyt[hf], t1_t, xd_t], w=[yb_t])
                pS = (ps[2], ps[3])
                pSt = [pst[2], pst[3]]
                S.mm([lambda e, g=g: e.matmul(pS[g // 2][:, (g % 2) * 256:(g % 2 + 1) * 256], lhsT=Bt[:, g, :],
                                               rhs=xdd[:, g * 4:(g + 1) * 4, :].rearrange("p h e -> p (h e)"),
                                               start=True, stop=True) for g in range(4)], r=[Bt_t, xd_t], w=pSt)
                if not first:
                    S.op("dve", lambda e: e.tensor_tensor(out=Hs[:], in0=Hs[:], in1=sm[:, 4, :].unsqueeze(2).to_broadcast([128, 16, 64]),
                                                          op=ALU.mult), r=smt + [H_t], w=[H_t])
                    for hf in range(2):
                        S.op("dve", lambda e, hf=hf: e.tensor_tensor(
                            out=Hs[:, hf * 8:(hf + 1) * 8, :], in0=pS[hf][:, :].rearrange("p (h e) -> p h e", h=8),
                            in1=Hs[:, hf * 8:(hf + 1) * 8, :], op=ALU.add), r=[pSt[hf], H_t], w=[H_t])
                else:
                    for hf in range(2):
                        S.op("act", lambda e, hf=hf: e.copy(out=Hs[:, hf * 8:(hf + 1) * 8, :].rearrange("p h e -> p (h e)"),
                                                            in_=pS[hf][:, :]), r=[pSt[hf]], w=[H_t])
                S.op("act", lambda e: e.copy(out=Hb[:], in_=Hs[:]), r=[H_t], w=[H_t])
                S.op("dve", lambda e: e.tensor_tensor(out=yb[:], in0=yb[:], in1=sz[:], op=ALU.mult), r=[yb_t, sz_t], w=[yb_t])
                for g in range(4):
                    S.op("act", lambda e, g=g: e.activation(out=sz[:, g * 256:(g + 1) * 256], in_=yb[:, g * 256:(g + 1) * 256],
                                                            func=AF.Square, accum_out=ysq[:, 0, g:g + 1]), r=[yb_t], w=[sz_t])
                S.op("dve", lambda e: e.tensor_scalar(out=ysq[:, 1, :], in0=ysq[:, 0, :], scalar1=1.0 / 256, scalar2=EPS,
                                                      op0=ALU.mult, op1=ALU.add), r=[sz_t], w=[sz_t])
                S.op("act", lambda e: e.sqrt(out=ysq[:, 1, :], in_=ysq[:, 1, :]), r=[sz_t], w=[sz_t])
                S.op("dve", lambda e: e.reciprocal(out=ysq[:, 1, :], in_=ysq[:, 1, :]), r=[sz_t], w=[sz_t])
                S.op("dve", lambda e: e.tensor_tensor(out=yn[:].rearrange("p (g c) -> p g c", g=4),
                                                      in0=yb[:].rearrange("p (g c) -> p g c", g=4),
                                                      in1=ysq[:, 1, :].unsqueeze(2).to_broadcast([128, 4, 256]), op=ALU.mult),
                     r=[yb_t, sz_t], w=[yn_t])
                pT_, pTt = ps[0], pst[0]
                pTb = pT_[:, :].bitcast(BF16)
                S.mm([lambda e, k=k: e.transpose(out=pTb[:, k * 128:(k + 1) * 128], in_=yn[:, k * 128:(k + 1) * 128],
                                                  identity=K.ident_b[:]) for k in range(8)], r=[yn_t, K.t], w=[pTt])
                for k in range(8):
                    S.op("act", lambda e, k=k: e.activation(out=yT[:, k, csl], in_=pTb[:, k * 128:(k + 1) * 128], func=AF.Copy,
                                                            scale=snwT[:, k:k + 1]), r=[pTt, tc], w=[yT_t])

        chunk_front(0)
        for ci in range(4):
            if ci + 1 < 4:
                chunk_front(ci + 1)
            chunk_back(ci)
        if "ssm" in C.dbg:
            dump(C, f"yT{s}_{tg}", yT[:].rearrange("p k t -> p (k t)"), [128, 8 * 512], BF16, [yT_t])
            dump(C, f"xbc{s}_{tg}", xbc[:].rearrange("p k t -> p (k t)"), [128, 16 * 512], BF16, [xbc_t])
        S.barrier()
        loc.close()
        loc = ExitStack()
        C.scope = loc
        W["wst"] = [sb(f"wst{i}", [128, 8, 512], BF16) for i in range(2)]
        W["wst_t"] = [T(f"wst{i}") for i in range(2)]
        wba = sb("wba", [128, 4, D], BF16)
        wbs = sb("wbs", [128, 8, D], BF16)
        wout = sb("wout", [128, 8, D], BF16)
        gpre = [load_w(G0), load_w(G0 + 512)]
        S.dma("pool", lambda e: e.dma_start(out=wba[:], in_=I.w_ba.rearrange("(k p) n -> p k n", p=128)), w=[tw])
        S.dma("pool", lambda e: e.dma_start(out=wbs[:], in_=I.w_bs.rearrange("(k p) n -> p k n", p=128)), w=[tw])
        S.dma("pool", lambda e: e.dma_start(out=wout[:], in_=I.w_out.rearrange("(k p) n -> p k n", p=128)), w=[tw])
        sg = sb("sg", [128, 16, 512], BF16)
        sg_t = T("sg")
        m1 = [sb(f"m1{i}", [128, 512]) for i in range(1)]
        m1_t = [T(f"m1{i}") for i in range(1)]
        mgT = sb("mgT", [128, 8, 512], BF16)
        mgT_t = T("mgT")
        xr = [sb(f"xr{i}", [128, D]) for i in range(2)]
        xr_t = [T(f"xr{i}") for i in range(2)]
        hh = [sb(f"hh{i}", [128, D]) for i in range(2)]
        hh_t = [T(f"hh{i}") for i in range(2)]
        for c in range(16):
            if c % 4 == 0:
                wg, wgt = gpre[c // 4]
            nb[0] += 1
            pq, pqt = ps[4 + nb[0] % 2], pst[4 + nb[0] % 2]
            S.mm([lambda e, k=k: e.matmul(pq[:, :], lhsT=wg[:, k, (c % 4) * 128:(c % 4 + 1) * 128], rhs=C.uT[:, k, tsl],
                                           start=(k == 0), stop=(k == 7)) for k in range(8)], r=[wgt, ut], w=[pqt])
            S.op("act", lambda e: e.activation(out=sg[:, c, :], in_=pq[:, :], func=AF.Sigmoid), r=[pqt], w=[sg_t])
            if c % 4 == 3 and c // 4 + 2 < 4:
                gpre.append(load_w(G0 + (c // 4 + 2) * 512))
        for dc in range(8):
            nb[0] += 1
            pa, pat = ps[nb[0] % 2], pst[nb[0] % 2]
            pb_, pbt = ps[2 + nb[0] % 2], pst[2 + nb[0] % 2]
            mm1, mm1t = m1[0], m1_t[0]
            S.mm([lambda e, k=k: e.matmul(pa[:, :], lhsT=wba[:, k, dc * 128:(dc + 1) * 128], rhs=C.attnT[:, k, tsl],
                                           start=(k == 0), stop=(k == 3)) for k in range(4)], r=[tw, C.attnT_t], w=[pat])
            S.mm([lambda e, k=k: e.matmul(pb_[:, :], lhsT=wbs[:, k, dc * 128:(dc + 1) * 128], rhs=yT[:, k, :],
                                           start=(k == 0), stop=(k == 7)) for k in range(8)], r=[tw, yT_t], w=[pbt])
            S.op("dve", lambda e: e.tensor_tensor(out=mm1[:], in0=pa[:, :], in1=sg[:, dc, :], op=ALU.mult),
                 r=[pat, sg_t], w=[mm1t])
            S.op("dve", lambda e: e.tensor_tensor(out=mgT[:, dc, :], in0=pb_[:, :], in1=sg[:, 8 + dc, :], op=ALU.mult),
                 r=[pbt, sg_t], w=[mgT_t])
            S.op("dve", lambda e: e.tensor_tensor(out=mgT[:, dc, :], in0=mgT[:, dc, :], in1=mm1[:], op=ALU.add),
                 r=[mm1t, mgT_t], w=[mgT_t])
        if "mg" in C.dbg:
            dump(C, f"mgT{s}_{tg}", mgT[:].rearrange("p k t -> p (k t)"), [128, 8 * 512], BF16, [mgT_t])
        rx = s * SEQ + tg * 512
        S.dma("sp", lambda e: e.dma_start(out=xr[0][:], in_=I.x[rx:rx + 128, :]), w=[xr_t[0]])
        for ti in range(4):
            nb[0] += 1
            r0 = s * SEQ + tg * 512 + ti * 128
            x_, x_t = xr[ti % 2], xr_t[ti % 2]
            h_, h_t = hh[ti % 2], hh_t[ti % 2]
            if ti + 1 < 4:
                S.dma("sp", lambda e: e.dma_start(out=xr[(ti + 1) % 2][:], in_=I.x[r0 + 128:r0 + 256, :]), w=[xr_t[(ti + 1) % 2]])
            ph = (ps[6], ps[7])
            pht = [pst[6], pst[7]]
            for hf in range(2):
                S.mm([lambda e, k=k, hf=hf: e.matmul(ph[hf][:, :], lhsT=mgT[:, k, ti * 128:(ti + 1) * 128],
                                                      rhs=wout[:, k, hf * 512:(hf + 1) * 512], start=(k == 0), stop=(k == 7))
                      for k in range(8)], r=[mgT_t, tw], w=[pht[hf]])
                S.op("dve", lambda e, hf=hf: e.tensor_tensor(out=h_[:, hf * 512:(hf + 1) * 512], in0=ph[hf][:, :],
                                                             in1=M.bc["g1"][:, hf * 512:(hf + 1) * 512], op=ALU.mult),
                     r=[pht[hf], M.t], w=[h_t])
            S.op("dve", lambda e: e.tensor_tensor(out=h_[:], in0=h_[:], in1=x_[:], op=ALU.add), r=[h_t, x_t], w=[h_t])
            phase_post_h(C, s, tg * 4 + ti, h_, h_t)
        S.barrier()
        loc.close()
        C.scope = ssd_scope


def phase_post_h(C, s, ti, h_, h_t):
    S = C.S
    r0 = s * SEQ + ti * 128
    S.dma("sp", lambda e: e.dma_start(out=C.h_scr[r0:r0 + 128, :], in_=h_[:]), r=[h_t], w=[C.h_scr_t])


def rms_rstd(C, src, src_t, ss, junk, junk_t, dim):
    S = C.S
    S.op("act", lambda e: e.activation(out=junk[:], in_=src[:], func=AF.Square, accum_out=ss[:, 0:1]), r=[src_t], w=[junk_t])
    S.op("dve", lambda e: e.tensor_scalar(out=ss[:, 1:2], in0=ss[:, 0:1], scalar1=1.0 / dim, scalar2=EPS,
                                          op0=ALU.mult, op1=ALU.add), r=[junk_t], w=[junk_t])
    S.op("act", lambda e: e.sqrt(out=ss[:, 1:2], in_=ss[:, 1:2]), r=[junk_t], w=[junk_t])
    S.op("dve", lambda e: e.reciprocal(out=ss[:, 1:2], in_=ss[:, 1:2]), r=[junk_t], w=[junk_t])


def phase_route(C):
    nc, S, sb, I, K, M = C.nc, C.S, C.sb, C.I, C.K, C.M
    ps, pst = C.ps, C.pst
    R = C.R
    tw = T("route_w", multi=True)
    wr = sb("wr", [128, 8, NE])
    wgus = sb("wgus", [128, 8, 512], BF16)
    wds = sb("wds", [128, 2, D], BF16)
    rb_bc = sb("rb_bc", [128, NE])
    iota = sb("iota", [128, NE])
    aid = sb("aid", [128, NTOK // 128, 8], I32)
    big = sb("big", [128, 4 * NE], I32)
    S.dma("sp", lambda e: e.dma_start(out=wr[:], in_=I.w_router.rearrange("(k p) n -> p k n", p=128)), w=[tw])
    S.dma("pool", lambda e: e.dma_start(out=wgus[:, :, 0:256], in_=I.w_gate_s.rearrange("(k p) n -> p k n", p=128)), w=[tw])
    S.dma("pool", lambda e: e.dma_start(out=wgus[:, :, 256:512], in_=I.w_up_s.rearrange("(k p) n -> p k n", p=128)), w=[tw])
    S.dma("pool", lambda e: e.dma_start(out=wds[:], in_=I.w_down_s.rearrange("(k p) n -> p k n", p=128)), w=[tw])
    S.dma("sp", lambda e: e.dma_start(out=rb_bc[:], in_=I.rbias_row.partition_broadcast(128)), w=[tw])
    S.dma("sp", lambda e: e.dma_start(out=iota[:], in_=I.iota_row.partition_broadcast(128)), w=[tw])
    S.dma("sp", lambda e: e.dma_start(out=aid[:], in_=I.aid), w=[tw])
    S.dma("sp", lambda e: e.dma_start(out=big[:], in_=I.bigtab), w=[tw])
    S.dma("sp", lambda e: e.dma_start(out=C.slot_info, in_=big[:]), r=[tw], w=[C.slot_t])
    base = sb("cnt_base", [128, NE])
    base_t = T("cnt_base")
    S.op("dve", lambda e: e.memset(base[:], 0.0), w=[base_t])
    hin = [sb(f"hin{i}", [128, D]) for i in range(2)]
    hin_t = [T(f"hin{i}") for i in range(2)]
    junk = sb("rjunk", [128, D])
    junk_t = T("rjunk")
    ss2 = [sb(f"rss{i}", [128, 2]) for i in range(2)]
    DB = {}
    for nm, shp, dt_ in (("u2", [128, D], F32), ("u2b", [128, D], BF16), ("u2Tf", [128, 8, 128], F32), ("u2Tb", [128, 8, 128], BF16),
                         ("sc", [128, NE], F32), ("bi", [128, NE], F32), ("mk", [128, 8, 32], F32), ("posf", [128, NE], F32),
                         ("mask8", [128, NE], BF16), ("rj", [128, NE], F32), ("m8g", [128, 8, 8], F32), ("sm", [128, 12, 8], F32),
                         ("smi", [128, 4, 8], I32), ("idx8", [128, 8], U32)):
        DB[nm] = [sb(f"r_{nm}{i}", shp, dt_) for i in range(2)]
    DBT = {nm: [T(f"r_{nm}{i}") for i in range(2)] for nm in ("u2", "u2b", "u2T", "rt")}
    DBTT = [{n_: T(f"rr_{n_}{i}", multi=(n_ in ("m8g", "sel", "pos"))) for n_ in
             ("sc", "bi", "m8g", "g", "mk", "v8", "idx", "mask", "posf", "sel", "pos")} for i in range(2)]
    rj2s = [sb(f"r_rj2_{i}", [128, NE]) for i in range(2)]
    sgl = sb("s_sgl", [128, 256])
    hsb = sb("s_hsb", [128, 256], BF16)
    hsT = sb("s_hsT", [128, 2, 128], BF16)
    ysh = sb("s_ysh", [128, D])
    sh_t = T("shared_tmp")
    for s_ in range(NSEQ):
        with ExitStack() as loc:
            C.scope = loc
            M.bc = {nm: sb(f"bc_{nm}", [128, D]) for nm in ("sh2", "sc2")}
            with ExitStack() as loc2:
                C.scope = loc2
                phase_mod(C, s_, which=("sh2", "sc2"))
                S.barrier()
            C.scope = loc
            def bind(ti):
                gt = s_ * (SEQ // 128) + ti
                r0 = gt * 128
                q_ = gt % 2
                return dict(gt=gt, r0=r0, q_=q_)

            def front_stage(ti):
                    gt = s_ * (SEQ // 128) + ti
                    r0 = gt * 128
                    h_, h_t = hin[gt % 2], hin_t[gt % 2]
                    q_ = gt % 2
                    ss = ss2[q_]
                    u2, u2b, u2Tf, u2Tb = DB["u2"][q_], DB["u2b"][q_], DB["u2Tf"][q_], DB["u2Tb"][q_]
                    sc, bi, mk, posf, mask8, rj = DB["sc"][q_], DB["bi"][q_], DB["mk"][q_], DB["posf"][q_], DB["mask8"][q_], DB["rj"][q_]
                    m8g, sm, smi, idx8 = DB["m8g"][q_], DB["sm"][q_], DB["smi"][q_], DB["idx8"][q_]
                    u2_t, u2b_t, u2T_t, rt = DBT["u2"][q_], DBT["u2b"][q_], DBT["u2T"][q_], DBT["rt"][q_]
                    rj2 = rj2s[q_]
                    S.dma("sp", lambda e: e.dma_start(out=h_[:], in_=C.h_scr[r0:r0 + 128, :]), r=[C.h_scr_t], w=[h_t])
                    rms_rstd(C, h_, h_t, ss, junk, junk_t, D)
                    S.op("dve", lambda e: e.scalar_tensor_tensor(out=u2[:], in0=h_[:], scalar=ss[:, 1:2], in1=M.bc["sc2"][:],
                                                                 op0=ALU.mult, op1=ALU.mult), r=[h_t, junk_t, M.t], w=[u2_t])
                    S.op("dve", lambda e: e.tensor_tensor(out=u2[:], in0=u2[:], in1=M.bc["sh2"][:], op=ALU.add), r=[u2_t, M.t], w=[u2_t])
                    S.op("act", lambda e: e.copy(out=u2b[:], in_=u2[:]), r=[u2_t], w=[u2b_t])
                    S.dma("pool", lambda e: e.dma_start(out=C.u2_rows[r0:r0 + 128, :], in_=u2b[:]), r=[u2b_t], w=[C.u2rows_t])
                    for hf in range(2):
                        S.mm([lambda e, k=k: e.transpose(out=ps[hf][:, (k % 4) * 128:(k % 4 + 1) * 128], in_=u2[:, k * 128:(k + 1) * 128],
                                                          identity=K.ident_f[:]) for k in range(hf * 4, hf * 4 + 4)],
                             r=[u2_t, K.t], w=[pst[hf]])
                        S.op("act", lambda e, hf=hf: e.copy(out=u2Tf[:, hf * 4:(hf + 1) * 4, :].rearrange("p k t -> p (k t)"), in_=ps[hf][:, :]),
                             r=[pst[hf]], w=[u2T_t])
                        S.op("act", lambda e, hf=hf: e.copy(out=u2Tb[:, hf * 4:(hf + 1) * 4, :].rearrange("p k t -> p (k t)"), in_=ps[hf][:, :]),
                             r=[pst[hf]], w=[u2T_t])
                    S.mm([lambda e, k=k: e.matmul(ps[2][:, 0:NE], lhsT=u2Tf[:, k, :], rhs=wr[:, k, :], start=(k == 0), stop=(k == 7))
                          for k in range(8)], r=[u2T_t, tw], w=[pst[2]])
                    w_ = [rt]
                    TT = DBTT[q_]
                    t_sc, t_bi, t_m8g, t_g, t_mk, t_v8, t_idx, t_mask, t_posf, t_sel, t_pos = (TT[n_] for n_ in (
                        "sc", "bi", "m8g", "g", "mk", "v8", "idx", "mask", "posf", "sel", "pos"))
                    S.op("act", lambda e: e.activation(out=sc[:], in_=ps[2][:, 0:NE], func=AF.Sigmoid), r=[pst[2]], w=[t_sc])
                    S.op("dve", lambda e: e.tensor_tensor(out=bi[:], in0=sc[:], in1=rb_bc[:], op=ALU.add), r=[t_sc, tw], w=[t_bi])
                    S.mm([lambda e, k=k: e.matmul(ps[4][:, :], lhsT=u2Tb[:, k, :], rhs=wgus[:, k, :], start=(k == 0), stop=(k == 7))
                          for k in range(8)], r=[u2T_t, tw], w=[pst[4]])
                    S.op("act", lambda e: e.activation(out=sgl[:], in_=ps[4][:, 0:256], func=AF.Silu), r=[pst[4]], w=[sh_t])
                    S.op("dve", lambda e: e.tensor_tensor(out=hsb[:], in0=sgl[:], in1=ps[4][:, 256:512], op=ALU.mult), r=[pst[4], sh_t], w=[sh_t])
                    p5b = ps[5][:, :].bitcast(BF16)
                    S.mm([lambda e, f=f: e.transpose(out=p5b[:, f * 128:(f + 1) * 128], in_=hsb[:, f * 128:(f + 1) * 128],
                                                      identity=K.ident_b[:]) for f in range(2)], r=[sh_t, K.t], w=[pst[5]])
                    S.op("act", lambda e: e.copy(out=hsT[:].rearrange("p f t -> p (f t)"), in_=p5b[:, 0:256]), r=[pst[5]], w=[sh_t])
                    for hf in range(2):
                        S.mm([lambda e, f=f, hf=hf: e.matmul(ps[6 + hf][:, :], lhsT=hsT[:, f, :], rhs=wds[:, f, hf * 512:(hf + 1) * 512],
                                                              start=(f == 0), stop=(f == 1)) for f in range(2)], r=[sh_t, tw], w=[pst[6 + hf]])
                    S.op("act", lambda e: e.copy(out=ysh[:, 0:512], in_=ps[6][:, :]), r=[pst[6]], w=[sh_t])
                    S.op("act", lambda e: e.copy(out=ysh[:, 512:1024], in_=ps[7][:, :]), r=[pst[7]], w=[sh_t])
                    S.dma("pool", lambda e: e.dma_start(out=C.sh_out[r0:r0 + 128, :], in_=ysh[:]), r=[sh_t], w=[C.sh_out_t])

            def back_stage(ti):
                    gt = s_ * (SEQ // 128) + ti
                    r0 = gt * 128
                    h_, h_t = hin[gt % 2], hin_t[gt % 2]
                    q_ = gt % 2
                    ss = ss2[q_]
                    u2, u2b, u2Tf, u2Tb = DB["u2"][q_], DB["u2b"][q_], DB["u2Tf"][q_], DB["u2Tb"][q_]
                    sc, bi, mk, posf, mask8, rj = DB["sc"][q_], DB["bi"][q_], DB["mk"][q_], DB["posf"][q_], DB["mask8"][q_], DB["rj"][q_]
                    m8g, sm, smi, idx8 = DB["m8g"][q_], DB["sm"][q_], DB["smi"][q_], DB["idx8"][q_]
                    u2_t, u2b_t, u2T_t, rt = DBT["u2"][q_], DBT["u2b"][q_], DBT["u2T"][q_], DBT["rt"][q_]
                    rj2 = rj2s[q_]
                    w_ = [rt]
                    TT = DBTT[q_]
                    t_sc, t_bi, t_m8g, t_g, t_mk, t_v8, t_idx, t_mask, t_posf, t_sel, t_pos = (TT[n_] for n_ in (
                        "sc", "bi", "m8g", "g", "mk", "v8", "idx", "mask", "posf", "sel", "pos"))
                    for g in range(8):
                        S.op("dve", lambda e, g=g: e.max(out=m8g[:, g, :], in_=bi[:, g * 32:(g + 1) * 32]), r=[t_bi], w=[t_m8g])
                    S.op("dve", lambda e: e.tensor_tensor(out=sm[:, 0, :], in0=m8g[:, :, 0], in1=m8g[:, :, 1], op=ALU.add), r=[t_m8g], w=[t_g])
                    S.op("dve", lambda e: e.max(out=sm[:, 1, :], in_=sm[:, 0, :]), r=[t_g], w=[t_g])
                    S.op("dve", lambda e: e.tensor_scalar(out=sm[:, 2, :], in0=sm[:, 0, :], scalar1=sm[:, 1, 3:4], scalar2=None,
                                                          op0=ALU.is_ge), r=[t_g], w=[t_g])
                    S.op("dve", lambda e: e.tensor_scalar(out=sm[:, 3, :], in0=sm[:, 2, :], scalar1=100.0, scalar2=-100.0,
                                                          op0=ALU.mult, op1=ALU.add), r=[t_g], w=[t_g])
                    S.op("dve", lambda e: e.tensor_tensor(out=mk[:], in0=bi[:].rearrange("p (g c) -> p g c", g=8),
                                                          in1=sm[:, 2, :].unsqueeze(2).to_broadcast([128, 8, 32]), op=ALU.mult),
                         r=[t_g, t_bi], w=[t_mk])
                    S.op("dve", lambda e: e.tensor_tensor(out=mk[:], in0=mk[:], in1=sm[:, 3, :].unsqueeze(2).to_broadcast([128, 8, 32]),
                                                          op=ALU.add), r=[t_g, t_mk], w=[t_mk])
                    mkf = mk[:].rearrange("p g c -> p (g c)")
                    S.op("dve", lambda e: e.max(out=sm[:, 4, :], in_=mkf), r=[t_mk], w=[t_v8])
                    S.op("dve", lambda e: e.tensor_scalar(out=mask8[:], in0=mkf, scalar1=sm[:, 4, 7:8], scalar2=None, op0=ALU.is_ge),
                         r=[t_mk, t_v8], w=[t_mask])
                    S.op("dve", lambda e: e.tensor_tensor(out=rj[:], in0=sc[:], in1=mask8[:], op=ALU.mult), r=[t_sc, t_mask], w=[t_sel])
                    S.op("dve", lambda e: e.max(out=sm[:, 6, :], in_=rj[:]), r=[t_sel], w=[t_sel])
                    S.op("dve", lambda e: e.max_index(out=idx8[:], in_max=sm[:, 6, :], in_values=rj[:]), r=[t_sel], w=[t_idx])
                    S.op("dve", lambda e: e.tensor_copy(out=sm[:, 5, :], in_=idx8[:]), r=[t_idx], w=[t_idx])
                    S.mm([lambda e: e.matmul(ps[3][:, 0:NE], lhsT=K.cm_b[:, 5, :], rhs=mask8[:], start=True, stop=True),
                          lambda e: e.matmul(ps[3][:, NE:2 * NE], lhsT=K.ones_b[:], rhs=mask8[:], start=True, stop=True)],
                         r=[t_mask, K.t], w=[pst[3]])
                    S.op("dve", lambda e: e.tensor_tensor(out=posf[:], in0=ps[3][:, 0:NE], in1=base[:], op=ALU.add),
                         r=[pst[3], base_t], w=[t_posf])
                    S.op("dve", lambda e: e.tensor_tensor(out=base[:], in0=ps[3][:, NE:2 * NE], in1=base[:], op=ALU.add),
                         r=[pst[3], t_posf], w=[base_t])
                    for k in range(8):
                        S.op("dve", lambda e, k=k: e.scalar_tensor_tensor(out=rj2[:], in0=iota[:], scalar=sm[:, 5, k:k + 1], in1=posf[:],
                                                                          op0=ALU.is_equal, op1=ALU.mult, accum_out=sm[:, 7, k:k + 1]),
                             r=[t_idx, t_posf, tw], w=[t_pos])
                    w_ = [rt]
                    rr = w_ + [t_idx, t_sel, t_pos]
                    S.op("dve", lambda e: e.tensor_copy(out=smi[:, 0, :], in_=sm[:, 7, :]), r=rr, w=w_)
                    S.op("dve", lambda e: e.tensor_scalar(out=smi[:, 1, :], in0=smi[:, 0, :], scalar1=127, scalar2=None,
                                                          op0=ALU.bitwise_and), r=w_, w=w_)
                    S.op("dve", lambda e: e.tensor_scalar(out=smi[:, 2, :], in0=smi[:, 0, :], scalar1=7, scalar2=None,
                                                          op0=ALU.arith_shift_right), r=w_, w=w_)
                    S.op("dve", lambda e: e.tensor_copy(out=sm[:, 10, :], in_=smi[:, 1, :]), r=w_, w=w_)
                    S.op("dve", lambda e: e.tensor_copy(out=sm[:, 11, :], in_=smi[:, 2, :]), r=w_, w=w_)
                    S.op("dve", lambda e: e.scalar_tensor_tensor(out=sm[:, 8, :], in0=sm[:, 5, :], scalar=4.0, in1=sm[:, 11, :],
                                                                 op0=ALU.mult, op1=ALU.add), r=rr, w=w_)
                    S.op("dve", lambda e: e.scalar_tensor_tensor(out=sm[:, 8, :], in0=sm[:, 10, :], scalar=float(4 * NE), in1=sm[:, 8, :],
                                                                 op0=ALU.mult, op1=ALU.add), r=w_, w=w_)
                    S.op("dve", lambda e: e.tensor_scalar(out=sm[:, 9, :], in0=sm[:, 7, :], scalar1=float(CAP), scalar2=None,
                                                          op0=ALU.is_lt), r=rr, w=w_)
                    S.op("dve", lambda e: e.tensor_scalar(out=sm[:, 10, :], in0=sm[:, 9, :], scalar1=-1.0e9, scalar2=1.0e9,
                                                          op0=ALU.mult, op1=ALU.add), r=w_, w=w_)
                    S.op("dve", lambda e: e.tensor_tensor(out=sm[:, 8, :], in0=sm[:, 8, :], in1=sm[:, 10, :], op=ALU.add), r=w_, w=w_)
                    S.op("dve", lambda e: e.tensor_copy(out=smi[:, 3, :], in_=sm[:, 8, :]), r=w_, w=w_)
                    S.op("dve", lambda e: e.reduce_sum(out=sm[:, 11, 0:1], in_=sm[:, 6, :], axis=AX.X), r=rr, w=w_)
                    S.op("dve", lambda e: e.reciprocal(out=sm[:, 11, 0:1], in_=sm[:, 11, 0:1]), r=w_, w=w_)
                    S.op("dve", lambda e: e.tensor_scalar(out=sm[:, 6, :], in0=sm[:, 6, :], scalar1=sm[:, 11, 0:1], scalar2=2.5,
                                                          op0=ALU.mult, op1=ALU.mult), r=rr, w=w_ + [t_sel])
                    S.op("dve", lambda e: e.tensor_tensor(out=R.wsel[:, gt, :], in0=sm[:, 6, :], in1=sm[:, 9, :], op=ALU.mult),
                         r=w_ + [t_sel], w=[R.wsel_t])
                    for k in range(8):
                        S.dma("pool", lambda e, k=k: e.indirect_dma_start(
                            out=C.slot_info_flat, out_offset=bass.IndirectOffsetOnAxis(ap=smi[:, 3, k:k + 1], axis=0),
                            in_=aid[:, gt, k:k + 1], in_offset=None, bounds_check=R.reg_slot, oob_is_err=False),
                            r=[rt, tw, C.slot_t], w=[C.slot_sc])
                    if "route" in C.dbg:
                        dump(C, f"ridx{gt}", sm[:, 5, :], [128, 8], F32, [rt])
                        dump(C, f"rslot{gt}", sm[:, 8, :], [128, 8], F32, [rt])

            NT = SEQ // 128
            front_stage(0)
            for ti in range(NT):
                if ti + 1 < NT:
                    front_stage(ti + 1)
                back_stage(ti)
            S.barrier()


def phase_experts(C):
    nc, S, sb, I, K, R = C.nc, C.S, C.sb, C.I, C.K, C.R
    ps, pst = C.ps, C.pst
    NB = CAP // 128
    NBLK = NE * NB
    SI = sb("SI", [128, NE * NB], I32)
    TK = sb("TK", [128, NE * NB], I32)
    si_t = T("SI")
    S.dma("sp", lambda e: e.dma_start(out=SI[:], in_=C.slot_info), r=[C.slot_t, C.slot_sc], w=[si_t])
    S.op("dve", lambda e: e.tensor_scalar(out=TK[:], in0=SI[:], scalar1=3, scalar2=None, op0=ALU.arith_shift_right),
         r=[si_t], w=[si_t])
    NW = 3
    wg = [sb(f"wg{i}", [128, 8, 256], BF16) for i in range(NW)]
    wu = [sb(f"wu{i}", [128, 8, 256], BF16) for i in range(NW)]
    wd = [sb(f"wd{i}", [128, 2, D], BF16) for i in range(NW)]
    wg_t = [T(f"ewg{i}") for i in range(NW)]
    wu_t = [T(f"ewu{i}") for i in range(NW)]
    wd_t = [T(f"ewd{i}") for i in range(NW)]
    xg = [[sb(f"xg{i}_{b}", [128, D], BF16) for b in range(NB)] for i in range(2)]
    xg_t = [[T(f"xg{i}_{b}") for b in range(NB)] for i in range(2)]
    for i in range(2):
        for b in range(NB):
            S.op("dve", lambda e, i=i, b=b: e.memset(xg[i][b][:], 0.0), w=[xg_t[i][b]])
    xgT = [sb(f"xgT{i}", [128, 8, 128], BF16) for i in range(2)]
    xgT_t = [T(f"xgT{i}") for i in range(2)]
    sgl = [sb(f"esgl{i}", [128, 256]) for i in range(2)]
    sgl_t = [T(f"esgl{i}") for i in range(2)]
    hb = [sb(f"ehb{i}", [128, 256], BF16) for i in range(2)]
    hb_t = [T(f"ehb{i}") for i in range(2)]
    hT = [sb(f"ehT{i}", [128, 2, 128], BF16) for i in range(2)]
    hT_t = [T(f"ehT{i}") for i in range(2)]
    NY = 3
    yo = [sb(f"eyo{i}", [128, D]) for i in range(NY)]
    yo_t = [T(f"eyo{i}") for i in range(NY)]
    yo_t2 = [T(f"eyo{i}b") for i in range(NY)]
    p4b = ps[4][:, :].bitcast(BF16)
    pht_t = [T("psHT0"), T("psHT1")]

    def load_weights(e_):
        i = e_ % NW
        S.dma("pool", lambda e: e.dma_start(out=wg[i][:], in_=I.w_gate_e[e_].rearrange("(p k) n -> p k n", p=128)), w=[wg_t[i]])
        S.dma("pool", lambda e: e.dma_start(out=wu[i][:], in_=I.w_up_e[e_].rearrange("(p k) n -> p k n", p=128)), w=[wu_t[i]])
        S.dma("pool", lambda e: e.dma_start(out=wd[i][:], in_=I.w_down_e[e_].rearrange("(p k) n -> p k n", p=128)), w=[wd_t[i]])

    def gathers(e_):
        i = e_ % 2
        for b in range(NB):
            col = e_ * NB + b
            S.dma("pool", lambda e, b=b, col=col: e.indirect_dma_start(
                out=xg[i][b][:, :], out_offset=None, in_=C.u2_rows,
                in_offset=bass.IndirectOffsetOnAxis(ap=TK[:, col:col + 1], axis=0), bounds_check=R.reg_tok, oob_is_err=False),
                r=[si_t, C.u2rows_t], w=[xg_t[i][b]])

    xgTe = [sb(f"xgTe{i}", [128, 8, CAP], BF16) for i in range(2)]
    xgTe_t = [[T(f"xgTe{i}_{b}") for b in range(NB)] for i in range(2)]
    sge = [sb(f"sge{i}", [128, CAP]) for i in range(2)]
    sge_t = [T(f"sge{i}") for i in range(2)]
    hTe = [sb(f"hTe{i}", [128, 2, CAP], BF16) for i in range(2)]
    hTe_t = [[T(f"hTe{i}_{f}") for f in range(2)] for i in range(2)]

    def stA(e_, b):
        n = e_ * NB + b
        j = n % 2
        pxb = ps[j][:, :].bitcast(BF16)
        S.mm([lambda e, k=k: e.transpose(out=pxb[:, k * 128:(k + 1) * 128], in_=xg[e_ % 2][b][:, k::8],
                                          identity=K.ident_b[:]) for k in range(8)], r=[xg_t[e_ % 2][b], K.t], w=[pst[j]])
        dst = xgTe[e_ % 2][:, :, b * 128:(b + 1) * 128]
        src = pxb.rearrange("p (k t) -> p k t", k=8)
        if b % 2 == 0:
            S.op("dve", lambda e: e.tensor_copy(out=dst, in_=src), r=[pst[j]], w=[xgTe_t[e_ % 2][b]])
        else:
            S.op("act", lambda e: e.copy(out=dst, in_=src), r=[pst[j]], w=[xgTe_t[e_ % 2][b]])

    def stB(e_, c):
        w = e_ % NW
        i = e_ % 2
        ww, wt = (wg, wg_t) if c < 2 else (wu, wu_t)
        f = c % 2
        S.mm([lambda e, k=k: e.matmul(ps[2 + c][:, :], lhsT=ww[w][:, k, f::2], rhs=xgTe[i][:, k, :], start=(k == 0), stop=(k == 7))
              for k in range(8)], r=xgTe_t[i] + [wt[w]], w=[pst[2 + c]])
        if c < 2:
            S.op("act", lambda e: e.activation(out=sge[c][:], in_=ps[2 + c][:, :], func=AF.Silu), r=[pst[2 + c]], w=[sge_t[c]])
        else:
            S.op("dve", lambda e: e.tensor_tensor(out=hTe[i][:, f, :], in0=sge[f][:], in1=ps[2 + c][:, :], op=ALU.mult),
                 r=[pst[2 + c], sge_t[f]], w=[hTe_t[i][f]])

    def stAB(eA, eB, b):
        n = eA * NB + b
        j = n % 2
        pxb = ps[j][:, :].bitcast(BF16)
        c = b
        w = eB % NW
        i = eB % 2
        ww, wt = (wg, wg_t) if c < 2 else (wu, wu_t)
        f = c % 2
        fns = []
        for k in range(8):
            fns.append(lambda e, k=k: e.matmul(ps[2 + c][:, :], lhsT=ww[w][:, k, f::2], rhs=xgTe[i][:, k, :],
                                               start=(k == 0), stop=(k == 7)))
            fns.append(lambda e, k=k: e.transpose(out=pxb[:, k * 128:(k + 1) * 128], in_=xg[eA % 2][b][:, k::8],
                                                  identity=K.ident_b[:]))
        S.mm(fns, r=[xg_t[eA % 2][b], K.t] + xgTe_t[i] + [wt[w]], w=[pst[j], pst[2 + c]])
        dst = xgTe[eA % 2][:, :, b * 128:(b + 1) * 128]
        src = pxb.rearrange("p (k t) -> p k t", k=8)
        if b % 2 == 0:
            S.op("dve", lambda e: e.tensor_copy(out=dst, in_=src), r=[pst[j]], w=[xgTe_t[eA % 2][b]])
        else:
            S.op("act", lambda e: e.copy(out=dst, in_=src), r=[pst[j]], w=[xgTe_t[eA % 2][b]])
        if c < 2:
            S.op("act", lambda e: e.activation(out=sge[c][:], in_=ps[2 + c][:, :], func=AF.Silu), r=[pst[2 + c]], w=[sge_t[c]])
        else:
            S.op("dve", lambda e: e.tensor_tensor(out=hTe[i][:, f, :], in0=sge[f][:], in1=ps[2 + c][:, :], op=ALU.mult),
                 r=[pst[2 + c], sge_t[f]], w=[hTe_t[i][f]])

    def stD(e_, b):
        n = e_ * NB + b
        i = e_ % 2
        w = e_ % NW
        y = n % NY
        for hf in range(2):
            S.mm([lambda e, f=f, hf=hf: e.matmul(ps[6 + hf][:, :], lhsT=hTe[i][:, f, b * 128:(b + 1) * 128],
                                                  rhs=wd[w][:, f, hf * 512:(hf + 1) * 512], start=(f == 0), stop=(f == 1))
                  for f in range(2)], r=hTe_t[i] + [wd_t[w]], w=[pst[6 + hf]])
        S.op("act", lambda e: e.copy(out=yo[y][:, 0:512], in_=ps[6][:, :]), r=[pst[6]], w=[yo_t[y]])
        S.op("dve", lambda e: e.tensor_copy(out=yo[y][:, 512:1024], in_=ps[7][:, :]), r=[pst[7]], w=[yo_t2[y]])
        S.dma("pool", lambda e: e.indirect_dma_start(
            out=C.ye2, out_offset=bass.IndirectOffsetOnAxis(ap=SI[:, n:n + 1], axis=0), in_=yo[y][:, :], in_offset=None,
            bounds_check=R.reg_aid, oob_is_err=False), r=[yo_t[y], yo_t2[y], si_t], w=[C.ye2_t])

    load_weights(0)
    gathers(0)
    load_weights(1)
    gathers(1)
    for b in range(NB):
        stA(0, b)
    for e_ in range(NE + 1):
        if e_ + 2 < NE:
            gathers(e_ + 2)
        for b in range(NB):
            if e_ + 1 < NE and e_ < NE:
                stAB(e_ + 1, e_, b)
            else:
                if e_ + 1 < NE:
                    stA(e_ + 1, b)
                if e_ < NE:
                    stB(e_, b)
            if e_ >= 1:
                stD(e_ - 1, b)
        if e_ + 2 < NE:
            load_weights(e_ + 2)


def phase_final(C):
    nc, S, sb, I, K, M, R = C.nc, C.S, C.sb, C.I, C.K, C.M, C.R
    M.g2_bc = [sb(f"bc_g2{b}", [128, D]) for b in range(NSEQ)]
    nfin_bc = sb("nfin_bc", [128, D])
    S.dma("sp", lambda e: e.dma_start(out=nfin_bc[:], in_=I.nfin_row.partition_broadcast(128)), w=[M.t])
    outer = C.scope
    with ExitStack() as loc:
        C.scope = loc
        phase_mod(C, "g2")
        S.barrier()
    C.scope = outer
    hin = [sb(f"fh{i}", [128, D]) for i in range(2)]
    hin_t = [T(f"fh{i}") for i in range(2)]
    shd = [sb(f"fsh{i}", [128, D]) for i in range(2)]
    shd_t = [T(f"fsh{i}") for i in range(2)]
    ye = [sb(f"fye{i}", [128, 8, D]) for i in range(2)]
    ye_t = [T(f"fye{i}") for i in range(2)]
    junk = sb("fjunk", [128, D])
    junk_t = T("fjunk")
    ss = [sb(f"fss{i}", [128, 2]) for i in range(2)]
    ob = [sb(f"fo{i}", [128, D]) for i in range(2)]
    ob_t = [T(f"fo{i}") for i in range(2)]
    for gt in range(NTOK // 128):
        s_ = gt // (SEQ // 128)
        r0 = gt * 128
        h_, h_t = hin[gt % 2], hin_t[gt % 2]
        a_, a_t = shd[gt % 2], shd_t[gt % 2]
        o_, o_t = ob[gt % 2], ob_t[gt % 2]
        y_, y_t = ye[gt % 2], ye_t[gt % 2]
        S.dma("sp", lambda e: e.dma_start(out=h_[:], in_=C.h_scr[r0:r0 + 128, :]), r=[C.h_scr_t], w=[h_t])
        S.dma("sp", lambda e: e.dma_start(out=a_[:], in_=C.sh_out[r0:r0 + 128, :]), r=[C.sh_out_t], w=[a_t])
        S.dma("sp", lambda e: e.dma_start(out=y_[:], in_=C.ye2[r0 * 8:(r0 + 128) * 8, :].rearrange("(t k) d -> t k d", k=8)),
              r=[C.ye2_t], w=[y_t])
        for k in range(8):
            S.op("dve", lambda e, k=k: e.scalar_tensor_tensor(out=a_[:], in0=y_[:, k, :], scalar=R.wsel[:, gt, k:k + 1],
                                                              in1=a_[:], op0=ALU.mult, op1=ALU.add),
                 r=[y_t, R.wsel_t, a_t], w=[a_t])
        S.op("dve", lambda e: e.tensor_tensor(out=a_[:], in0=a_[:], in1=M.g2_bc[s_][:], op=ALU.mult), r=[a_t, M.t], w=[a_t])
        S.op("dve", lambda e: e.tensor_tensor(out=a_[:], in0=a_[:], in1=h_[:], op=ALU.add), r=[a_t, h_t], w=[a_t])
        rms_rstd(C, a_, a_t, ss[gt % 2], junk, junk_t, D)
        S.op("dve", lambda e: e.scalar_tensor_tensor(out=o_[:], in0=a_[:], scalar=ss[gt % 2][:, 1:2], in1=nfin_bc[:],
                                                     op0=ALU.mult, op1=ALU.mult), r=[a_t, junk_t, M.t], w=[o_t])
        S.dma("pool", lambda e: e.dma_start(out=C.out[r0:r0 + 128, :], in_=o_[:]), r=[o_t])


def host_inputs(inputs):
    f = np.float32
    x = np.asarray(inputs["x"], f)
    c = np.asarray(inputs["c"], f)
    pos = np.asarray(inputs["positions"], np.int32)
    shared = {}
    shared["w_mod"] = np.ascontiguousarray(inputs["w_mod"][0], f)
    bm = np.asarray(inputs["b_mod"][0], f)
    shared["b_modT"] = np.ascontiguousarray(bm.reshape(48, 128).T)
    shared["b_mod_row"] = bm.reshape(1, -1)
    shared["nmwT"] = np.ascontiguousarray(np.asarray(inputs["norm_mix_w"][0], f).reshape(8, 128).T)
    shared["nfw_row"] = np.asarray(inputs["norm_ffn_w"][0], f).reshape(1, -1)
    shared["nfin_row"] = np.asarray(inputs["norm_final_w"], f).reshape(1, -1)
    shared["w_in"] = np.ascontiguousarray(inputs["w_in"][0], f)
    shared["ident"] = np.eye(128, dtype=f)
    shared["w_ba"] = np.ascontiguousarray(inputs["w_branch_attn"][0], f)
    shared["w_bs"] = np.ascontiguousarray(inputs["w_branch_ssm"][0], f)
    shared["w_out"] = np.ascontiguousarray(inputs["w_out"][0], f)
    cw = np.asarray(inputs["conv_w"][0], f)
    shared["conv_wT"] = np.ascontiguousarray(cw.reshape(4, 16, 128).transpose(2, 1, 0))
    shared["conv_bT"] = np.ascontiguousarray(np.asarray(inputs["conv_b"][0], f).reshape(16, 128).T)
    shared["snwT"] = np.ascontiguousarray(np.asarray(inputs["ssm_norm_w"][0], f).reshape(8, 128).T)
    shared["hrow"] = np.concatenate([np.asarray(inputs[k][0], f) for k in ("dt_bias", "a_log", "d_skip")]).reshape(1, 48)
    cm = np.zeros((128, 6, 128), f)
    for m in range(32):
        cm[(m + 16) % 32, 0, m] = 1.0
    kk, qq = np.meshgrid(np.arange(128), np.arange(128), indexing="ij")
    cm[:, 1, :] = np.where(kk <= qq, 0.0, NEG)
    cm[:, 2, :] = np.where(kk >= qq, 0.0, NEG)
    cm[:, 3, :] = (kk <= qq).astype(f)
    cm[:, 4, :] = (kk > qq).astype(f)
    cm[:, 5, :] = (kk < qq).astype(f)
    shared["w_router"] = np.ascontiguousarray(inputs["w_router"][0], f)
    shared["rbias_row"] = np.asarray(inputs["router_bias"][0], f).reshape(1, -1)
    shared["iota_row"] = np.arange(NE, dtype=f).reshape(1, -1)
    tokid = (np.arange(NTOK // 128)[None, :, None] * 128 + np.arange(128)[:, None, None])
    shared["aid"] = np.ascontiguousarray((tokid * 8 + np.arange(8)[None, None, :]).astype(np.int32))
    shared["bigtab"] = np.full((128, 4 * NE), 0x3FFFFFF8, np.int32)
    shared["w_gate_s"] = np.ascontiguousarray(inputs["w_gate_s"][0], f)
    shared["w_gate_e"] = np.ascontiguousarray(inputs["w_gate_e"][0], f)
    shared["w_up_e"] = np.ascontiguousarray(inputs["w_up_e"][0], f)
    shared["w_down_e"] = np.ascontiguousarray(inputs["w_down_e"][0], f)
    shared["w_up_s"] = np.ascontiguousarray(inputs["w_up_s"][0], f)
    shared["w_down_s"] = np.ascontiguousarray(inputs["w_down_s"][0], f)
    shared["cmat"] = cm
    rc = np.zeros((128, 2), f)
    half = 16
    invf = (500000.0 ** (-np.arange(half, dtype=np.float32) / half)).astype(f)
    rc[:32, 0] = np.concatenate([invf, invf])
    rc[:32, 1] = np.concatenate([-np.ones(16, f), np.ones(16, f)])
    shared["ropec"] = rc
    maps = []
    for cid in range(NCORES):
        m = dict(shared)
        m["x"] = np.ascontiguousarray(x[cid * NSEQ:(cid + 1) * NSEQ].reshape(NTOK, D))
        cc = c[cid * NSEQ:(cid + 1) * NSEQ]
        m["cT"] = np.ascontiguousarray(cc.reshape(NSEQ, 8, 128).transpose(2, 1, 0))
        m["pos"] = np.ascontiguousarray(pos[cid * NSEQ:(cid + 1) * NSEQ])
        maps.append(m)
    return maps


_CACHE = {}


def kernel(**inputs):
    if "nc" not in _CACHE:
        _CACHE["nc"] = build()
    nc, C = _CACHE["nc"]
    maps = host_inputs(inputs)
    res = run_bass_kernel_spmd(nc, maps, core_ids=list(range(NCORES)))
    out = np.concatenate([r["out"].reshape(NSEQ, SEQ, D) for r in res.results], axis=0)
    return out.astype(np.float32)
```
